# Optimizing a Trainium2 kernel written in Bass

```python
import jax
import jax.numpy as jnp
from jax import lax
import numpy as np


D_MODEL = 1024
BATCH = 8
SEQ = 4096
DEPTH = 2

CHUNK = 64
Q_BLOCK = 128
D_PLE = 256
D_FF = 4 * D_MODEL
NORM_EPS = 1e-6

MLA_HEADS = 4
MLA_NOPE = 128
MLA_ROPE = 64
MLA_V = 128
MLA_Q_RANK = 256
MLA_KV_RANK = 128
ROPE_THETA = 10000.0

RW_HEADS = 8
RW_HEAD = 64
RW_DIM = RW_HEADS * RW_HEAD
RW_DECAY_LORA = 64
RW_AAA_LORA = 64
RW_GATE_LORA = 128
RW_LN_EPS = 64e-5

CA_HEADS = 8
CA_HEAD = 64
CA_DIM = CA_HEADS * CA_HEAD
CA_LEFT_CHUNKS = 8
CA_BAND = (CA_LEFT_CHUNKS + 1) * CHUNK
REL_MIN = -(CHUNK - 1)
REL_MAX = 256
REL_SIZE = REL_MAX - REL_MIN + 1

N_BRANCH = 3
BRANCH_DIM = 512
MIX_DIM = MLA_HEADS * MLA_V + RW_DIM + CA_DIM

MLA_COLS = MLA_Q_RANK + MLA_KV_RANK + MLA_ROPE
RW_COLS = 3 * RW_DIM + RW_DECAY_LORA + RW_AAA_LORA + RW_GATE_LORA
CA_COLS = 3 * CA_DIM
GATE_COLS = N_BRANCH * D_MODEL
IN_COLS = MLA_COLS + RW_COLS + CA_COLS + GATE_COLS
IN_SPLITS = (MLA_COLS, MLA_COLS + RW_COLS, MLA_COLS + RW_COLS + CA_COLS)
RW_SPLITS = (RW_DIM, 2 * RW_DIM, 3 * RW_DIM, 3 * RW_DIM + RW_DECAY_LORA,
             3 * RW_DIM + RW_DECAY_LORA + RW_AAA_LORA)

kernel_name = 'hybrid_mla_rwkv7_chunkattn_stream_block'

F32 = jnp.float32


def rms_norm(x, g):
    xf = x.astype(F32)
    y = xf * lax.rsqrt(jnp.mean(xf * xf, axis=-1, keepdims=True) + NORM_EPS)
    return (y * g.astype(F32)).astype(x.dtype)


def rope(x, positions):
    half = x.shape[-1] // 2
    inv_freq = 1.0 / (ROPE_THETA ** (jnp.arange(half, dtype=F32) / half))
    ang = positions.astype(F32)[..., None] * inv_freq
    ang = ang.reshape(ang.shape[:2] + (1,) * (x.ndim - 3) + (half,))
    cos, sin = jnp.cos(ang), jnp.sin(ang)
    x1 = x[..., :half].astype(F32)
    x2 = x[..., half:].astype(F32)
    return jnp.concatenate([x1 * cos - x2 * sin, x2 * cos + x1 * sin], axis=-1).astype(x.dtype)


def chunk_causal_attention(q, k, v):
    b, s, h, dk = q.shape
    n_blk = s // Q_BLOCK
    scale = dk ** -0.5
    key_chunk = jnp.arange(s) // CHUNK
    q_blocks = q.reshape(b, n_blk, Q_BLOCK, h, dk).transpose(1, 0, 2, 3, 4)

    def one_block(args):
        qb, blk = args
        sc = jnp.einsum('bqhd,bkhd->bhqk', qb, k, preferred_element_type=F32) * scale
        q_chunk = (blk * Q_BLOCK + jnp.arange(Q_BLOCK)) // CHUNK
        sc = jnp.where(key_chunk[None, :] <= q_chunk[:, None], sc, -jnp.inf)
        pr = jax.nn.softmax(sc, axis=-1).astype(v.dtype)
        return jnp.einsum('bhqk,bkhd->bqhd', pr, v)

    out = lax.map(one_block, (q_blocks, jnp.arange(n_blk)))
    return out.transpose(1, 0, 2, 3, 4).reshape(b, s, h, v.shape[-1])


def mla_branch(z, positions, q_norm_g, kv_norm_g, w_uq, w_ukv):
    b, s, _ = z.shape
    z_q, z_kv, z_kr = jnp.split(z, [MLA_Q_RANK, MLA_Q_RANK + MLA_KV_RANK], axis=-1)
    q = (rms_norm(z_q, q_norm_g) @ w_uq).reshape(b, s, MLA_HEADS, MLA_NOPE + MLA_ROPE)
    q = jnp.concatenate([q[..., :MLA_NOPE], rope(q[..., MLA_NOPE:], positions)], axis=-1)
    kv = (rms_norm(z_kv, kv_norm_g) @ w_ukv).reshape(b, s, MLA_HEADS, MLA_NOPE + MLA_V)
    k_rope = jnp.broadcast_to(rope(z_kr, positions)[:, :, None, :], (b, s, MLA_HEADS, MLA_ROPE))
    k = jnp.concatenate([kv[..., :MLA_NOPE], k_rope], axis=-1)
    o = chunk_causal_attention(q, k, kv[..., MLA_NOPE:])
    return o.reshape(b, s, MLA_HEADS * MLA_V)


def token_shift(z, mu):
    prev = jnp.pad(z, ((0, 0), (1, 0), (0, 0)))[:, :-1]
    return z + (prev - z) * mu


def wkv7_scan(r, w, k, v, kk, a):
    b, s, h, n = r.shape

    def step(state, inp):
        r_t, w_t, k_t, v_t, kk_t, a_t = inp
        sa = jnp.einsum('bhvk,bhk->bhv', state, -kk_t)
        state = (state * w_t[:, :, None, :] + sa[..., None] * (kk_t * a_t)[:, :, None, :]
                 + v_t[..., None] * k_t[:, :, None, :])
        return state, jnp.einsum('bhvk,bhk->bhv', state, r_t)

    xs = tuple(t.astype(F32).transpose(1, 0, 2, 3) for t in (r, w, k, v, kk, a))
    _, ys = lax.scan(step, jnp.zeros((b, h, n, n), F32), xs)
    return ys.transpose(1, 0, 2, 3)


def rwkv7_branch(z, mu, w0, w_up, a0, a_up, g_up, k_k, k_a, r_k, ln_w, ln_b):
    b, s, _ = z.shape
    r, k, v, xw, xa, xg = jnp.split(token_shift(z, mu), RW_SPLITS, axis=-1)
    w_log = -jax.nn.softplus(-(w0 + jnp.tanh(xw) @ w_up).astype(F32)) - 0.5
    decay = jnp.exp(-jnp.exp(w_log))
    a = jax.nn.sigmoid(a0 + xa @ a_up)
    g = jax.nn.sigmoid(xg) @ g_up
    heads = lambda t: t.reshape(b, s, RW_HEADS, RW_HEAD)
    kk = heads(k * k_k).astype(F32)
    kk = kk * lax.rsqrt(jnp.maximum(jnp.sum(kk * kk, axis=-1, keepdims=True), 1e-24))
    k = k * (1.0 + (a - 1.0) * k_a)
    y = wkv7_scan(heads(r), heads(decay), heads(k), heads(v), kk, heads(a))
    mean = jnp.mean(y, axis=-1, keepdims=True)
    var = jnp.mean(jnp.square(y - mean), axis=-1, keepdims=True)
    yn = ((y - mean) * lax.rsqrt(var + RW_LN_EPS)).reshape(b, s, RW_DIM)
    yn = yn * ln_w.astype(F32) + ln_b.astype(F32)
    bonus = jnp.sum(heads(r * k).astype(F32) * r_k.astype(F32), axis=-1, keepdims=True) * heads(v).astype(F32)
    out = (yn + bonus.reshape(b, s, RW_DIM)) * g.astype(F32)
    return out.astype(z.dtype)


def chunk_band_attention(z, rel_bias):
    b, s, _ = z.shape
    q, k, v = [t.reshape(b, s, CA_HEADS, CA_HEAD) for t in jnp.split(z, [CA_DIM, 2 * CA_DIM], axis=-1)]
    n_chunks = s // CHUNK
    pad = CA_LEFT_CHUNKS * CHUNK
    kp = jnp.pad(k, ((0, 0), (pad, 0), (0, 0), (0, 0)))
    vp = jnp.pad(v, ((0, 0), (pad, 0), (0, 0), (0, 0)))
    band = jnp.arange(CA_BAND)
    dist = pad + jnp.arange(CHUNK)[:, None] - band[None, :]
    rel_idx = jnp.clip(dist, REL_MIN, REL_MAX) - REL_MIN
    bias = rel_bias.astype(F32)[rel_idx].transpose(2, 0, 1)
    q_chunks = q.reshape(b, n_chunks, CHUNK, CA_HEADS, CA_HEAD).transpose(1, 0, 2, 3, 4)
    scale = CA_HEAD ** -0.5

    def one_chunk(args):
        qc, c = args
        kb = lax.dynamic_slice_in_dim(kp, c * CHUNK, CA_BAND, axis=1)
        vb = lax.dynamic_slice_in_dim(vp, c * CHUNK, CA_BAND, axis=1)
        sc = jnp.einsum('bqhd,bkhd->bhqk', qc, kb, preferred_element_type=F32) * scale + bias
        valid = band >= (CA_LEFT_CHUNKS - c) * CHUNK
        sc = jnp.where(valid, sc, -jnp.inf)
        pr = jax.nn.softmax(sc, axis=-1).astype(vb.dtype)
        return jnp.einsum('bhqk,bkhd->bqhd', pr, vb)

    out = lax.map(one_chunk, (q_chunks, jnp.arange(n_chunks)))
    return out.transpose(1, 0, 2, 3, 4).reshape(b, s, CA_DIM)


def setup_inputs(seed: int = 0) -> dict:
    key = jax.random.key(seed)
    ks = iter(jax.random.split(key, 40))

    def nrm(shape, scale):
        return scale * jax.random.normal(next(ks), shape, F32)

    def gain(shape):
        return 1.0 + nrm(shape, 0.05)

    L = DEPTH
    x = nrm((BATCH, SEQ, D_MODEL), 1.0)
    p = nrm((L, BATCH, SEQ, D_PLE), 1.0)
    offsets = jax.random.randint(next(ks), (BATCH,), 0, 1024, dtype=jnp.int32) * CHUNK
    positions = offsets[:, None] + jnp.arange(SEQ, dtype=jnp.int32)[None, :]
    return {
        'x': x,
        'p': p,
        'positions': positions,
        'pre_mix_g': gain((L, D_MODEL)),
        'w_in': nrm((L, D_MODEL, IN_COLS), D_MODEL ** -0.5),
        'mla_q_norm_g': gain((L, MLA_Q_RANK)),
        'mla_kv_norm_g': gain((L, MLA_KV_RANK)),
        'mla_w_uq': nrm((L, MLA_Q_RANK, MLA_HEADS * (MLA_NOPE + MLA_ROPE)), MLA_Q_RANK ** -0.5),
        'mla_w_ukv': nrm((L, MLA_KV_RANK, MLA_HEADS * (MLA_NOPE + MLA_V)), MLA_KV_RANK ** -0.5),
        'rw_mu': jax.random.uniform(next(ks), (L, RW_COLS), F32),
        'rw_w0': jax.random.uniform(next(ks), (L, RW_DIM), F32, -4.0, 0.0),
        'rw_w_up': nrm((L, RW_DECAY_LORA, RW_DIM), 0.1),
        'rw_a0': nrm((L, RW_DIM), 0.5),
        'rw_a_up': nrm((L, RW_AAA_LORA, RW_DIM), 0.1),
        'rw_g_up': nrm((L, RW_GATE_LORA, RW_DIM), RW_GATE_LORA ** -0.5),
        'rw_k_k': 0.85 + nrm((L, RW_DIM), 0.05),
        'rw_k_a': 1.0 + nrm((L, RW_DIM), 0.05),
        'rw_r_k': nrm((L, RW_HEADS, RW_HEAD), 0.1),
        'rw_ln_w': gain((L, RW_DIM)),
        'rw_ln_b': nrm((L, RW_DIM), 0.02),
        'ca_rel_bias': nrm((L, REL_SIZE, CA_HEADS), 0.5),
        'w_branch': nrm((L, MIX_DIM, D_MODEL), BRANCH_DIM ** -0.5),
        'w_out': nrm((L, D_MODEL, D_MODEL), D_MODEL ** -0.5),
        'post_mix_g': gain((L, D_MODEL)),
        'pre_ff_g': gain((L, D_MODEL)),
        'w_ff1': nrm((L, D_MODEL, D_FF), D_MODEL ** -0.5),
        'w_ff2': nrm((L, D_FF, D_MODEL), D_FF ** -0.5),
        'post_ff_g': gain((L, D_MODEL)),
        'w_ple_gate': nrm((L, D_MODEL, D_MODEL), D_MODEL ** -0.5),
        'w_ple_proj': nrm((L, D_PLE, D_MODEL), D_PLE ** -0.5),
    }


def reference(x, p, positions, pre_mix_g, w_in, mla_q_norm_g, mla_kv_norm_g, mla_w_uq, mla_w_ukv,
              rw_mu, rw_w0, rw_w_up, rw_a0, rw_a_up, rw_g_up, rw_k_k, rw_k_a, rw_r_k, rw_ln_w, rw_ln_b,
              ca_rel_bias, w_branch, w_out, post_mix_g, pre_ff_g, w_ff1, w_ff2, post_ff_g,
              w_ple_gate, w_ple_proj):
    b, s, _ = x.shape
    h = x
    for i in range(DEPTH):
        u = rms_norm(h, pre_mix_g[i])
        z = u @ w_in[i]
        z_mla, z_rw, z_ca, z_gate = jnp.split(z, IN_SPLITS, axis=-1)
        o_mla = mla_branch(z_mla, positions, mla_q_norm_g[i], mla_kv_norm_g[i], mla_w_uq[i], mla_w_ukv[i])
        o_rw = rwkv7_branch(z_rw, rw_mu[i], rw_w0[i], rw_w_up[i], rw_a0[i], rw_a_up[i], rw_g_up[i],
                            rw_k_k[i], rw_k_a[i], rw_r_k[i], rw_ln_w[i], rw_ln_b[i])
        o_ca = chunk_band_attention(z_ca, ca_rel_bias[i])
        branches = jnp.stack([o_mla, o_rw, o_ca], axis=2)
        y = jnp.einsum('bsnc,ncd->bsnd', branches, w_branch[i].reshape(N_BRANCH, BRANCH_DIM, D_MODEL))
        gates = jax.nn.sigmoid(z_gate).reshape(b, s, N_BRANCH, D_MODEL)
        merged = jnp.sum(gates * y, axis=2)
        h = h + rms_norm(merged @ w_out[i], post_mix_g[i])
        f = rms_norm(h, pre_ff_g[i])
        f = jnp.square(jax.nn.relu(f @ w_ff1[i])) @ w_ff2[i]
        h = h + rms_norm(f, post_ff_g[i])
        h = h + jax.nn.sigmoid(h @ w_ple_gate[i]) * (p[i] @ w_ple_proj[i])
    return h
```

```python
import contextlib
import numpy as np
import ml_dtypes
import concourse.bass as bass
import concourse.mybir as mybir
from concourse.bass_utils import run_bass_kernel_spmd

F32 = mybir.dt.float32
BF16 = mybir.dt.bfloat16
I32 = mybir.dt.int32
AF = mybir.ActivationFunctionType
ALU = mybir.AluOpType

D = 1024
DFF = 4096
DPLE = 256
INC = 6848
NL = 2
SEQ = 4096
NB = 8
T = 256
EPS = 1e-6
LN_EPS = 64e-5
CC = float(np.exp(-0.5))
MLA_SCALE = float(192 ** -0.5)
TWO_PI = 6.2831845
NEG = -30000.0

VC = {}
_o = 0
for _n, _k in [("pre_mix_g", 8), ("q_g", 2), ("kv_g", 1), ("mu", 14), ("w0", 4), ("a0", 4), ("k_k", 4),
               ("k_a", 4), ("r_k", 4), ("ln_w", 4), ("ln_b", 4), ("post_mix_g", 8), ("pre_ff_g", 8),
               ("post_ff_g", 8)]:
    VC[_n] = _o
    _o += _k
NV = _o
CI_ID, CI_J, CI_ONE, CI_BLK, CI_SU, CI_U, CI_SL = [i * 128 for i in range(7)]
CI_INVF = 7 * 128
CI_SGN = CI_INVF + 1
NCONST = CI_SGN + 1

WSPEC = {
    "w_in": (1024, INC), "w_in_sw": (1024, 64), "w_uq": (256, 768), "w_uq_sw": (256, 256),
    "w_ukT": (512, 128), "w_ukv": (128, 1024), "rw_w_up": (64, 512), "rw_a_up": (64, 512),
    "rw_g_up": (128, 512), "w_branch": (1536, 1024), "w_out": (1024, 1024), "w_ff1": (1024, 4096),
    "w_ff2": (4096, 1024), "w_ple_gate": (1024, 1024), "w_ple_proj": (256, 1024),
}
WORDER = ["w_in", "w_in_sw", "w_uq", "w_uq_sw", "w_ukT", "w_ukv", "rw_w_up", "rw_a_up", "rw_g_up",
          "w_branch", "w_out", "w_ff1", "w_ff2", "w_ple_gate", "w_ple_proj"]


class Dep:
    __slots__ = ("w", "r", "al")

    def __init__(self):
        self.w = None
        self.r = {}
        self.al = []


class Buf:
    def __init__(self, ap, d=None):
        self.ap = ap
        self.d = d if d is not None else Dep()

    def __getitem__(self, k):
        return self.ap[k]


def v3(ap, a):
    return ap.rearrange("p (a b) -> p a b", a=a)


ENGS = ("pe", "act", "dve", "pool", "sp")
NDS = 8


class Builder:
    def __init__(self, nc, es):
        self.nc = nc
        self.ops = {e: [] for e in ENGS}
        self.cnt = {e: 0 for e in ENGS}
        self.waited = {e: {} for e in ENGS}
        self.sems = []
        self.semid = {}
        for e in ENGS:
            self.semid[e] = len(self.sems)
            self.sems.append(es.enter_context(nc.semaphore("s_" + e)))
        self.dsem = {}
        self.dcnt = {}
        for q in ("sp", "pool", "act"):
            self.dsem[q] = []
            self.dcnt[q] = 0
            for i in range(NDS):
                self.dsem[q].append(len(self.sems))
                self.sems.append(es.enter_context(nc.semaphore("d_%s%d" % (q, i))))

    def _waits(self, eng, reads, writes):
        need = {}

        def add(sid, val):
            if need.get(sid, 0) < val:
                need[sid] = val

        for d in reads:
            if d.w is not None:
                add(*d.w)
        for d in writes:
            if d.w is not None:
                add(*d.w)
            for sid, val in d.r.items():
                add(sid, val)
            for a in d.al:
                if a.w is not None:
                    add(*a.w)
                for sid, val in a.r.items():
                    add(sid, val)
        out = []
        wd = self.waited[eng]
        pe_sid = self.semid["pe"]
        for sid, val in need.items():
            if eng == "pe" and sid == pe_sid:
                continue
            if wd.get(sid, 0) >= val:
                continue
            wd[sid] = val
            out.append((sid, val))
        return out

    def _mark(self, reads, writes, sid, val):
        for d in reads:
            if d.r.get(sid, 0) < val:
                d.r[sid] = val
        for d in writes:
            d.w = (sid, val)
            d.r = {}

    def op(self, eng, fn, R=(), W=()):
        R = [x.d if isinstance(x, Buf) else x for x in R]
        W = [x.d if isinstance(x, Buf) else x for x in W]
        ws = self._waits(eng, R, W)
        self.cnt[eng] += 1
        sid = self.semid[eng]
        self.ops[eng].append((ws, fn, sid, 1))
        self._mark(R, W, sid, self.cnt[eng])

    def dma(self, q, out, in_, R=(), W=(), **kw):
        R = [x.d if isinstance(x, Buf) else x for x in R]
        W = [x.d if isinstance(x, Buf) else x for x in W]
        ws = self._waits(q, R, W)
        i = self.dcnt[q]
        self.dcnt[q] += 1
        sid = self.dsem[q][i % NDS]
        val = 16 * (i // NDS + 1)
        if val > 16 and self.waited[q].get(sid, 0) < val - 16:
            self.waited[q][sid] = val - 16
            ws.append((sid, val - 16))
        self.ops[q].append((ws, lambda e, o=out, i_=in_, k=kw: e.dma_start(out=o, in_=i_, **k), sid, 16))
        self._mark(R, W, sid, val)

    def mm(self, out, lhsT, rhs, R, W, start=True, stop=True):
        self.pes = getattr(self, "pes", 0) + (2 if lhsT.dtype == F32 else 1)
        self.op("pe", lambda e: e.matmul(out, lhsT, rhs, start=start, stop=stop, skip_group_check=True), R, W)

    def tr(self, out, in_, ident, R, W):
        self.pes = getattr(self, "pes", 0) + 1
        self.op("pe", lambda e: e.transpose(out, in_, ident), R, W)

    def A(self, out, in_, func, R, W, scale=None, bias=None):
        kw = {}
        if scale is not None:
            kw["scale"] = scale
        if bias is not None:
            kw["bias"] = bias
        self.op("act", lambda e: e.activation(out, in_, func, **kw), R, W)

    def tt(self, eng, out, a, b, op, R, W):
        self.op(eng, lambda e: e.tensor_tensor(out, a, b, op), R, W)

    def ts(self, eng, out, a, s1, op0, R, W, s2=None, op1=None):
        if op1 is None:
            self.op(eng, lambda e: e.tensor_scalar(out, a, s1, None, op0), R, W)
        else:
            self.op(eng, lambda e: e.tensor_scalar(out, a, s1, s2, op0, op1), R, W)

    def stt(self, eng, out, a, s, b, op0, op1, R, W):
        self.op(eng, lambda e: e.scalar_tensor_tensor(out, a, s, b, op0, op1), R, W)

    def cp(self, eng, out, in_, R, W):
        if eng == "act":
            self.op("act", lambda e: e.activation(out, in_, AF.Copy), R, W)
        else:
            self.op(eng, lambda e: e.tensor_copy(out, in_), R, W)

    def rcp(self, out, in_, R, W):
        self.op("dve", lambda e: e.reciprocal(out, in_), R, W)

    def ms(self, eng, ap, val, W):
        self.op(eng, lambda e: e.memset(ap, val), (), W)

    def emit(self, block):
        sems = self.sems
        B = self

        def run(e, name):
            for ws, fn, sid, inc in B.ops[name]:
                for s_, v_ in ws:
                    e.wait_ge(sems[s_], v_)
                fn(e).then_inc(sems[sid], inc)

        fin = []
        for en in ENGS:
            if self.cnt[en] > 0:
                fin.append((self.semid[en], self.cnt[en]))
        for q in ("sp", "pool", "act"):
            n = self.dcnt[q]
            for k in range(NDS):
                cntk = (n - k + NDS - 1) // NDS if n > k else 0
                if cntk > 0:
                    fin.append((self.dsem[q][k], 16 * cntk))

        @block.tensor
        def _(e):
            run(e, "pe")

        @block.scalar
        def _(e):
            run(e, "act")

        @block.vector
        def _(e):
            run(e, "dve")

        @block.gpsimd
        def _(e):
            run(e, "pool")

        @block.sync
        def _(e):
            run(e, "sp")
            for s_, v_ in fin:
                e.wait_ge(sems[s_], v_)


def build(S=SEQ, nl=NL, dbg=False, stage=99):
    NT = S // T
    NBLK = S // 128
    nc = bass.Bass("TRN2", target_bir_lowering=False)
    es = contextlib.ExitStack()
    es.__enter__()
    B = Builder(nc, es)

    def dram(name, shape, dt, kind):
        return nc.dram_tensor(name, list(shape), dt, kind=kind).ap()

    xT = dram("xT", [D, S], F32, "ExternalInput")
    pT = dram("pT", [nl, DPLE, S], F32, "ExternalInput")
    pos = dram("pos", [1, S], I32, "ExternalInput")
    vecs = dram("vecs", [nl, 128, NV], F32, "ExternalInput")
    consts = dram("consts", [128, NCONST], F32, "ExternalInput")
    relr = dram("relr", [nl, 8, 320], F32, "ExternalInput")
    wf = {}
    wb = {}
    wdep = {}
    for n in WORDER:
        r, c = WSPEC[n]
        wf[n] = dram(n, [nl, r, c], F32, "ExternalInput")
        wb[n] = dram(n + "_b", [nl, r, c], BF16, "Internal")
        for l in range(nl):
            wdep[(n, l)] = Dep()
    outT = dram("outT", [D, S], F32, "ExternalOutput")
    hscr = [dram("hscr%d" % i, [D, S], F32, "Internal") for i in range(max(nl - 1, 1))]
    hscr_d = [[Dep() for _ in range(NT)] for _ in range(max(nl - 1, 1))]
    ext = dram("ext", [nl, 8, 768], F32, "Internal")
    ext_d = [Dep() for _ in range(nl)]
    dbg_t = {}
    if dbg:
        for n in ("o_mla", "o_rw", "o_ca"):
            dbg_t[n] = dram("dbg_" + n, [512, S], BF16, "ExternalOutput")

    def sb(name, shape, dt):
        return Buf(es.enter_context(nc.sbuf_tensor(name, list(shape), dt))[:])

    cst = sb("cst", [128, NCONST], F32)
    vec = [sb("vec%d" % l, [128, NV], F32) for l in range(nl)]
    omka = [sb("omka%d" % l, [128, 4], F32) for l in range(nl)]
    cbf = sb("cbf", [128, 4 * 128], BF16)
    ident_bf = cbf[:, 0:128]
    J_bf = cbf[:, 128:256]
    ones_bf = cbf[:, 256:384]
    blk_bf = cbf[:, 384:512]
    ident_f = cst[:, CI_ID:CI_ID + 128]
    m_su = cst[:, CI_SU:CI_SU + 128]
    m_u = cst[:, CI_U:CI_U + 128]
    m_sl = cst[:, CI_SL:CI_SL + 128]

    hT = sb("hT", [128, 8 * T], F32)
    hT3 = v3(hT.ap, 8)
    uT = sb("uT", [128, 8 * T], BF16)
    uT3 = v3(uT.ap, 8)
    NW = 3
    wsl = [sb("wsl%d" % i, [128, 4096], BF16) for i in range(NW)]
    wsl_i = [0]
    Kc = sb("Kc", [128, S], BF16)
    Kr = sb("Kr", [64, S], BF16)
    Vc = sb("Vc", [128, S], BF16)
    Vc3 = v3(Vc.ap, NBLK)
    Kc_d = [Dep() for _ in range(NT)]
    CK = sb("CK", [128, 4 * 1024], BF16)
    CK3 = v3(CK.ap, 4)
    CV = sb("CV", [128, 8 * 512], BF16)
    CV3 = v3(CV.ap, 8)
    CK_d = [Dep() for _ in range(4)]
    CV_d = [Dep() for _ in range(8)]
    Xb = sb("Xb", [128, 40 * 128], BF16)
    Xb3 = v3(Xb.ap, 40)
    o_mla = sb("o_mla", [128, 4 * T], BF16)
    o_rw = sb("o_rw", [128, 4 * T], BF16)
    o_ca = sb("o_ca", [128, 4 * T], BF16)
    Sst = sb("Sst", [128, 256], F32)
    zlast = sb("zlast", [128, 16], F32)
    small = sb("small", [128, 64], F32)

    AW = 24200
    arena = es.enter_context(nc.sbuf_tensor("arena", [128, AW], F32))[:]
    abufs = []
    aoff = {}

    def al(phase, name, n, dt):
        words = (n + 1) // 2 if dt == BF16 else n
        o = aoff.get(phase, 0)
        aoff[phase] = o + words
        assert o + words <= AW, (phase, name, o + words)
        ap = arena[:, o:o + words]
        if dt == BF16:
            ap = ap.bitcast(BF16)[:, 0:n]
        b = Buf(ap)
        for (ph2, o2, e2, b2) in abufs:
            if ph2 != phase and o2 < o + words and o < e2:
                b.d.al.append(b2.d)
                b2.d.al.append(b.d)
        abufs.append((phase, o, o + words, b))
        return b

    def al_all(name, n, dt):
        words = (n + 1) // 2 if dt == BF16 else n
        o = max([aoff.get(p, 0) for p in ("mla", "ca", "rw", "ffn")])
        for p in ("mla", "ca", "rw", "ffn"):
            assert aoff.get(p, 0) <= o
            aoff[p] = o + words
        ap = arena[:, o:o + words]
        if dt == BF16:
            ap = ap.bitcast(BF16)[:, 0:n]
        return Buf(ap)

    sqb = al_all("sqb", 8 * T, BF16)
    sqb3 = v3(sqb.ap, 8)
    rt = al_all("rt", T, F32)
    rt2 = al_all("rt2", T, F32)
    mo = al_all("mo", 8 * T, F32)
    mo3 = v3(mo.ap, 8)

    PS = [Buf(es.enter_context(nc.psum_tensor("ps%d" % i, [128, 512], F32))[:]) for i in range(8)]
    rot = {"d": 0}

    def psd():
        i = rot["d"]
        rot["d"] = (i + 1) % 4
        return PS[i]

    block = es.enter_context(nc.Block())

    B.dma("sp", cst.ap, consts, W=[cst])
    for l in range(nl):
        B.dma("sp", vec[l].ap, vecs[l], W=[vec[l]])
    for l in range(nl):
        for n in WORDER:
            r, c = WSPEC[n]
            npc = (c + 2047) // 2048
            pc = c // npc
            assert pc * npc == c
            for i in range(npc):
                B.dma("pool", wb[n][l, :, i * pc:(i + 1) * pc], wf[n][l, :, i * pc:(i + 1) * pc], W=[wdep[(n, l)]])
    B.cp("dve", cbf[:, 0:512], cst[:, 0:512], [cst], [cbf])
    for l in range(nl):
        ka = vec[l][:, VC["k_a"]:VC["k_a"] + 4]
        B.ts("dve", omka[l].ap, ka, -1.0, ALU.mult, [vec[l]], [omka[l]], s2=1.0, op1=ALU.add)

    def vcol(l, name, i):
        c = VC[name] + i
        return vec[l][:, c:c + 1]

    def wload(name, l, dram_ap, shape, prt=None):
        s = wsl[wsl_i[0]]
        wsl_i[0] = (wsl_i[0] + 1) % NW
        n = int(np.prod(shape[1:]))
        p0, p1 = prt if prt is not None else (0, shape[0])
        view = s.ap[p0:p1, 0:n]
        if len(shape) == 3:
            view = view.rearrange("p (a b) -> p a b", a=shape[1])
        elif len(shape) == 4:
            view = view.rearrange("p (a b c) -> p a b c", a=shape[1], b=shape[2])
        B.dma("sp", view, dram_ap, R=[wdep[(name, l)]], W=[s])
        return view, s

    def wcols(name, l, c0, n, kc):
        ap = wb[name][l, 0:kc * 128, c0:c0 + n].rearrange("(k p) n -> p k n", p=128)
        return wload(name, l, ap, [128, kc, n])

    def rms(src3, nk, ktot, l, gname, out_fn, R, Wd, eps=EPS, ps=None):
        B.A(sqb3[:, 0:nk, :], src3, AF.Square, R, [sqb])
        p = psd()
        for k in range(nk):
            B.mm(p[:, 0:T], ones_bf, sqb3[:, k, :], [sqb, cbf], [p], start=(k == 0), stop=(k == nk - 1))
        B.A(rt.ap, p[:, 0:T], AF.Sqrt, [p], [rt], scale=1.0 / ktot, bias=eps_ap(eps))
        B.rcp(rt2.ap, rt.ap, [rt], [rt2])

    eps_t = sb("eps_t", [128, 4], F32)
    B.ms("dve", eps_t[:, 0:1], EPS, [eps_t])
    B.ms("dve", eps_t[:, 1:2], LN_EPS, [eps_t])
    B.ms("dve", eps_t[:, 2:3], 0.0, [eps_t])
    B.ms("dve", eps_t[:, 3:4], 0.25, [eps_t])

    def eps_ap(e):
        return eps_t[:, 0:1] if e == EPS else eps_t[:, 1:2]

    zq = al("mla", "zq", 2 * T, F32)
    zq3 = v3(zq.ap, 2)
    zqn = al("mla", "zqn", 2 * T, BF16)
    zqn3 = v3(zqn.ap, 2)
    qn = [al("mla", "qn%d" % i, T, BF16) for i in range(2)]
    Qabs = al("mla", "Qabs", 4 * T, BF16)
    Qabs3 = v3(Qabs.ap, 4)
    Qrope = al("mla", "Qrope", 4 * T, BF16)
    Qrope3 = v3(Qrope.ap, 4)
    zkv = al("mla", "zkv", T, F32)
    cosT = al("mla", "cosT", T, F32)
    sinT = al("mla", "sinT", T, F32)
    posi = al("mla", "posi", T, F32)
    posf = al("mla", "posf", T, F32)
    tq = al("mla", "tq", T, F32)
    tq2 = al("mla", "tq2", T, F32)
    rp1 = al("mla", "rp1", T, F32)
    rp2 = al("mla", "rp2", T, F32)
    PTm = [al("mla", "PTm%d" % i, 2 * T, BF16) for i in range(2)]
    rinv = al("mla", "rinv", 2 * T, F32)
    On = al("mla", "On", 2 * T, BF16)
    Qs = al("ca", "Qs", 4 * T, BF16)
    Qs3 = v3(Qs.ap, 4)
    PTc = [al("ca", "PTc%d" % i, 512, BF16) for i in range(2)]
    rinvc = al("ca", "rinvc", 512, F32)
    aT = al("ffn", "aT", 32 * T, BF16)
    aT3 = v3(aT.ap, 32)
    rl = [al("ffn", "rl%d" % i, T, F32) for i in range(2)]
    mrg = al("ffn", "mrg", 8 * T, F32)
    mrg3 = v3(mrg.ap, 8)
    mrgb = al("ffn", "mrgb", 8 * T, BF16)
    mrgb3 = v3(mrgb.ap, 8)
    gsb = [al("ffn", "gsb%d" % i, T, F32) for i in range(2)]
    tmpf = [al("ffn", "tmpf%d" % i, T, F32) for i in range(2)]
    pb = al("ffn", "pb", 2 * T, BF16)
    pb3 = v3(pb.ap, 2)
    zb = [al("rw", "zb%d" % i, T + 2, F32) for i in range(2)]
    dd = al("rw", "dd", T, F32)
    zr = al("rw", "zr", T, F32)
    zk = al("rw", "zk", T, F32)
    zs12 = zr
    zs13 = zk
    zv = al("rw", "zv", T, F32)
    txw = al("rw", "txw", T, BF16)
    xab = al("rw", "xab", T, BF16)
    sgb = al("rw", "sgb", T, BF16)
    tmp = {n: al("rw", n, T, F32) for n in ("sgw", "cs", "csc", "dC", "E1", "aa", "kk", "ssm", "rs", "tb", "tc", "km")}
    tmp["csx"] = tmp["cs"]
    tmp["E3"] = tmp["cs"]
    tmp["E2"] = tmp["csc"]
    tmp["E4"] = tmp["dC"]
    tmp["kkn"] = tmp["kk"]
    kk2 = al("rw", "kk2", T, BF16)
    rkr = al("rw", "rkr", T, BF16)
    vbf = al("rw", "vbf", T, BF16)
    bh = al("rw", "bh", T, BF16)
    kh = al("rw", "kh", T, BF16)
    aTt = al("rw", "aTt", 4 * T, BF16)
    bTt = al("rw", "bTt", 4 * T, BF16)
    kTt = al("rw", "kTt", 4 * T, BF16)
    rTt = al("rw", "rTt", 4 * T, BF16)
    aT_3, bT_3, kT_3, rT_3 = (v3(x.ap, 4) for x in (aTt, bTt, kTt, rTt))
    TM = al("rw", "TM", 3 * 2 * 4 * 128, BF16)
    TM5 = TM.ap.rearrange("p (j c h n) -> p j c h n", j=3, c=2, h=4)
    AM = al("rw", "AM", 2 * 4 * 4 * 2 * 128, BF16)
    AM6 = AM.ap.rearrange("p (c h t e n) -> p c h t e n", c=2, h=4, t=4, e=2)
    AM_d = [[Dep() for _ in range(4)] for _ in range(2)]
    for cc_ in range(2):
        for hh_ in range(4):
            AM_d[cc_][hh_].al = AM.d.al
    Qd = [al("rw", "Qd0", 4 * 128, BF16)] * 2
    QTd = [al("rw", "QTd0", 4 * 128, BF16)] * 2
    Xd = [al("rw", "Xd0", 4 * 128, F32)] * 2
    Xbb = al("rw", "Xbb", 4 * 128, BF16)
    yb = al("rw", "y", 4 * T, F32)
    y3 = v3(yb.ap, 4)
    bonus = al("rw", "bonus", 4 * T, F32)
    bonus3 = v3(bonus.ap, 4)
    gbuf = al("rw", "g", 4 * T, BF16)
    g3 = v3(gbuf.ap, 4)
    Smid = al("rw", "Smid", 256, F32)
    Smb = al("rw", "Smb", 256, BF16)
    W0b = al("rw", "W0b", 512, BF16)
    Ut = al("rw", "Ut", 512, BF16)
    lwa = al("rw", "lwa", 512, BF16)
    lwg = al("rw", "lwg", 512, BF16)
    ybf = al("rw", "ybf", T, BF16)
    yc = al("rw", "yc", T, F32)
    ysq = al("rw", "ysq", T, BF16)
    sd = al("rw", "sd", T, F32)
    t1 = al("rw", "t1", T, F32)
    t2 = al("rw", "t2", T, F32)

    def layer(l):
        src = xT if l == 0 else hscr[l - 1]
        dst = outT if l == nl - 1 else hscr[l]
        src_d = None if l == 0 else hscr_d[l - 1]
        dst_d = None if l == nl - 1 else hscr_d[l]
        B.ms("dve", Sst.ap, 0.0, [Sst])
        B.ms("dve", zlast.ap, 0.0, [zlast])
        etap = mo.ap[0:8, 0:768]
        rl_ap = mo.ap[0:8, 768:768 + 320]
        B.dma("sp", rl_ap, relr[l], W=[mo])
        B.cp("dve", etap[:, 383:703], rl_ap, [mo], [mo])
        B.cp("dve", etap[:, 0:383], rl_ap[:, 0:1].to_broadcast([8, 383]), [mo], [mo])
        B.cp("dve", etap[:, 703:768], rl_ap[:, 319:320].to_broadcast([8, 65]), [mo], [mo])
        B.dma("sp", ext[l], etap, R=[mo], W=[ext_d[l]])
        for r in range(5):
            for h in range(8):
                off = 639 - 128 * r - 127
                srcap = bass.AP(tensor=ext.tensor, offset=(l * 8 + h) * 768 + off, ap=[[1, 128], [1, 128]])
                B.dma("pool", Xb3[:, r * 8 + h, :], srcap, R=[ext_d[l]], W=[Xb])
        for h in range(8):
            B.ms("pool", Xb3[64:128, 0 * 8 + h, 64:128], NEG, [Xb])
            B.ms("pool", Xb3[0:64, 4 * 8 + h, 0:64], NEG, [Xb])

        for tt in range(NT):
            tile(l, tt, src, dst, src_d, dst_d)

    def mark(name, l, tt):
        PHASES.append((name, l, tt, getattr(B, "pes", 0)))

    def tile(l, tt, src, dst, src_d, dst_d):
        t0 = tt * T
        mark("start", l, tt)
        R_src = [] if src_d is None else [src_d[tt]]
        B.dma("sp", hT3, src[:, t0:t0 + T].rearrange("(k p) t -> p k t", p=128), R=R_src, W=[hT])
        rms(hT3, 8, D, l, "pre_mix_g", None, [hT], None)
        for k in range(8):
            B.stt("dve", uT3[:, k, :], hT3[:, k, :], vcol(l, "pre_mix_g", k), rt2.ap, ALU.mult, ALU.mult,
                  [hT, vec[l], rt2], [uT])

        def fin():
            W_dst = [] if dst_d is None else [dst_d[tt]]
            B.dma("sp", dst[:, t0:t0 + T].rearrange("(k p) t -> p k t", p=128), hT3, R=[hT], W=W_dst)

        if stage <= 0:
            return fin()

        def zmm(p, wv, ws, c0, n, M=None):
            for k in range(8):
                B.mm(p[0:n, 0:T], wv[:, k, c0:c0 + n], uT3[:, k, :], [ws, uT], [p], start=(k == 0), stop=(k == 7))

        mark("mla", l, tt)
        wv, ws = wcols("w_in", l, 0, 448, 8)
        wsw, wsws = wcols("w_in_sw", l, 0, 64, 8)
        for c in range(2):
            p = psd()
            zmm(p, wv, ws, c * 128, 128)
            B.cp("act", zq3[:, c, :], p[:, 0:T], [p], [zq])
        p = psd()
        zmm(p, wv, ws, 256, 128)
        B.cp("act", zkv.ap, p[:, 0:T], [p], [zkv])
        B.dma("sp", posi.ap[0:64, :].bitcast(I32), pos[0:1, t0:t0 + T].to_broadcast([64, T]), W=[posi])
        B.cp("dve", posf[0:64, :], posi.ap[0:64, :].bitcast(I32), [posi], [posf])
        B.ts("dve", tq[0:64, :], posf[0:64, :], cst[0:64, CI_INVF:CI_INVF + 1], ALU.mult, [posf, cst], [tq],
             s2=float(1.0 / (2 * np.pi)), op1=ALU.mult)
        MAGIC = 12582912.0
        for (dstb, shift) in ((sinT, 0.0), (cosT, 0.25)):
            if shift != 0.0:
                B.ts("dve", tq2[0:64, :], tq[0:64, :], shift, ALU.add, [tq], [tq2])
                srcq = tq2
            else:
                srcq = tq
            B.ts("dve", rp1[0:64, :], srcq[0:64, :], MAGIC, ALU.add, [srcq], [rp1], s2=MAGIC, op1=ALU.subtract)
            B.tt("dve", rp2[0:64, :], srcq[0:64, :], rp1[0:64, :], ALU.subtract, [srcq, rp1], [rp2])
            if shift == 0.0:
                B.A(dstb[0:64, :], rp2[0:64, :], AF.Sin, [rp2, cst], [dstb], scale=cst[0:64, CI_SGN:CI_SGN + 1])
            else:
                B.A(dstb[0:64, :], rp2[0:64, :], AF.Sin, [rp2], [dstb], scale=TWO_PI)
        p1 = psd()
        zmm(p1, wv, ws, 384, 64)
        p2 = psd()
        zmm(p2, wsw, wsws, 0, 64)
        B.tt("dve", rp1[0:64, :], p1[0:64, 0:T], cosT[0:64, :], ALU.mult, [p1, cosT], [rp1])
        B.tt("dve", rp2[0:64, :], p2[0:64, 0:T], sinT[0:64, :], ALU.mult, [p2, sinT], [rp2])
        B.tt("dve", Kr[0:64, t0:t0 + T], rp1[0:64, :], rp2[0:64, :], ALU.add, [rp1, rp2], [Kc_d[tt]])
        rms(zkv.ap.rearrange("p (a b) -> p a b", a=1), 1, 128, l, "kv_g", None, [zkv], None)
        B.stt("dve", Kc[:, t0:t0 + T], zkv.ap, vcol(l, "kv_g", 0), rt2.ap, ALU.mult, ALU.mult, [zkv, vec[l], rt2],
              [Kc_d[tt]])
        pt = PS[7]
        ptb = pt.ap.bitcast(BF16)
        for i in range(2):
            B.tr(ptb[:, i * 128:(i + 1) * 128], Kc[:, t0 + i * 128:t0 + (i + 1) * 128], ident_bf, [Kc_d[tt], cbf], [pt])
        B.cp("act", Vc[:, (2 * tt) * 128:(2 * tt + 2) * 128], ptb[:, 0:256], [pt], [Kc_d[tt]])
        rms(zq3, 2, 256, l, "q_g", None, [zq], None)
        for c in range(2):
            B.stt("dve", zqn3[:, c, :], zq3[:, c, :], vcol(l, "q_g", c), rt2.ap, ALU.mult, ALU.mult,
                  [zq, vec[l], rt2], [zqn])
        wq = wb["w_uq"][l].rearrange("(k p) n -> p k n", p=128)
        wqv, wqs = wload("w_uq", l, wq, [128, 2, 768])
        wqsw = wb["w_uq_sw"][l].rearrange("(k p) n -> p k n", p=128)
        wqswv, wqsws = wload("w_uq_sw", l, wqsw, [128, 2, 256])
        wkt = wb["w_ukT"][l].rearrange("(h p) n -> p h n", p=128)
        wktv, wkts = wload("w_ukT", l, wkt, [128, 4, 128])
        for h in range(4):
            p = psd()
            for k in range(2):
                B.mm(p[:, 0:T], wqv[:, k, h * 192:h * 192 + 128], zqn3[:, k, :], [wqs, zqn], [p], start=(k == 0),
                     stop=(k == 1))
            q_ = qn[h % 2]
            B.cp("act", q_.ap, p[:, 0:T], [p], [q_])
            p = psd()
            B.mm(p[:, 0:T], wktv[:, h, :], q_.ap, [wkts, q_], [p])
            B.cp("act", Qabs3[:, h, :], p[:, 0:T], [p], [Qabs])
            p1 = psd()
            for k in range(2):
                B.mm(p1[0:64, 0:T], wqv[:, k, h * 192 + 128:h * 192 + 192], zqn3[:, k, :], [wqs, zqn], [p1],
                     start=(k == 0), stop=(k == 1))
            p2 = psd()
            for k in range(2):
                B.mm(p2[0:64, 0:T], wqswv[:, k, h * 64:(h + 1) * 64], zqn3[:, k, :], [wqsws, zqn], [p2],
                     start=(k == 0), stop=(k == 1))
            B.tt("dve", rp1[0:64, :], p1[0:64, 0:T], cosT[0:64, :], ALU.mult, [p1, cosT], [rp1])
            B.tt("dve", rp2[0:64, :], p2[0:64, 0:T], sinT[0:64, :], ALU.mult, [p2, sinT], [rp2])
            B.tt("dve", Qrope3[0:64, h, :], rp1[0:64, :], rp2[0:64, :], ALU.add, [rp1, rp2], [Qrope])
        mark("mla_attn", l, tt)
        wkv = wb["w_ukv"][l]
        wkvv, wkvs = wload("w_ukv", l, wkv, [128, 1024])
        nkb = 2 * (tt + 1)
        Kdeps = [Kc_d[j // 2] for j in range(nkb)]
        for hp in range(2):
            Ob, Sb = PS[4], PS[5]
            O3 = v3(Ob.ap, 2)
            S3 = v3(Sb.ap, 2)
            def m_scores(j):
                jd = j - 2 * tt
                q0 = max(jd, 0) * 128
                ps_ = PS[j % 2 + 2]
                ps3 = v3(ps_.ap, 2)
                for hh in range(2):
                    h = 2 * hp + hh
                    B.mm(ps3[:, hh, q0:T], Kc[:, j * 128:(j + 1) * 128], Qabs3[:, h, q0:T], [Kdeps[j], Qabs], [ps_],
                         start=True, stop=False)
                    B.mm(ps3[:, hh, q0:T], Kr[0:64, j * 128:(j + 1) * 128], Qrope3[0:64, h, q0:T],
                         [Kdeps[j], Qrope], [ps_], start=False, stop=True)

            def m_rest(j):
                jd = j - 2 * tt
                q0 = max(jd, 0) * 128
                ps_ = PS[j % 2 + 2]
                ps3 = v3(ps_.ap, 2)
                PT = PTm[j % 2]
                PT3 = v3(PT.ap, 2)
                B.A(PT3[:, :, q0:T], ps3[:, :, q0:T], AF.Exp, [ps_], [PT], scale=MLA_SCALE)
                if jd >= 0:
                    B.ms("pool", PT3[64:128, :, q0:q0 + 64], 0.0, [PT])
                for hh in range(2):
                    B.mm(O3[:, hh, q0:T], Vc3[:, j, :], PT3[:, hh, q0:T], [Kdeps[j], PT], [Ob],
                         start=(j == 0 and hh == 0), stop=(j == nkb - 1))
                for hh in range(2):
                    B.mm(S3[:, hh, q0:T], ones_bf, PT3[:, hh, q0:T], [cbf, PT], [Sb],
                         start=(j == 0 and hh == 0), stop=(j == nkb - 1))

            m_scores(0)
            for j in range(nkb):
                if j + 1 < nkb:
                    m_scores(j + 1)
                m_rest(j)
            B.rcp(rinv.ap, Sb.ap, [Sb], [rinv])
            B.tt("dve", On.ap, Ob.ap, rinv.ap, ALU.mult, [Ob, rinv], [On])
            On3 = v3(On.ap, 2)
            for hh in range(2):
                h = 2 * hp + hh
                p = psd()
                B.mm(p[:, 0:T], wkvv[:, h * 256 + 128:h * 256 + 256], On3[:, hh, :], [wkvs, On], [p])
                B.cp("act", o_mla[:, h * T:(h + 1) * T], p[:, 0:T], [p], [o_mla])

        if stage <= 1:
            return fin()
        mark("ca", l, tt)
        wv, ws = wcols("w_in", l, 2240, 512, 8)
        for c in range(4):
            p = psd()
            zmm(p, wv, ws, c * 128, 128)
            B.A(Qs3[:, c, :], p[:, 0:T], AF.Copy, [p], [Qs], scale=0.125)
        wv, ws = wcols("w_in", l, 2752, 512, 8)
        sl0 = (2 * tt) % 8
        for c in range(4):
            p = psd()
            zmm(p, wv, ws, c * 128, 128)
            B.cp("act", CK3[:, c, sl0 * 128:sl0 * 128 + T], p[:, 0:T], [p], [CK_d[sl0 // 2]])
        wv, ws = wcols("w_in", l, 3264, 512, 8)
        for i in range(2):
            p = psd()
            for k in range(8):
                B.mm(p.ap, uT3[:, k, i * 128:(i + 1) * 128], wv[:, k, :], [uT, ws], [p], start=(k == 0), stop=(k == 7))
            B.cp("act", CV3[:, sl0 + i, :], p.ap, [p], [CV_d[sl0 + i]])
        for i in range(2):
            qb = 2 * tt + i
            Ob, Sb = PS[4], PS[5]
            O3 = v3(Ob.ap, 4)
            S3 = v3(Sb.ap, 4)
            bl = list(range(max(0, qb - 4), qb + 1))
            units = [(b, e) for b in bl for e in range(2)]

            def c_scores(b, e):
                r = qb - b
                slot = b % 8
                ps_ = PS[2 + e]
                ps3 = v3(ps_.ap, 4)
                pb_ = e * 64
                for ch in range(4):
                    h = ch * 2 + e
                    B.mm(ps3[:, ch, :], CK3[pb_:pb_ + 64, ch, slot * 128:(slot + 1) * 128],
                         Qs3[pb_:pb_ + 64, ch, i * 128:(i + 1) * 128], [CK_d[slot // 2], Qs], [ps_],
                         start=True, stop=False)
                    B.mm(ps3[:, ch, :], Xb3[:, r * 8 + h, :], J_bf, [Xb, cbf], [ps_], start=False, stop=True)

            def c_rest(b, e):
                slot = b % 8
                ps_ = PS[2 + e]
                PT = PTc[e]
                PT3 = v3(PT.ap, 4)
                pb_ = e * 64
                B.A(PT.ap, ps_.ap, AF.Exp, [ps_], [PT])
                for ch in range(4):
                    h = ch * 2 + e
                    first = (b == bl[0] and ch == 0)
                    B.mm(O3[pb_:pb_ + 64, ch, :], CV3[:, slot, h * 64:(h + 1) * 64], PT3[:, ch, :],
                         [CV_d[slot], PT], [Ob], start=first, stop=True)
                for ch in range(4):
                    first = (b == bl[0] and ch == 0)
                    B.mm(S3[pb_:pb_ + 64, ch, :], ones_bf[:, 0:64], PT3[:, ch, :], [cbf, PT], [Sb],
                         start=first, stop=True)

            c_scores(*units[0])
            for ui, u_ in enumerate(units):
                if ui + 1 < len(units):
                    c_scores(*units[ui + 1])
                c_rest(*u_)
            B.rcp(rinvc.ap, Sb.ap, [Sb], [rinvc])
            B.tt("dve", v3(o_ca.ap, 4)[:, :, i * 128:(i + 1) * 128], O3, v3(rinvc.ap, 4), ALU.mult, [Ob, rinvc],
                 [o_ca])

        if stage <= 2:
            return fin()
        mark("rw", l, tt)
        rwkv(l, tt)
        if dbg and l == 0 and tt == 0:
            for n, bsrc in (("aT", aTt), ("bT", bTt), ("kT", kTt), ("rT", rTt), ("y", yb), ("bonus", bonus), ("g", gbuf),
                            ("TM", TM), ("AM", AM), ("Sst", Sst), ("small", small)):
                if n not in dbg_t:
                    dbg_t[n] = dram("dbg_" + n, [128, bsrc.ap.shape[1]], bsrc.ap.dtype, "ExternalOutput")
                B.dma("sp", dbg_t[n], bsrc.ap, R=[bsrc] + ([AM_d[c_][h_] for c_ in range(2) for h_ in range(4)] if n == "AM" else []))
        if stage <= 3 or (stage >= 21 and stage <= 26):
            return fin()

        if dbg and l == 0:
            for n, bsrc in (("o_mla", o_mla), ("o_rw", o_rw), ("o_ca", o_ca)):
                B.dma("sp", dbg_t[n][:, t0:t0 + T].rearrange("(k p) t -> p k t", p=128), v3(bsrc.ap, 4), R=[bsrc])

        mark("merge", l, tt)
        obr = (o_mla, o_rw, o_ca)
        for cg in range(2):
            for n in range(3):
                gv, gs = wcols("w_in", l, 3776 + n * 1024 + cg * 512, 512, 8)
                bap = wb["w_branch"][l, n * 512:(n + 1) * 512, cg * 512:(cg + 1) * 512].rearrange(
                    "(k p) n -> p k n", p=128)
                bv, bs = wload("w_branch", l, bap, [128, 4, 512])
                ob3 = v3(obr[n].ap, 4)
                for cl in range(4):
                    c = cg * 4 + cl
                    pg = psd()
                    zmm(pg, gv, gs, cl * 128, 128)
                    gb = gsb[(c * 3 + n) % 2]
                    B.A(gb.ap, pg[:, 0:T], AF.Sigmoid, [pg], [gb])
                    py = psd()
                    for k in range(4):
                        B.mm(py[:, 0:T], bv[:, k, cl * 128:(cl + 1) * 128], ob3[:, k, :], [bs, obr[n]], [py],
                             start=(k == 0), stop=(k == 3))
                    if n == 0:
                        B.tt("dve", mrg3[:, c, :], py[:, 0:T], gb.ap, ALU.mult, [py, gb], [mrg])
                    else:
                        tf = tmpf[(c * 3 + n) % 2]
                        B.tt("dve", tf.ap, py[:, 0:T], gb.ap, ALU.mult, [py, gb], [tf])
                        if n == 1:
                            B.tt("pool", mrg3[:, c, :], mrg3[:, c, :], tf.ap, ALU.add, [mrg, tf], [mrg])
                        else:
                            B.tt("pool", mrgb3[:, c, :], mrg3[:, c, :], tf.ap, ALU.add, [mrg, tf], [mrgb])
        for cg in range(2):
            wv, ws = wcols("w_out", l, cg * 512, 512, 8)
            for cl in range(4):
                c = cg * 4 + cl
                p = psd()
                for k in range(8):
                    B.mm(p[:, 0:T], wv[:, k, cl * 128:(cl + 1) * 128], mrgb3[:, k, :], [ws, mrgb], [p], start=(k == 0),
                         stop=(k == 7))
                B.cp("act", mo3[:, c, :], p[:, 0:T], [p], [mo])
        rms(mo3, 8, D, l, "post_mix_g", None, [mo], None)
        for k in range(8):
            tf = tmpf[k % 2]
            B.stt("dve", tf.ap, mo3[:, k, :], vcol(l, "post_mix_g", k), rt2.ap, ALU.mult, ALU.mult,
                  [mo, vec[l], rt2], [tf])
            B.tt("pool", hT3[:, k, :], hT3[:, k, :], tf.ap, ALU.add, [hT, tf], [hT])
        mark("ffn", l, tt)
        rms(hT3, 8, D, l, "pre_ff_g", None, [hT], None)
        for k in range(8):
            B.stt("dve", uT3[:, k, :], hT3[:, k, :], vcol(l, "pre_ff_g", k), rt2.ap, ALU.mult, ALU.mult,
                  [hT, vec[l], rt2], [uT])
        for cg in range(8):
            wv, ws = wcols("w_ff1", l, cg * 512, 512, 8)
            for cl in range(4):
                j = cg * 4 + cl
                p = psd()
                zmm(p, wv, ws, cl * 128, 128)
                r_ = rl[j % 2]
                B.A(r_.ap, p[:, 0:T], AF.Relu, [p], [r_])
                B.tt("pool", aT3[:, j, :], r_.ap, r_.ap, ALU.mult, [r_], [aT])
        for cg in range(2):
            accs = [PS[4 + i] for i in range(4)]
            for kg in range(4):
                wap = wb["w_ff2"][l, kg * 1024:(kg + 1) * 1024, cg * 512:(cg + 1) * 512].rearrange(
                    "(k p) n -> p k n", p=128)
                wv, ws = wload("w_ff2", l, wap, [128, 8, 512])
                for cl in range(4):
                    for kk in range(8):
                        B.mm(accs[cl][:, 0:T], wv[:, kk, cl * 128:(cl + 1) * 128], aT3[:, kg * 8 + kk, :], [ws, aT],
                             [accs[cl]], start=(kg == 0 and kk == 0), stop=(kg == 3 and kk == 7))
            for cl in range(4):
                B.cp("act", mo3[:, cg * 4 + cl, :], accs[cl][:, 0:T], [accs[cl]], [mo])
        rms(mo3, 8, D, l, "post_ff_g", None, [mo], None)
        for k in range(8):
            tf = tmpf[k % 2]
            B.stt("dve", tf.ap, mo3[:, k, :], vcol(l, "post_ff_g", k), rt2.ap, ALU.mult, ALU.mult,
                  [mo, vec[l], rt2], [tf])
            B.tt("pool", hT3[:, k, :], hT3[:, k, :], tf.ap, ALU.add, [hT, tf], [hT])
        mark("ple", l, tt)
        B.cp("act", uT.ap, hT.ap, [hT], [uT])
        B.dma("pool", pb3, pT[l, :, t0:t0 + T].rearrange("(k p) t -> p k t", p=128), W=[pb])
        ppv, pps = wload("w_ple_proj", l, wb["w_ple_proj"][l].rearrange("(k p) n -> p k n", p=128), [128, 2, 1024])
        for cg in range(2):
            wv, ws = wcols("w_ple_gate", l, cg * 512, 512, 8)
            for cl in range(4):
                c = cg * 4 + cl
                pg = psd()
                zmm(pg, wv, ws, cl * 128, 128)
                gb = gsb[c % 2]
                B.A(gb.ap, pg[:, 0:T], AF.Sigmoid, [pg], [gb])
                pp = psd()
                for k in range(2):
                    B.mm(pp[:, 0:T], ppv[:, k, c * 128:(c + 1) * 128], pb3[:, k, :], [pps, pb], [pp], start=(k == 0),
                         stop=(k == 1))
                tf = tmpf[c % 2]
                B.tt("dve", tf.ap, pp[:, 0:T], gb.ap, ALU.mult, [pp, gb], [tf])
                B.tt("pool", hT3[:, c, :], hT3[:, c, :], tf.ap, ALU.add, [hT, tf], [hT])
        W_dst = [] if dst_d is None else [dst_d[tt]]
        B.dma("sp", dst[:, t0:t0 + T].rearrange("(k p) t -> p k t", p=128), hT3, R=[hT], W=W_dst)

    def rwkv(l, tt):
        zi = [0]

        def zchunk(wv, ws, c0, cidx, dest):
            p = psd()
            for k in range(8):
                B.mm(p[:, 0:T], wv[:, k, c0:c0 + 128] if c0 is not None else wv[:, k, :], uT3[:, k, :], [ws, uT], [p],
                     start=(k == 0), stop=(k == 7))
            z = zb[zi[0] % 2]
            zi[0] += 1
            B.cp("act", z[:, 1:T + 1], p[:, 0:T], [p], [z])
            B.cp("pool", z[:, 0:1], zlast[:, cidx:cidx + 1], [zlast], [z])
            B.tt("dve", dd.ap, z[:, 0:T], z[:, 1:T + 1], ALU.subtract, [z], [dd])
            B.stt("dve", dest.ap, dd.ap, vcol(l, "mu", cidx), z[:, 1:T + 1], ALU.mult, ALU.add, [dd, vec[l], z], [dest])
            B.cp("pool", zlast[:, cidx:cidx + 1], z[:, T:T + 1], [z], [zlast])

        wv, ws = wcols("w_in", l, 448 + 1536, 256, 8)
        zchunk(wv, ws, 0, 12, zs12)
        zchunk(wv, ws, 128, 13, zs13)
        B.A(txw[0:64, :], zs12[0:64, :], AF.Tanh, [zs12], [txw])
        B.cp("act", xab[64:128, :], zs12[64:128, :], [zs12], [xab])
        B.A(sgb.ap, zs13.ap, AF.Sigmoid, [zs13], [sgb])
        if stage == 21:
            return
        s_wa = lwa
        B.dma("sp", s_wa.ap[0:64, 0:512], wb["rw_w_up"][l], R=[wdep[("rw_w_up", l)]], W=[s_wa])
        B.dma("sp", s_wa.ap[64:128, 0:512], wb["rw_a_up"][l], R=[wdep[("rw_a_up", l)]], W=[s_wa])
        gups = lwg
        gupv = lwg.ap
        B.dma("sp", gupv, wb["rw_g_up"][l], R=[wdep[("rw_g_up", l)]], W=[lwg])
        sm = small
        for hp in range(4):
            s4 = wsl[wsl_i[0]]
            wsl_i[0] = (wsl_i[0] + 1) % NW
            wv4 = s4.ap[:, 0:3072].rearrange("p (j k n) -> p j k n", j=3, k=8)
            ws4 = s4
            for j_ in range(3):
                c0_ = 448 + j_ * 512 + hp * 128
                B.dma("sp", wv4[:, j_, :, :], wb["w_in"][l, :, c0_:c0_ + 128].rearrange("(k p) n -> p k n", p=128),
                      R=[wdep[("w_in", l)]], W=[s4])
            zchunk(wv4[:, 0, :, :], ws4, None, hp, zr)
            zchunk(wv4[:, 1, :, :], ws4, None, 4 + hp, zk)
            zchunk(wv4[:, 2, :, :], ws4, None, 8 + hp, zv)
            X = tmp
            cols = slice(hp * 128, (hp + 1) * 128)
            p = psd()
            B.mm(p[:, 0:T], s_wa.ap[0:64, cols], txw[0:64, :], [s_wa, txw], [p])
            B.A(X["sgw"].ap, p[:, 0:T], AF.Sigmoid, [p, vec[l]], [X["sgw"]], bias=vcol(l, "w0", hp))
            p = psd()
            B.mm(p[:, 0:T], s_wa.ap[64:128, cols], xab[64:128, :], [s_wa, xab], [p])
            B.A(X["aa"].ap, p[:, 0:T], AF.Sigmoid, [p, vec[l]], [X["aa"]], bias=vcol(l, "a0", hp))
            p = psd()
            B.mm(p[:, 0:T], gupv[:, cols], sgb.ap, [gups, sgb], [p])
            B.cp("act", g3[:, hp, :], p[:, 0:T], [p], [gbuf])
            cs = X["cs"]
            onec = cst[:, CI_ONE:CI_ONE + 128]
            for c in range(2):
                sl_ = slice(c * 128, (c + 1) * 128)
                B.op("dve", lambda e, sl_=sl_: e.tensor_tensor_scan(X["cs"][:, sl_], onec, X["sgw"][:, sl_], 0.0,
                                                                    ALU.mult, ALU.add), [cst, X["sgw"]], [X["cs"]])
            for c in range(2):
                B.ts("dve", X["csc"][:, c * 128:(c + 1) * 128], cs[:, c * 128:(c + 1) * 128],
                     cs[:, c * 128 + 63:c * 128 + 64], ALU.subtract, [cs], [X["csc"]])
            for c in range(2):
                B.cp("pool", sm[:, hp * 2 + c:hp * 2 + c + 1], cs[:, c * 128 + 63:c * 128 + 64], [cs], [sm])
                B.cp("pool", sm[:, 8 + hp * 2 + c:8 + hp * 2 + c + 1], X["csc"][:, c * 128 + 127:c * 128 + 128],
                     [X["csc"]], [sm])
            B.tt("dve", X["csx"].ap, X["csc"].ap, X["sgw"].ap, ALU.subtract, [X["csc"], X["sgw"]], [X["csx"]])
            for c in range(2):
                B.ts("dve", X["dC"][:, c * 128:(c + 1) * 128], X["csc"][:, c * 128:(c + 1) * 128],
                     X["csc"][:, c * 128 + 127:c * 128 + 128], ALU.subtract, [X["csc"]], [X["dC"]])
            B.A(X["E1"].ap, X["csc"].ap, AF.Exp, [X["csc"]], [X["E1"]], scale=-CC)
            B.A(X["E2"].ap, X["csc"].ap, AF.Exp, [X["csc"]], [X["E2"]], scale=CC)
            B.A(X["E3"].ap, X["csx"].ap, AF.Exp, [X["csx"]], [X["E3"]], scale=-CC)
            B.A(X["E4"].ap, X["dC"].ap, AF.Exp, [X["dC"]], [X["E4"]], scale=CC)
            B.ts("dve", X["kk"].ap, zk.ap, vcol(l, "k_k", hp), ALU.mult, [zk, vec[l]], [X["kk"]])
            B.A(kk2.ap, X["kk"].ap, AF.Square, [X["kk"]], [kk2])
            p = psd()
            B.mm(p[:, 0:T], blk_bf, kk2.ap, [cbf, kk2], [p])
            B.ts("dve", X["ssm"].ap, p[:, 0:T], 1e-24, ALU.max, [p], [X["ssm"]])
            B.A(X["rs"].ap, X["ssm"].ap, AF.Sqrt, [X["ssm"]], [X["rs"]])
            B.rcp(X["ssm"].ap, X["rs"].ap, [X["rs"]], [X["ssm"]])
            B.tt("dve", X["kkn"].ap, X["kk"].ap, X["ssm"].ap, ALU.mult, [X["kk"], X["ssm"]], [X["kkn"]])
            B.stt("dve", aT_3[:, hp, :], X["kkn"].ap, -1.0, X["E3"].ap, ALU.mult, ALU.mult, [X["kkn"], X["E3"]], [aTt])
            B.tt("dve", X["tb"].ap, X["kkn"].ap, X["aa"].ap, ALU.mult, [X["kkn"], X["aa"]], [X["tb"]])
            B.tt("dve", bT_3[:, hp, :], X["tb"].ap, X["E2"].ap, ALU.mult, [X["tb"], X["E2"]], [bTt])
            B.tt("pool", bh.ap, X["tb"].ap, X["E4"].ap, ALU.mult, [X["tb"], X["E4"]], [bh])
            B.ts("dve", X["tc"].ap, X["aa"].ap, vcol(l, "k_a", hp), ALU.mult, [X["aa"], vec[l], omka[l]], [X["tc"]],
                 s2=omka[l][:, hp:hp + 1], op1=ALU.add)
            B.tt("dve", X["km"].ap, zk.ap, X["tc"].ap, ALU.mult, [zk, X["tc"]], [X["km"]])
            B.tt("dve", kT_3[:, hp, :], X["km"].ap, X["E2"].ap, ALU.mult, [X["km"], X["E2"]], [kTt])
            B.tt("pool", kh.ap, X["km"].ap, X["E4"].ap, ALU.mult, [X["km"], X["E4"]], [kh])
            B.tt("dve", rT_3[:, hp, :], zr.ap, X["E1"].ap, ALU.mult, [zr, X["E1"]], [rTt])
            B.stt("dve", rkr.ap, zr.ap, vcol(l, "r_k", hp), X["km"].ap, ALU.mult, ALU.mult, [zr, vec[l], X["km"]], [rkr])
            p = psd()
            B.mm(p[:, 0:T], blk_bf, rkr.ap, [cbf, rkr], [p])
            B.tt("dve", bonus3[:, hp, :], p[:, 0:T], zv.ap, ALU.mult, [p, zv], [bonus])
            B.cp("pool", vbf.ap, zv.ap, [zv], [vbf])
            if stage == 22:
                continue
            pt = PS[7]
            ptb = pt.ap.bitcast(BF16)
            ptb4 = ptb[:, 0:768].rearrange("p (j c n) -> p j c n", j=3, c=2)
            for j_, sbuf_ in enumerate((vbf, bh, kh)):
                for c in range(2):
                    B.tr(ptb4[:, j_, c, :], sbuf_[:, c * 128:(c + 1) * 128], ident_bf, [sbuf_, cbf], [pt])
            B.cp("act", TM5[:, :, :, hp, :], ptb4, [pt], [TM])
            if stage == 23:
                continue
            bA, bB, bC = PS[4], PS[5], PS[6]
            bA4 = v3(bA.ap, 4)
            bB4 = v3(bB.ap, 4)
            bC4 = v3(bC.ap, 4)
            bks = ((PS[4], PS[5], PS[6]), (PS[1], PS[2], PS[3]))
            Q4 = Qd[0].ap.rearrange("p (c e n) -> p c e n", c=2, e=2)
            QT4 = QTd[0].ap.rearrange("p (c e n) -> p c e n", c=2, e=2)
            for e in range(2):
                kA, kB, kC = bks[e]
                kA4 = kA.ap.rearrange("p (c t n) -> p c t n", c=2, t=2)
                kB4 = kB.ap.rearrange("p (c t n) -> p c t n", c=2, t=2)
                kC3 = v3(kC.ap[:, 0:256], 2)
                pr = slice(e * 64, e * 64 + 64)
                for c in range(2):
                    cs_ = slice(c * 128, (c + 1) * 128)
                    B.mm(kA4[:, c, 0, :], kT_3[pr, hp, cs_], aT_3[pr, hp, cs_], [kTt, aTt], [kA])
                    B.mm(kA4[:, c, 1, :], bT_3[pr, hp, cs_], aT_3[pr, hp, cs_], [bTt, aTt], [kA])
                    B.mm(kB4[:, c, 0, :], bT_3[pr, hp, cs_], rT_3[pr, hp, cs_], [bTt, rTt], [kB])
                    B.mm(kB4[:, c, 1, :], kT_3[pr, hp, cs_], rT_3[pr, hp, cs_], [kTt, rTt], [kB])
                    B.mm(kC3[:, c, :], aT_3[pr, hp, cs_], bT_3[pr, hp, cs_], [aTt, bTt], [kC])
                amds = [AM_d[0][hp], AM_d[1][hp]]
                B.tt("dve", AM6[:, :, hp, 0, e, :], kA4[:, :, 0, :], m_su.unsqueeze(1).to_broadcast([128, 2, 128]),
                     ALU.mult, [kA, cst], amds)
                B.tt("dve", Q4[:, :, e, :], kA4[:, :, 1, :], m_su.unsqueeze(1).to_broadcast([128, 2, 128]),
                     ALU.mult, [kA, cst], [Qd[0]])
                for c in range(2):
                    B.tt("dve", AM6[:, c, hp, 1:3, e, :], kB4[:, c, :, :], m_u.unsqueeze(1).to_broadcast([128, 2, 128]),
                         ALU.mult, [kB, cst], [amds[c]])
                B.tt("dve", QT4[:, :, e, :], kC3, m_sl.unsqueeze(1).to_broadcast([128, 2, 128]), ALU.mult, [kC, cst],
                     [QTd[0]])
            if stage == 24:
                continue
            B.tt("dve", v3(Xd[0].ap, 4), v3(Qd[0].ap, 4), ident_f.unsqueeze(1).to_broadcast([128, 4, 128]), ALU.add,
                 [Qd[0], cst], [Xd[0]])
            B.cp("pool", Xbb.ap, Xd[0].ap, [Xd[0]], [Xbb])
            Qc, QTc, Xc = v3(Qd[0].ap, 4), v3(QTd[0].ap, 4), v3(Xd[0].ap, 4)
            Xb4 = v3(Xbb.ap, 4)
            for j in range(6):
                for m in range(4):
                    B.mm(bA4[:, m, :], Qc[:, m, :], QTc[:, m, :], [Qd[0], QTd[0]], [bA])
                if j < 5:
                    for m in range(4):
                        B.mm(bB4[:, m, :], QTc[:, m, :], Qc[:, m, :], [Qd[0], QTd[0]], [bB])
                B.cp("act", QTc, bA4, [bA], [QTd[0]])
                if j < 5:
                    B.cp("act", Qc, bB4, [bB], [Qd[0]])
                for m in range(4):
                    B.mm(bC4[:, m, :], QTc[:, m, :], Xb4[:, m, :], [QTd[0], Xbb], [bC])
                if j < 5:
                    B.tt("dve", Xc, Xc, bC4, ALU.add, [Xd[0], bC], [Xd[0]])
                    B.cp("pool", Xbb.ap, Xd[0].ap, [Xd[0]], [Xbb])
                else:
                    for c in range(2):
                        B.tt("dve", AM6[:, c, hp, 3, :, :], Xc[:, c * 2:c * 2 + 2, :], bC4[:, c * 2:c * 2 + 2, :],
                             ALU.add, [Xd[0], bC], [AM_d[c][hp]])
        if stage <= 25 and stage >= 21:
            return
        mark("rw_chain", l, tt)
        B.A(sm[:, 16:24], sm[:, 0:8], AF.Exp, [sm], [sm], scale=-CC)
        B.A(sm[:, 24:32], sm[:, 8:16], AF.Exp, [sm], [sm], scale=-CC)
        Pm3 = sm[:, 16:24].rearrange("p (h c) -> p h c", c=2)
        PC3 = sm[:, 24:32].rearrange("p (h c) -> p h c", c=2)
        S3_ = v3(Sst.ap, 4)
        Smid3 = v3(Smid.ap, 4)
        Smb3 = v3(Smb.ap, 4)
        allAM = [AM_d[c][h] for c in range(2) for h in range(4)]
        for c in range(2):
            cs_ = slice(c * 128, (c + 1) * 128)
            amc = AM_d[c]
            B.tt("dve", Smid3, S3_, Pm3[:, :, c:c + 1].to_broadcast([128, 4, 64]), ALU.mult, [Sst, sm], [Smid])
            B.cp("dve", Smb.ap, Smid.ap, [Smid], [Smb])
            pWe = (PS[0], PS[1])
            pU, pS = PS[2], PS[3]
            pYe = (PS[4], PS[5])
            pU3 = v3(pU.ap, 8)
            pS3 = v3(pS.ap[:, 0:256], 4)
            W0b3 = v3(W0b.ap, 8)
            Ut3 = v3(Ut.ap, 8)
            W0b4 = W0b.ap.rearrange("p (h e v) -> p h e v", h=4, e=2)
            for e in range(2):
                pr = slice(e * 64, e * 64 + 64)
                pW3 = v3(pWe[e].ap[:, 0:256], 4)
                for hp in range(4):
                    B.mm(pW3[:, hp, :], AM6[:, c, hp, 0, e, :], TM5[:, 0, c, hp, e * 64:e * 64 + 64], [amc[hp], TM],
                         [pWe[e]], start=True, stop=False)
                    B.mm(pW3[:, hp, :], aT_3[pr, hp, cs_], Smb3[pr, hp, :], [aTt, Smb], [pWe[e]], start=False, stop=True)
                B.cp("act", W0b4[:, :, e, :], pW3, [pWe[e]], [W0b])
            for hp in range(4):
                for e in range(2):
                    h = hp * 2 + e
                    B.mm(pU3[:, h, :], AM6[:, c, hp, 3, e, :], W0b3[:, h, :], [amc[hp], W0b], [pU])
            B.cp("act", Ut.ap, pU.ap, [pU], [Ut])
            for e in range(2):
                pr = slice(e * 64, e * 64 + 64)
                pY3 = v3(pYe[e].ap, 4)
                for hp in range(4):
                    h = hp * 2 + e
                    B.mm(pY3[pr, hp, :], Smb3[pr, hp, :], rT_3[pr, hp, cs_], [Smb, rTt], [pYe[e]], start=True, stop=False)
                    B.mm(pY3[pr, hp, :], Ut3[:, h, :], AM6[:, c, hp, 1, e, :], [Ut, amc[hp]], [pYe[e]], start=False,
                         stop=False)
                    B.mm(pY3[pr, hp, :], TM5[:, 0, c, hp, e * 64:e * 64 + 64], AM6[:, c, hp, 2, e, :], [TM, amc[hp]],
                         [pYe[e]], start=False, stop=True)
                B.cp("act", y3[pr, :, cs_], pY3[pr, :, :], [pYe[e]], [yb])
            for hp in range(4):
                for e in range(2):
                    h = hp * 2 + e
                    pr = slice(e * 64, e * 64 + 64)
                    B.mm(pS3[pr, hp, :], TM5[:, 1, c, hp, e * 64:e * 64 + 64], Ut3[:, h, :], [TM, Ut], [pS], start=True,
                         stop=False)
                    B.mm(pS3[pr, hp, :], TM5[:, 2, c, hp, e * 64:e * 64 + 64], TM5[:, 0, c, hp, e * 64:e * 64 + 64],
                         [TM], [pS], start=False, stop=True)
            B.tt("dve", S3_, Smid3, PC3[:, :, c:c + 1].to_broadcast([128, 4, 64]), ALU.mult, [Smid, sm], [Sst])
            B.tt("dve", S3_, S3_, pS3, ALU.add, [Sst, pS], [Sst])
        if stage == 26:
            return
        mark("rw_norm", l, tt)
        for hp in range(4):
            B.cp("pool", ybf.ap, y3[:, hp, :], [yb], [ybf])
            p = psd()
            B.mm(p[:, 0:T], blk_bf, ybf.ap, [cbf, ybf], [p])
            B.stt("dve", yc.ap, p[:, 0:T], -1.0 / 64, y3[:, hp, :], ALU.mult, ALU.add, [p, yb], [yc])
            B.A(ysq.ap, yc.ap, AF.Square, [yc], [ysq])
            p = psd()
            B.mm(p[:, 0:T], blk_bf, ysq.ap, [cbf, ysq], [p])
            B.A(sd.ap, p[:, 0:T], AF.Sqrt, [p, eps_t], [sd], scale=1.0 / 64, bias=eps_ap(LN_EPS))
            B.rcp(t1.ap, sd.ap, [sd], [t1])
            B.tt("dve", t2.ap, yc.ap, t1.ap, ALU.mult, [yc, t1], [t2])
            B.ts("dve", t1.ap, t2.ap, vcol(l, "ln_w", hp), ALU.mult, [t2, vec[l]], [t1], s2=vcol(l, "ln_b", hp),
                 op1=ALU.add)
            B.tt("dve", t2.ap, t1.ap, bonus3[:, hp, :], ALU.add, [t1, bonus], [t2])
            B.tt("dve", o_rw[:, hp * T:(hp + 1) * T], t2.ap, g3[:, hp, :], ALU.mult, [t2, gbuf], [o_rw])

    for l in range(nl):
        layer(l)
    B.emit(block)
    es.__exit__(None, None, None)
    return nc


def _consts():
    c = np.zeros((128, NCONST), np.float32)
    i = np.arange(128)
    c[:, CI_ID:CI_ID + 128] = np.eye(128)
    c[:, CI_J:CI_J + 128] = np.eye(128)[::-1]
    c[:, CI_ONE:CI_ONE + 128] = 1.0
    blk = np.zeros((128, 128), np.float32)
    blk[:64, :64] = 1
    blk[64:, 64:] = 1
    c[:, CI_BLK:CI_BLK + 128] = blk
    c[:, CI_SU:CI_SU + 128] = (i[None, :] > i[:, None])
    c[:, CI_U:CI_U + 128] = (i[None, :] >= i[:, None])
    c[:, CI_SL:CI_SL + 128] = (i[None, :] < i[:, None])
    half = 32
    invf = (1.0 / (np.float32(10000.0) ** (np.arange(half, dtype=np.float32) / np.float32(half)))).astype(np.float32)
    c[:64, CI_INVF] = np.concatenate([invf, invf])
    c[:64, CI_SGN] = np.concatenate([-np.ones(32), np.ones(32)]) * TWO_PI
    return c


def prep_shared(inp, nl=NL):
    f = lambda a: np.ascontiguousarray(np.asarray(a, dtype=np.float32))
    sh = {}
    w_in = f(inp["w_in"])[:nl]
    sh["w_in"] = w_in
    kr = w_in[:, :, 384:448]
    sh["w_in_sw"] = np.ascontiguousarray(np.concatenate([kr[:, :, 32:], kr[:, :, :32]], axis=-1))
    wuq = f(inp["mla_w_uq"])[:nl]
    sh["w_uq"] = wuq
    r4 = wuq.reshape(nl, 256, 4, 192)[:, :, :, 128:]
    sh["w_uq_sw"] = np.ascontiguousarray(np.concatenate([r4[..., 32:], r4[..., :32]], axis=-1).reshape(nl, 256, 256))
    wukv = f(inp["mla_w_ukv"])[:nl]
    sh["w_ukv"] = wukv
    nope = wukv.reshape(nl, 128, 4, 256)[:, :, :, :128]
    sh["w_ukT"] = np.ascontiguousarray(nope.transpose(0, 2, 3, 1).reshape(nl, 512, 128))
    sh["rw_w_up"] = f(inp["rw_w_up"])[:nl]
    sh["rw_a_up"] = f(inp["rw_a_up"])[:nl]
    sh["rw_g_up"] = f(inp["rw_g_up"])[:nl]
    for n in ("w_branch", "w_out", "w_ff1", "w_ff2", "w_ple_gate", "w_ple_proj"):
        sh[n] = f(inp[n])[:nl]
    vecs = np.zeros((nl, 128, NV), np.float32)

    def put(name, arr):
        a = f(arr)[:nl].reshape(nl, -1, 128)
        vecs[:, :, VC[name]:VC[name] + a.shape[1]] = a.transpose(0, 2, 1)

    put("pre_mix_g", inp["pre_mix_g"])
    put("q_g", inp["mla_q_norm_g"])
    put("kv_g", inp["mla_kv_norm_g"])
    put("mu", inp["rw_mu"])
    put("w0", inp["rw_w0"])
    put("a0", inp["rw_a0"])
    put("k_k", inp["rw_k_k"])
    put("k_a", inp["rw_k_a"])
    put("r_k", np.asarray(inp["rw_r_k"]).reshape(-1, 512))
    put("ln_w", inp["rw_ln_w"])
    put("ln_b", inp["rw_ln_b"])
    put("post_mix_g", inp["post_mix_g"])
    put("pre_ff_g", inp["pre_ff_g"])
    put("post_ff_g", inp["post_ff_g"])
    sh["vecs"] = vecs
    sh["consts"] = _consts()
    rel = f(inp["ca_rel_bias"])[:nl]
    sh["relr"] = np.ascontiguousarray(rel[:, ::-1, :].transpose(0, 2, 1))
    return sh


def prep_core(inp, b, S, nl=NL):
    d = {}
    d["xT"] = np.ascontiguousarray(np.asarray(inp["x"], dtype=np.float32)[b, :S].T)
    d["pT"] = np.ascontiguousarray(np.asarray(inp["p"], dtype=np.float32)[:nl, b, :S].transpose(0, 2, 1))
    d["pos"] = np.ascontiguousarray(np.asarray(inp["positions"]).astype(np.int32)[b:b + 1, :S])
    return d


_NC_CACHE = {}
PHASES = []


def kernel(**inputs):
    key = (SEQ, NL)
    if key not in _NC_CACHE:
        _NC_CACHE[key] = build(SEQ, NL)
    nc = _NC_CACHE[key]
    sh = prep_shared(inputs)
    in_maps = []
    for b in range(NB):
        m = dict(sh)
        m.update(prep_core(inputs, b, SEQ))
        in_maps.append(m)
    res = run_bass_kernel_spmd(nc, in_maps, core_ids=list(range(NB)))
    out = np.stack([np.asarray(r["outT"]).T for r in res.results], axis=0)
    return np.ascontiguousarray(out.astype(np.float32))
```

```python
import contextlib
import numpy as np
import ml_dtypes
import concourse.bass as bass
import concourse.mybir as mybir
from concourse.bass_utils import run_bass_kernel_spmd

F32 = mybir.dt.float32
BF16 = mybir.dt.bfloat16
I32 = mybir.dt.int32
AF = mybir.ActivationFunctionType
ALU = mybir.AluOpType

D = 1024
DFF = 4096
DPLE = 256
INC = 6848
NL = 2
SEQ = 4096
NB = 8
T = 256
EPS = 1e-6
LN_EPS = 64e-5
CC = float(np.exp(-0.5))
MLA_SCALE = float(192 ** -0.5)
TWO_PI = 6.2831845
NEG = -30000.0

VC = {}
_o = 0
for _n, _k in [("pre_mix_g", 8), ("q_g", 2), ("kv_g", 1), ("mu", 14), ("w0", 4), ("a0", 4), ("k_k", 4),
               ("k_a", 4), ("r_k", 4), ("ln_w", 4), ("ln_b", 4), ("post_mix_g", 8), ("pre_ff_g", 8),
               ("post_ff_g", 8)]:
    VC[_n] = _o
    _o += _k
NV = _o
CI_ID, CI_J, CI_ONE, CI_BLK, CI_SU, CI_U, CI_SL = [i * 128 for i in range(7)]
CI_INVF = 7 * 128
CI_SGN = CI_INVF + 1
NCONST = CI_SGN + 1

WSPEC = {
    "w_in": (1024, INC), "w_in_sw": (1024, 64), "w_uq": (256, 768), "w_uq_sw": (256, 256),
    "w_ukT": (512, 128), "w_ukv": (128, 1024), "rw_w_up": (64, 512), "rw_a_up": (64, 512),
    "rw_g_up": (128, 512), "w_branch": (1536, 1024), "w_out": (1024, 1024), "w_ff1": (1024, 4096),
    "w_ff2": (4096, 1024), "w_ple_gate": (1024, 1024), "w_ple_proj": (256, 1024),
}
WORDER = ["w_in", "w_in_sw", "w_uq", "w_uq_sw", "w_ukT", "w_ukv", "rw_w_up", "rw_a_up", "rw_g_up",
          "w_branch", "w_out", "w_ff1", "w_ff2", "w_ple_gate", "w_ple_proj"]


class Dep:
    __slots__ = ("w", "r", "al")

    def __init__(self):
        self.w = None
        self.r = {}
        self.al = []


class Buf:
    def __init__(self, ap, d=None):
        self.ap = ap
        self.d = d if d is not None else Dep()

    def __getitem__(self, k):
        return self.ap[k]


def v3(ap, a):
    return ap.rearrange("p (a b) -> p a b", a=a)


ENGS = ("pe", "act", "dve", "pool", "sp")
NDS = 8


class Builder:
    def __init__(self, nc, es):
        self.nc = nc
        self.ops = {e: [] for e in ENGS}
        self.cnt = {e: 0 for e in ENGS}
        self.waited = {e: {} for e in ENGS}
        self.sems = []
        self.semid = {}
        for e in ENGS:
            self.semid[e] = len(self.sems)
            self.sems.append(es.enter_context(nc.semaphore("s_" + e)))
        self.dsem = {}
        self.dcnt = {}
        for q in ("sp", "pool", "act"):
            self.dsem[q] = []
            self.dcnt[q] = 0
            for i in range(NDS):
                self.dsem[q].append(len(self.sems))
                self.sems.append(es.enter_context(nc.semaphore("d_%s%d" % (q, i))))

    def _waits(self, eng, reads, writes):
        need = {}

        def add(sid, val):
            if need.get(sid, 0) < val:
                need[sid] = val

        for d in reads:
            if d.w is not None:
                add(*d.w)
        for d in writes:
            if d.w is not None:
                add(*d.w)
            for sid, val in d.r.items():
                add(sid, val)
            for a in d.al:
                if a.w is not None:
                    add(*a.w)
                for sid, val in a.r.items():
                    add(sid, val)
        out = []
        wd = self.waited[eng]
        pe_sid = self.semid["pe"]
        for sid, val in need.items():
            if eng == "pe" and sid == pe_sid:
                continue
            if wd.get(sid, 0) >= val:
                continue
            wd[sid] = val
            out.append((sid, val))
        return out

    def _mark(self, reads, writes, sid, val):
        for d in reads:
            if d.r.get(sid, 0) < val:
                d.r[sid] = val
        for d in writes:
            d.w = (sid, val)
            d.r = {}

    def op(self, eng, fn, R=(), W=()):
        R = [x.d if isinstance(x, Buf) else x for x in R]
        W = [x.d if isinstance(x, Buf) else x for x in W]
        ws = self._waits(eng, R, W)
        self.cnt[eng] += 1
        sid = self.semid[eng]
        self.ops[eng].append((ws, fn, sid, 1))
        self._mark(R, W, sid, self.cnt[eng])

    def dma(self, q, out, in_, R=(), W=(), **kw):
        R = [x.d if isinstance(x, Buf) else x for x in R]
        W = [x.d if isinstance(x, Buf) else x for x in W]
        ws = self._waits(q, R, W)
        i = self.dcnt[q]
        self.dcnt[q] += 1
        sid = self.dsem[q][i % NDS]
        val = 16 * (i // NDS + 1)
        if val > 16 and self.waited[q].get(sid, 0) < val - 16:
            self.waited[q][sid] = val - 16
            ws.append((sid, val - 16))
        self.ops[q].append((ws, lambda e, o=out, i_=in_, k=kw: e.dma_start(out=o, in_=i_, **k), sid, 16))
        self._mark(R, W, sid, val)

    def mm(self, out, lhsT, rhs, R, W, start=True, stop=True):
        self.pes = getattr(self, "pes", 0) + (2 if lhsT.dtype == F32 else 1)
        self.op("pe", lambda e: e.matmul(out, lhsT, rhs, start=start, stop=stop, skip_group_check=True), R, W)

    def tr(self, out, in_, ident, R, W):
        self.pes = getattr(self, "pes", 0) + 1
        self.op("pe", lambda e: e.transpose(out, in_, ident), R, W)

    def A(self, out, in_, func, R, W, scale=None, bias=None):
        kw = {}
        if scale is not None:
            kw["scale"] = scale
        if bias is not None:
            kw["bias"] = bias
        self.op("act", lambda e: e.activation(out, in_, func, **kw), R, W)

    def tt(self, eng, out, a, b, op, R, W):
        self.op(eng, lambda e: e.tensor_tensor(out, a, b, op), R, W)

    def ts(self, eng, out, a, s1, op0, R, W, s2=None, op1=None):
        if op1 is None:
            self.op(eng, lambda e: e.tensor_scalar(out, a, s1, None, op0), R, W)
        else:
            self.op(eng, lambda e: e.tensor_scalar(out, a, s1, s2, op0, op1), R, W)

    def stt(self, eng, out, a, s, b, op0, op1, R, W):
        self.op(eng, lambda e: e.scalar_tensor_tensor(out, a, s, b, op0, op1), R, W)

    def cp(self, eng, out, in_, R, W):
        if eng == "act":
            self.op("act", lambda e: e.activation(out, in_, AF.Copy), R, W)
        else:
            self.op(eng, lambda e: e.tensor_copy(out, in_), R, W)

    def rcp(self, out, in_, R, W):
        self.op("dve", lambda e: e.reciprocal(out, in_), R, W)

    def ms(self, eng, ap, val, W):
        self.op(eng, lambda e: e.memset(ap, val), (), W)

    def emit(self, block):
        sems = self.sems
        B = self

        def run(e, name):
            for ws, fn, sid, inc in B.ops[name]:
                for s_, v_ in ws:
                    e.wait_ge(sems[s_], v_)
                fn(e).then_inc(sems[sid], inc)

        fin = []
        for en in ENGS:
            if self.cnt[en] > 0:
                fin.append((self.semid[en], self.cnt[en]))
        for q in ("sp", "pool", "act"):
            n = self.dcnt[q]
            for k in range(NDS):
                cntk = (n - k + NDS - 1) // NDS if n > k else 0
                if cntk > 0:
                    fin.append((self.dsem[q][k], 16 * cntk))

        @block.tensor
        def _(e):
            run(e, "pe")

        @block.scalar
        def _(e):
            run(e, "act")

        @block.vector
        def _(e):
            run(e, "dve")

        @block.gpsimd
        def _(e):
            run(e, "pool")

        @block.sync
        def _(e):
            run(e, "sp")
            for s_, v_ in fin:
                e.wait_ge(sems[s_], v_)


def build(S=SEQ, nl=NL, dbg=False, stage=99):
    NT = S // T
    NBLK = S // 128
    nc = bass.Bass("TRN2", target_bir_lowering=False)
    es = contextlib.ExitStack()
    es.__enter__()
    B = Builder(nc, es)

    def dram(name, shape, dt, kind):
        return nc.dram_tensor(name, list(shape), dt, kind=kind).ap()

    xT = dram("xT", [D, S], F32, "ExternalInput")
    pT = dram("pT", [nl, DPLE, S], F32, "ExternalInput")
    pos = dram("pos", [1, S], I32, "ExternalInput")
    vecs = dram("vecs", [nl, 128, NV], F32, "ExternalInput")
    consts = dram("consts", [128, NCONST], F32, "ExternalInput")
    relr = dram("relr", [nl, 8, 320], F32, "ExternalInput")
    wf = {}
    wb = {}
    wdep = {}
    for n in WORDER:
        r, c = WSPEC[n]
        wf[n] = dram(n, [nl, r, c], F32, "ExternalInput")
        wb[n] = dram(n + "_b", [nl, r, c], BF16, "Internal")
        for l in range(nl):
            wdep[(n, l)] = Dep()
    outT = dram("outT", [D, S], F32, "ExternalOutput")
    hscr = [dram("hscr%d" % i, [D, S], F32, "Internal") for i in range(max(nl - 1, 1))]
    hscr_d = [[Dep() for _ in range(NT)] for _ in range(max(nl - 1, 1))]
    ext = dram("ext", [nl, 8, 768], F32, "Internal")
    ext_d = [Dep() for _ in range(nl)]
    dbg_t = {}
    if dbg:
        for n in ("o_mla", "o_rw", "o_ca"):
            dbg_t[n] = dram("dbg_" + n, [512, S], BF16, "ExternalOutput")

    def sb(name, shape, dt):
        return Buf(es.enter_context(nc.sbuf_tensor(name, list(shape), dt))[:])

    cst = sb("cst", [128, NCONST], F32)
    vec = [sb("vec%d" % l, [128, NV], F32) for l in range(nl)]
    omka = [sb("omka%d" % l, [128, 4], F32) for l in range(nl)]
    cbf = sb("cbf", [128, 4 * 128], BF16)
    ident_bf = cbf[:, 0:128]
    J_bf = cbf[:, 128:256]
    ones_bf = cbf[:, 256:384]
    blk_bf = cbf[:, 384:512]
    ident_f = cst[:, CI_ID:CI_ID + 128]
    m_su = cst[:, CI_SU:CI_SU + 128]
    m_u = cst[:, CI_U:CI_U + 128]
    m_sl = cst[:, CI_SL:CI_SL + 128]

    hT = sb("hT", [128, 8 * T], F32)
    hT3 = v3(hT.ap, 8)
    uT = sb("uT", [128, 8 * T], BF16)
    uT3 = v3(uT.ap, 8)
    NW = 3
    wsl = [sb("wsl%d" % i, [128, 4096], BF16) for i in range(NW)]
    wsl_i = [0]
    Kc = sb("Kc", [128, S], BF16)
    Kr = sb("Kr", [64, S], BF16)
    Vc = sb("Vc", [128, S], BF16)
    Vc3 = v3(Vc.ap, NBLK)
    Kc_d = [Dep() for _ in range(NT)]
    CK = sb("CK", [128, 4 * 1024], BF16)
    CK3 = v3(CK.ap, 4)
    CV = sb("CV", [128, 8 * 512], BF16)
    CV3 = v3(CV.ap, 8)
    CK_d = [Dep() for _ in range(4)]
    CV_d = [Dep() for _ in range(8)]
    Xb = sb("Xb", [128, 40 * 128], BF16)
    Xb3 = v3(Xb.ap, 40)
    o_mla = sb("o_mla", [128, 4 * T], BF16)
    o_rw = sb("o_rw", [128, 4 * T], BF16)
    o_ca = sb("o_ca", [128, 4 * T], BF16)
    Sst = sb("Sst", [128, 256], F32)
    zlast = sb("zlast", [128, 16], F32)
    small = sb("small", [128, 64], F32)

    AW = 26400
    arena = es.enter_context(nc.sbuf_tensor("arena", [128, AW], F32))[:]
    abufs = []
    aoff = {}

    def al(phase, name, n, dt):
        words = (n + 1) // 2 if dt == BF16 else n
        o = aoff.get(phase, 0)
        aoff[phase] = o + words
        assert o + words <= AW, (phase, name, o + words)
        ap = arena[:, o:o + words]
        if dt == BF16:
            ap = ap.bitcast(BF16)[:, 0:n]
        b = Buf(ap)
        for (ph2, o2, e2, b2) in abufs:
            if ph2 != phase and o2 < o + words and o < e2:
                b.d.al.append(b2.d)
                b2.d.al.append(b.d)
        abufs.append((phase, o, o + words, b))
        return b

    def al_all(name, n, dt):
        words = (n + 1) // 2 if dt == BF16 else n
        o = max([aoff.get(p, 0) for p in ("mla", "ca", "rw", "ffn")])
        for p in ("mla", "ca", "rw", "ffn"):
            assert aoff.get(p, 0) <= o
            aoff[p] = o + words
        ap = arena[:, o:o + words]
        if dt == BF16:
            ap = ap.bitcast(BF16)[:, 0:n]
        return Buf(ap)

    sqb = al_all("sqb", 8 * T, BF16)
    sqb3 = v3(sqb.ap, 8)
    rt = al_all("rt", T, F32)
    rt2 = al_all("rt2", T, F32)
    mo = al_all("mo", 8 * T, F32)
    mo3 = v3(mo.ap, 8)

    PS = [Buf(es.enter_context(nc.psum_tensor("ps%d" % i, [128, 512], F32))[:]) for i in range(8)]
    rot = {"d": 0, "n": 4}

    def psd():
        i = rot["d"] % rot["n"]
        rot["d"] = (i + 1) % rot["n"]
        return PS[i]

    block = es.enter_context(nc.Block())

    B.dma("sp", cst.ap, consts, W=[cst])
    for l in range(nl):
        B.dma("sp", vec[l].ap, vecs[l], W=[vec[l]])
    for l in range(nl):
        for n in WORDER:
            r, c = WSPEC[n]
            npc = (c + 2047) // 2048
            pc = c // npc
            assert pc * npc == c
            for i in range(npc):
                B.dma("pool", wb[n][l, :, i * pc:(i + 1) * pc], wf[n][l, :, i * pc:(i + 1) * pc], W=[wdep[(n, l)]])
    B.cp("dve", cbf[:, 0:512], cst[:, 0:512], [cst], [cbf])
    for l in range(nl):
        ka = vec[l][:, VC["k_a"]:VC["k_a"] + 4]
        B.ts("dve", omka[l].ap, ka, -1.0, ALU.mult, [vec[l]], [omka[l]], s2=1.0, op1=ALU.add)

    def vcol(l, name, i):
        c = VC[name] + i
        return vec[l][:, c:c + 1]

    def wload(name, l, dram_ap, shape, prt=None):
        s = wsl[wsl_i[0]]
        wsl_i[0] = (wsl_i[0] + 1) % NW
        n = int(np.prod(shape[1:]))
        p0, p1 = prt if prt is not None else (0, shape[0])
        view = s.ap[p0:p1, 0:n]
        if len(shape) == 3:
            view = view.rearrange("p (a b) -> p a b", a=shape[1])
        elif len(shape) == 4:
            view = view.rearrange("p (a b c) -> p a b c", a=shape[1], b=shape[2])
        B.dma("sp", view, dram_ap, R=[wdep[(name, l)]], W=[s])
        return view, s

    def wcols(name, l, c0, n, kc):
        ap = wb[name][l, 0:kc * 128, c0:c0 + n].rearrange("(k p) n -> p k n", p=128)
        return wload(name, l, ap, [128, kc, n])

    def rms(src3, nk, ktot, l, gname, out_fn, R, Wd, eps=EPS, ps=None):
        B.A(sqb3[:, 0:nk, :], src3, AF.Square, R, [sqb])
        p = psd()
        for k in range(nk):
            B.mm(p[:, 0:T], ones_bf, sqb3[:, k, :], [sqb, cbf], [p], start=(k == 0), stop=(k == nk - 1))
        B.A(rt.ap, p[:, 0:T], AF.Sqrt, [p], [rt], scale=1.0 / ktot, bias=eps_ap(eps))
        B.rcp(rt2.ap, rt.ap, [rt], [rt2])

    eps_t = sb("eps_t", [128, 4], F32)
    B.ms("dve", eps_t[:, 0:1], EPS, [eps_t])
    B.ms("dve", eps_t[:, 1:2], LN_EPS, [eps_t])
    B.ms("dve", eps_t[:, 2:3], 0.0, [eps_t])
    B.ms("dve", eps_t[:, 3:4], 0.25, [eps_t])

    def eps_ap(e):
        return eps_t[:, 0:1] if e == EPS else eps_t[:, 1:2]

    zq = al("mla", "zq", 2 * T, F32)
    zq3 = v3(zq.ap, 2)
    zqn = al("mla", "zqn", 2 * T, BF16)
    zqn3 = v3(zqn.ap, 2)
    qn = [al("mla", "qn%d" % i, T, BF16) for i in range(2)]
    Qabs = al("mla", "Qabs", 4 * T, BF16)
    Qabs3 = v3(Qabs.ap, 4)
    Qrope = al("mla", "Qrope", 4 * T, BF16)
    Qrope3 = v3(Qrope.ap, 4)
    zkv = al("mla", "zkv", T, F32)
    cosT = al("mla", "cosT", T, F32)
    sinT = al("mla", "sinT", T, F32)
    posi = al("mla", "posi", T, F32)
    posf = al("mla", "posf", T, F32)
    tq = al("mla", "tq", T, F32)
    tq2 = al("mla", "tq2", T, F32)
    rp1 = al("mla", "rp1", T, F32)
    rp2 = al("mla", "rp2", T, F32)
    PTm = [al("mla", "PTm%d" % i, 2 * T, BF16) for i in range(2)]
    rinv = al("mla", "rinv", 2 * T, F32)
    On = al("mla", "On", 2 * T, BF16)
    Qs = al("rw", "Qs", 4 * T, BF16)
    Qs3 = v3(Qs.ap, 4)
    PTc = [al("rw", "PTc%d" % i, 512, BF16) for i in range(2)]
    rinvc = al("rw", "rinvc", 512, F32)
    aT = al("ffn", "aT", 32 * T, BF16)
    aT3 = v3(aT.ap, 32)
    rl = [al("ffn", "rl%d" % i, T, F32) for i in range(2)]
    mrg = al("ffn", "mrg", 8 * T, F32)
    mrg3 = v3(mrg.ap, 8)
    mrgb = al("ffn", "mrgb", 8 * T, BF16)
    mrgb3 = v3(mrgb.ap, 8)
    gsb = [al("ffn", "gsb%d" % i, T, F32) for i in range(2)]
    tmpf = [al("ffn", "tmpf%d" % i, T, F32) for i in range(2)]
    pb = al("ffn", "pb", 2 * T, BF16)
    pb3 = v3(pb.ap, 2)
    zb = [al("rw", "zb%d" % i, T + 2, F32) for i in range(2)]
    dd = al("rw", "dd", T, F32)
    zr = al("rw", "zr", T, F32)
    zk = al("rw", "zk", T, F32)
    zs12 = zr
    zs13 = zk
    zv = al("rw", "zv", T, F32)
    txw = al("rw", "txw", T, BF16)
    xab = al("rw", "xab", T, BF16)
    sgb = al("rw", "sgb", T, BF16)
    tmp = {n: al("rw", n, T, F32) for n in ("sgw", "cs", "csc", "dC", "E1", "aa", "kk", "ssm", "rs", "tb", "tc", "km")}
    tmp["csx"] = tmp["cs"]
    tmp["E3"] = tmp["cs"]
    tmp["E2"] = tmp["csc"]
    tmp["E4"] = tmp["dC"]
    tmp["kkn"] = tmp["kk"]
    kk2 = al("rw", "kk2", T, BF16)
    rkr = al("rw", "rkr", T, BF16)
    vbf = al("rw", "vbf", T, BF16)
    bh = al("rw", "bh", T, BF16)
    kh = al("rw", "kh", T, BF16)
    aTt = al("rw", "aTt", 4 * T, BF16)
    bTt = al("rw", "bTt", 4 * T, BF16)
    kTt = al("rw", "kTt", 4 * T, BF16)
    rTt = al("rw", "rTt", 4 * T, BF16)
    aT_3, bT_3, kT_3, rT_3 = (v3(x.ap, 4) for x in (aTt, bTt, kTt, rTt))
    TM = al("rw", "TM", 3 * 2 * 4 * 128, BF16)
    TM5 = TM.ap.rearrange("p (j c h n) -> p j c h n", j=3, c=2, h=4)
    AM = al("rw", "AM", 2 * 4 * 4 * 2 * 128, BF16)
    AM6 = AM.ap.rearrange("p (c h t e n) -> p c h t e n", c=2, h=4, t=4, e=2)
    AM_d = [[Dep() for _ in range(4)] for _ in range(2)]
    for cc_ in range(2):
        for hh_ in range(4):
            AM_d[cc_][hh_].al = AM.d.al
    Qd = [al("rw", "Qd%d" % i, 4 * 128, BF16) for i in range(2)]
    QTd = [al("rw", "QTd%d" % i, 4 * 128, BF16) for i in range(2)]
    Xd = [al("rw", "Xd%d" % i, 4 * 128, F32) for i in range(2)]
    Xbbs = [al("rw", "Xbb%d" % i, 4 * 128, BF16) for i in range(2)]
    yb = al("rw", "y", 4 * T, F32)
    y3 = v3(yb.ap, 4)
    bonus = al("rw", "bonus", 4 * T, F32)
    bonus3 = v3(bonus.ap, 4)
    gbuf = al("rw", "g", 4 * T, BF16)
    g3 = v3(gbuf.ap, 4)
    Smid = al("rw", "Smid", 256, F32)
    Smb = al("rw", "Smb", 256, BF16)
    W0b = al("rw", "W0b", 512, BF16)
    Ut = al("rw", "Ut", 512, BF16)
    lwa = al("rw", "lwa", 512, BF16)
    lwg = al("rw", "lwg", 512, BF16)
    ybf = al("rw", "ybf", T, BF16)
    yc = al("rw", "yc", T, F32)
    ysq = al("rw", "ysq", T, BF16)
    sd = al("rw", "sd", T, F32)
    t1 = al("rw", "t1", T, F32)
    t2 = al("rw", "t2", T, F32)

    def layer(l):
        src = xT if l == 0 else hscr[l - 1]
        dst = outT if l == nl - 1 else hscr[l]
        src_d = None if l == 0 else hscr_d[l - 1]
        dst_d = None if l == nl - 1 else hscr_d[l]
        B.ms("dve", Sst.ap, 0.0, [Sst])
        B.ms("dve", zlast.ap, 0.0, [zlast])
        etap = mo.ap[0:8, 0:768]
        rl_ap = mo.ap[0:8, 768:768 + 320]
        B.dma("sp", rl_ap, relr[l], W=[mo])
        B.cp("dve", etap[:, 383:703], rl_ap, [mo], [mo])
        B.cp("dve", etap[:, 0:383], rl_ap[:, 0:1].to_broadcast([8, 383]), [mo], [mo])
        B.cp("dve", etap[:, 703:768], rl_ap[:, 319:320].to_broadcast([8, 65]), [mo], [mo])
        B.dma("sp", ext[l], etap, R=[mo], W=[ext_d[l]])
        for r in range(5):
            for h in range(8):
                off = 639 - 128 * r - 127
                srcap = bass.AP(tensor=ext.tensor, offset=(l * 8 + h) * 768 + off, ap=[[1, 128], [1, 128]])
                B.dma("pool", Xb3[:, r * 8 + h, :], srcap, R=[ext_d[l]], W=[Xb])
        for h in range(8):
            B.ms("pool", Xb3[64:128, 0 * 8 + h, 64:128], NEG, [Xb])
            B.ms("pool", Xb3[0:64, 4 * 8 + h, 0:64], NEG, [Xb])

        for tt in range(NT):
            tile(l, tt, src, dst, src_d, dst_d)

    def mark(name, l, tt):
        PHASES.append((name, l, tt, getattr(B, "pes", 0)))

    def tile(l, tt, src, dst, src_d, dst_d):
        t0 = tt * T
        mark("start", l, tt)
        R_src = [] if src_d is None else [src_d[tt]]
        B.dma("sp", hT3, src[:, t0:t0 + T].rearrange("(k p) t -> p k t", p=128), R=R_src, W=[hT])
        rms(hT3, 8, D, l, "pre_mix_g", None, [hT], None)
        for k in range(8):
            B.stt("dve", uT3[:, k, :], hT3[:, k, :], vcol(l, "pre_mix_g", k), rt2.ap, ALU.mult, ALU.mult,
                  [hT, vec[l], rt2], [uT])

        def fin():
            W_dst = [] if dst_d is None else [dst_d[tt]]
            B.dma("sp", dst[:, t0:t0 + T].rearrange("(k p) t -> p k t", p=128), hT3, R=[hT], W=W_dst)

        if stage <= 0:
            return fin()

        def zmm(p, wv, ws, c0, n, M=None):
            for k in range(8):
                B.mm(p[0:n, 0:T], wv[:, k, c0:c0 + n], uT3[:, k, :], [ws, uT], [p], start=(k == 0), stop=(k == 7))

        mark("mla", l, tt)
        wv, ws = wcols("w_in", l, 0, 448, 8)
        wsw, wsws = wcols("w_in_sw", l, 0, 64, 8)
        for c in range(2):
            p = psd()
            zmm(p, wv, ws, c * 128, 128)
            B.cp("act", zq3[:, c, :], p[:, 0:T], [p], [zq])
        p = psd()
        zmm(p, wv, ws, 256, 128)
        B.cp("act", zkv.ap, p[:, 0:T], [p], [zkv])
        B.dma("sp", posi.ap[0:64, :].bitcast(I32), pos[0:1, t0:t0 + T].to_broadcast([64, T]), W=[posi])
        B.cp("dve", posf[0:64, :], posi.ap[0:64, :].bitcast(I32), [posi], [posf])
        B.ts("dve", tq[0:64, :], posf[0:64, :], cst[0:64, CI_INVF:CI_INVF + 1], ALU.mult, [posf, cst], [tq],
             s2=float(1.0 / (2 * np.pi)), op1=ALU.mult)
        MAGIC = 12582912.0
        for (dstb, shift) in ((sinT, 0.0), (cosT, 0.25)):
            if shift != 0.0:
                B.ts("dve", tq2[0:64, :], tq[0:64, :], shift, ALU.add, [tq], [tq2])
                srcq = tq2
            else:
                srcq = tq
            B.ts("dve", rp1[0:64, :], srcq[0:64, :], MAGIC, ALU.add, [srcq], [rp1], s2=MAGIC, op1=ALU.subtract)
            B.tt("dve", rp2[0:64, :], srcq[0:64, :], rp1[0:64, :], ALU.subtract, [srcq, rp1], [rp2])
            if shift == 0.0:
                B.A(dstb[0:64, :], rp2[0:64, :], AF.Sin, [rp2, cst], [dstb], scale=cst[0:64, CI_SGN:CI_SGN + 1])
            else:
                B.A(dstb[0:64, :], rp2[0:64, :], AF.Sin, [rp2], [dstb], scale=TWO_PI)
        p1 = psd()
        zmm(p1, wv, ws, 384, 64)
        p2 = psd()
        zmm(p2, wsw, wsws, 0, 64)
        B.tt("dve", rp1[0:64, :], p1[0:64, 0:T], cosT[0:64, :], ALU.mult, [p1, cosT], [rp1])
        B.tt("dve", rp2[0:64, :], p2[0:64, 0:T], sinT[0:64, :], ALU.mult, [p2, sinT], [rp2])
        B.tt("dve", Kr[0:64, t0:t0 + T], rp1[0:64, :], rp2[0:64, :], ALU.add, [rp1, rp2], [Kc_d[tt]])
        rms(zkv.ap.rearrange("p (a b) -> p a b", a=1), 1, 128, l, "kv_g", None, [zkv], None)
        B.stt("dve", Kc[:, t0:t0 + T], zkv.ap, vcol(l, "kv_g", 0), rt2.ap, ALU.mult, ALU.mult, [zkv, vec[l], rt2],
              [Kc_d[tt]])
        pt = PS[7]
        ptb = pt.ap.bitcast(BF16)
        for i in range(2):
            B.tr(ptb[:, i * 128:(i + 1) * 128], Kc[:, t0 + i * 128:t0 + (i + 1) * 128], ident_bf, [Kc_d[tt], cbf], [pt])
        B.cp("act", Vc[:, (2 * tt) * 128:(2 * tt + 2) * 128], ptb[:, 0:256], [pt], [Kc_d[tt]])
        rms(zq3, 2, 256, l, "q_g", None, [zq], None)
        for c in range(2):
            B.stt("dve", zqn3[:, c, :], zq3[:, c, :], vcol(l, "q_g", c), rt2.ap, ALU.mult, ALU.mult,
                  [zq, vec[l], rt2], [zqn])
        wq = wb["w_uq"][l].rearrange("(k p) n -> p k n", p=128)
        wqv, wqs = wload("w_uq", l, wq, [128, 2, 768])
        wqsw = wb["w_uq_sw"][l].rearrange("(k p) n -> p k n", p=128)
        wqswv, wqsws = wload("w_uq_sw", l, wqsw, [128, 2, 256])
        wkt = wb["w_ukT"][l].rearrange("(h p) n -> p h n", p=128)
        wktv, wkts = wload("w_ukT", l, wkt, [128, 4, 128])
        for h in range(4):
            p = psd()
            for k in range(2):
                B.mm(p[:, 0:T], wqv[:, k, h * 192:h * 192 + 128], zqn3[:, k, :], [wqs, zqn], [p], start=(k == 0),
                     stop=(k == 1))
            q_ = qn[h % 2]
            B.cp("act", q_.ap, p[:, 0:T], [p], [q_])
            p = psd()
            B.mm(p[:, 0:T], wktv[:, h, :], q_.ap, [wkts, q_], [p])
            B.cp("act", Qabs3[:, h, :], p[:, 0:T], [p], [Qabs])
            p1 = psd()
            for k in range(2):
                B.mm(p1[0:64, 0:T], wqv[:, k, h * 192 + 128:h * 192 + 192], zqn3[:, k, :], [wqs, zqn], [p1],
                     start=(k == 0), stop=(k == 1))
            p2 = psd()
            for k in range(2):
                B.mm(p2[0:64, 0:T], wqswv[:, k, h * 64:(h + 1) * 64], zqn3[:, k, :], [wqsws, zqn], [p2],
                     start=(k == 0), stop=(k == 1))
            B.tt("dve", rp1[0:64, :], p1[0:64, 0:T], cosT[0:64, :], ALU.mult, [p1, cosT], [rp1])
            B.tt("dve", rp2[0:64, :], p2[0:64, 0:T], sinT[0:64, :], ALU.mult, [p2, sinT], [rp2])
            B.tt("dve", Qrope3[0:64, h, :], rp1[0:64, :], rp2[0:64, :], ALU.add, [rp1, rp2], [Qrope])
        mark("mla_attn", l, tt)
        wkv = wb["w_ukv"][l]
        wkvv, wkvs = wload("w_ukv", l, wkv, [128, 1024])
        nkb = 2 * (tt + 1)
        Kdeps = [Kc_d[j // 2] for j in range(nkb)]
        for hp in range(2):
            Ob, Sb = PS[4], PS[5]
            O3 = v3(Ob.ap, 2)
            S3 = v3(Sb.ap, 2)
            def m_scores(j):
                jd = j - 2 * tt
                q0 = max(jd, 0) * 128
                ps_ = PS[j % 2 + 2]
                ps3 = v3(ps_.ap, 2)
                for hh in range(2):
                    h = 2 * hp + hh
                    B.mm(ps3[:, hh, q0:T], Kc[:, j * 128:(j + 1) * 128], Qabs3[:, h, q0:T], [Kdeps[j], Qabs], [ps_],
                         start=True, stop=False)
                    B.mm(ps3[:, hh, q0:T], Kr[0:64, j * 128:(j + 1) * 128], Qrope3[0:64, h, q0:T],
                         [Kdeps[j], Qrope], [ps_], start=False, stop=True)

            def m_rest(j):
                jd = j - 2 * tt
                q0 = max(jd, 0) * 128
                ps_ = PS[j % 2 + 2]
                ps3 = v3(ps_.ap, 2)
                PT = PTm[j % 2]
                PT3 = v3(PT.ap, 2)
                B.A(PT3[:, :, q0:T], ps3[:, :, q0:T], AF.Exp, [ps_], [PT], scale=MLA_SCALE)
                if jd >= 0:
                    B.ms("pool", PT3[64:128, :, q0:q0 + 64], 0.0, [PT])
                for hh in range(2):
                    B.mm(O3[:, hh, q0:T], Vc3[:, j, :], PT3[:, hh, q0:T], [Kdeps[j], PT], [Ob],
                         start=(j == 0 and hh == 0), stop=(j == nkb - 1))
                for hh in range(2):
                    B.mm(S3[:, hh, q0:T], ones_bf, PT3[:, hh, q0:T], [cbf, PT], [Sb],
                         start=(j == 0 and hh == 0), stop=(j == nkb - 1))

            m_scores(0)
            for j in range(nkb):
                if j + 1 < nkb:
                    m_scores(j + 1)
                m_rest(j)
            B.rcp(rinv.ap, Sb.ap, [Sb], [rinv])
            B.tt("dve", On.ap, Ob.ap, rinv.ap, ALU.mult, [Ob, rinv], [On])
            On3 = v3(On.ap, 2)
            for hh in range(2):
                h = 2 * hp + hh
                p = psd()
                B.mm(p[:, 0:T], wkvv[:, h * 256 + 128:h * 256 + 256], On3[:, hh, :], [wkvs, On], [p])
                B.cp("act", o_mla[:, h * T:(h + 1) * T], p[:, 0:T], [p], [o_mla])

        if stage <= 1:
            return fin()
        mark("ca", l, tt)
        wv, ws = wcols("w_in", l, 2240, 512, 8)
        for c in range(4):
            p = psd()
            zmm(p, wv, ws, c * 128, 128)
            B.A(Qs3[:, c, :], p[:, 0:T], AF.Copy, [p], [Qs], scale=0.125)
        wv, ws = wcols("w_in", l, 2752, 512, 8)
        sl0 = (2 * tt) % 8
        for c in range(4):
            p = psd()
            zmm(p, wv, ws, c * 128, 128)
            B.cp("act", CK3[:, c, sl0 * 128:sl0 * 128 + T], p[:, 0:T], [p], [CK_d[sl0 // 2]])
        wv, ws = wcols("w_in", l, 3264, 512, 8)
        for i in range(2):
            p = psd()
            for k in range(8):
                B.mm(p.ap, uT3[:, k, i * 128:(i + 1) * 128], wv[:, k, :], [uT, ws], [p], start=(k == 0), stop=(k == 7))
            B.cp("act", CV3[:, sl0 + i, :], p.ap, [p], [CV_d[sl0 + i]])
        def ca_attn():
            for i in range(2):
                qb = 2 * tt + i
                Ob, Sb = PS[4], PS[5]
                O3 = v3(Ob.ap, 4)
                S3 = v3(Sb.ap, 4)
                bl = list(range(max(0, qb - 4), qb + 1))
                units = [(b, e) for b in bl for e in range(2)]

                def c_scores(b, e):
                    r = qb - b
                    slot = b % 8
                    ps_ = PS[2 + e]
                    ps3 = v3(ps_.ap, 4)
                    pb_ = e * 64
                    for ch in range(4):
                        h = ch * 2 + e
                        B.mm(ps3[:, ch, :], CK3[pb_:pb_ + 64, ch, slot * 128:(slot + 1) * 128],
                             Qs3[pb_:pb_ + 64, ch, i * 128:(i + 1) * 128], [CK_d[slot // 2], Qs], [ps_],
                             start=True, stop=False)
                        B.mm(ps3[:, ch, :], Xb3[:, r * 8 + h, :], J_bf, [Xb, cbf], [ps_], start=False, stop=True)

                def c_rest(b, e):
                    slot = b % 8
                    ps_ = PS[2 + e]
                    PT = PTc[e]
                    PT3 = v3(PT.ap, 4)
                    pb_ = e * 64
                    B.A(PT.ap, ps_.ap, AF.Exp, [ps_], [PT])
                    for ch in range(4):
                        h = ch * 2 + e
                        first = (b == bl[0] and ch == 0)
                        B.mm(O3[pb_:pb_ + 64, ch, :], CV3[:, slot, h * 64:(h + 1) * 64], PT3[:, ch, :],
                             [CV_d[slot], PT], [Ob], start=first, stop=True)
                    for ch in range(4):
                        first = (b == bl[0] and ch == 0)
                        B.mm(S3[pb_:pb_ + 64, ch, :], ones_bf[:, 0:64], PT3[:, ch, :], [cbf, PT], [Sb],
                             start=first, stop=True)

                c_scores(*units[0])
                for ui, u_ in enumerate(units):
                    if ui + 1 < len(units):
                        c_scores(*units[ui + 1])
                    c_rest(*u_)
                    yield 1
                B.rcp(rinvc.ap, Sb.ap, [Sb], [rinvc])
                B.tt("dve", v3(o_ca.ap, 4)[:, :, i * 128:(i + 1) * 128], O3, v3(rinvc.ap, 4), ALU.mult, [Ob, rinvc],
                     [o_ca])


        if stage <= 2:
            for _ in ca_attn():
                pass
            return fin()
        mark("rw", l, tt)
        rwkv(l, tt, ca_attn())
        if dbg and l == 0 and tt == 0:
            for n, bsrc in (("aT", aTt), ("bT", bTt), ("kT", kTt), ("rT", rTt), ("y", yb), ("bonus", bonus), ("g", gbuf),
                            ("TM", TM), ("AM", AM), ("Sst", Sst), ("small", small)):
                if n not in dbg_t:
                    dbg_t[n] = dram("dbg_" + n, [128, bsrc.ap.shape[1]], bsrc.ap.dtype, "ExternalOutput")
                B.dma("sp", dbg_t[n], bsrc.ap, R=[bsrc] + ([AM_d[c_][h_] for c_ in range(2) for h_ in range(4)] if n == "AM" else []))
        if stage <= 3 or (stage >= 21 and stage <= 26):
            return fin()

        if dbg and l == 0:
            for n, bsrc in (("o_mla", o_mla), ("o_rw", o_rw), ("o_ca", o_ca)):
                B.dma("sp", dbg_t[n][:, t0:t0 + T].rearrange("(k p) t -> p k t", p=128), v3(bsrc.ap, 4), R=[bsrc])

        mark("merge", l, tt)
        obr = (o_mla, o_rw, o_ca)
        for cg in range(2):
            for n in range(3):
                gv, gs = wcols("w_in", l, 3776 + n * 1024 + cg * 512, 512, 8)
                bap = wb["w_branch"][l, n * 512:(n + 1) * 512, cg * 512:(cg + 1) * 512].rearrange(
                    "(k p) n -> p k n", p=128)
                bv, bs = wload("w_branch", l, bap, [128, 4, 512])
                ob3 = v3(obr[n].ap, 4)
                for cl in range(4):
                    c = cg * 4 + cl
                    pg = psd()
                    zmm(pg, gv, gs, cl * 128, 128)
                    gb = gsb[(c * 3 + n) % 2]
                    B.A(gb.ap, pg[:, 0:T], AF.Sigmoid, [pg], [gb])
                    py = psd()
                    for k in range(4):
                        B.mm(py[:, 0:T], bv[:, k, cl * 128:(cl + 1) * 128], ob3[:, k, :], [bs, obr[n]], [py],
                             start=(k == 0), stop=(k == 3))
                    if n == 0:
                        B.tt("dve", mrg3[:, c, :], py[:, 0:T], gb.ap, ALU.mult, [py, gb], [mrg])
                    else:
                        tf = tmpf[(c * 3 + n) % 2]
                        B.tt("dve", tf.ap, py[:, 0:T], gb.ap, ALU.mult, [py, gb], [tf])
                        if n == 1:
                            B.tt("pool", mrg3[:, c, :], mrg3[:, c, :], tf.ap, ALU.add, [mrg, tf], [mrg])
                        else:
                            B.tt("pool", mrgb3[:, c, :], mrg3[:, c, :], tf.ap, ALU.add, [mrg, tf], [mrgb])
        for cg in range(2):
            wv, ws = wcols("w_out", l, cg * 512, 512, 8)
            for cl in range(4):
                c = cg * 4 + cl
                p = psd()
                for k in range(8):
                    B.mm(p[:, 0:T], wv[:, k, cl * 128:(cl + 1) * 128], mrgb3[:, k, :], [ws, mrgb], [p], start=(k == 0),
                         stop=(k == 7))
                B.cp("act", mo3[:, c, :], p[:, 0:T], [p], [mo])
        rms(mo3, 8, D, l, "post_mix_g", None, [mo], None)
        for k in range(8):
            tf = tmpf[k % 2]
            B.stt("dve", tf.ap, mo3[:, k, :], vcol(l, "post_mix_g", k), rt2.ap, ALU.mult, ALU.mult,
                  [mo, vec[l], rt2], [tf])
            B.tt("pool", hT3[:, k, :], hT3[:, k, :], tf.ap, ALU.add, [hT, tf], [hT])
        mark("ffn", l, tt)
        rms(hT3, 8, D, l, "pre_ff_g", None, [hT], None)
        for k in range(8):
            B.stt("dve", uT3[:, k, :], hT3[:, k, :], vcol(l, "pre_ff_g", k), rt2.ap, ALU.mult, ALU.mult,
                  [hT, vec[l], rt2], [uT])
        for cg in range(8):
            wv, ws = wcols("w_ff1", l, cg * 512, 512, 8)
            for cl in range(4):
                j = cg * 4 + cl
                p = psd()
                zmm(p, wv, ws, cl * 128, 128)
                r_ = rl[j % 2]
                B.A(r_.ap, p[:, 0:T], AF.Relu, [p], [r_])
                B.tt("pool", aT3[:, j, :], r_.ap, r_.ap, ALU.mult, [r_], [aT])
        for cg in range(2):
            accs = [PS[4 + i] for i in range(4)]
            for kg in range(4):
                wap = wb["w_ff2"][l, kg * 1024:(kg + 1) * 1024, cg * 512:(cg + 1) * 512].rearrange(
                    "(k p) n -> p k n", p=128)
                wv, ws = wload("w_ff2", l, wap, [128, 8, 512])
                for cl in range(4):
                    for kk in range(8):
                        B.mm(accs[cl][:, 0:T], wv[:, kk, cl * 128:(cl + 1) * 128], aT3[:, kg * 8 + kk, :], [ws, aT],
                             [accs[cl]], start=(kg == 0 and kk == 0), stop=(kg == 3 and kk == 7))
            for cl in range(4):
                B.cp("act", mo3[:, cg * 4 + cl, :], accs[cl][:, 0:T], [accs[cl]], [mo])
        rms(mo3, 8, D, l, "post_ff_g", None, [mo], None)
        for k in range(8):
            tf = tmpf[k % 2]
            B.stt("dve", tf.ap, mo3[:, k, :], vcol(l, "post_ff_g", k), rt2.ap, ALU.mult, ALU.mult,
                  [mo, vec[l], rt2], [tf])
            B.tt("pool", hT3[:, k, :], hT3[:, k, :], tf.ap, ALU.add, [hT, tf], [hT])
        mark("ple", l, tt)
        B.cp("act", uT.ap, hT.ap, [hT], [uT])
        B.dma("pool", pb3, pT[l, :, t0:t0 + T].rearrange("(k p) t -> p k t", p=128), W=[pb])
        ppv, pps = wload("w_ple_proj", l, wb["w_ple_proj"][l].rearrange("(k p) n -> p k n", p=128), [128, 2, 1024])
        for cg in range(2):
            wv, ws = wcols("w_ple_gate", l, cg * 512, 512, 8)
            for cl in range(4):
                c = cg * 4 + cl
                pg = psd()
                zmm(pg, wv, ws, cl * 128, 128)
                gb = gsb[c % 2]
                B.A(gb.ap, pg[:, 0:T], AF.Sigmoid, [pg], [gb])
                pp = psd()
                for k in range(2):
                    B.mm(pp[:, 0:T], ppv[:, k, c * 128:(c + 1) * 128], pb3[:, k, :], [pps, pb], [pp], start=(k == 0),
                         stop=(k == 1))
                tf = tmpf[c % 2]
                B.tt("dve", tf.ap, pp[:, 0:T], gb.ap, ALU.mult, [pp, gb], [tf])
                B.tt("pool", hT3[:, c, :], hT3[:, c, :], tf.ap, ALU.add, [hT, tf], [hT])
        W_dst = [] if dst_d is None else [dst_d[tt]]
        B.dma("sp", dst[:, t0:t0 + T].rearrange("(k p) t -> p k t", p=128), hT3, R=[hT], W=W_dst)

    def rwkv(l, tt, ca):
        zi = [0]

        def zchunk(wv, ws, c0, cidx, dest):
            p = psd()
            for k in range(8):
                B.mm(p[:, 0:T], wv[:, k, c0:c0 + 128] if c0 is not None else wv[:, k, :], uT3[:, k, :], [ws, uT], [p],
                     start=(k == 0), stop=(k == 7))
            z = zb[zi[0] % 2]
            zi[0] += 1
            B.cp("act", z[:, 1:T + 1], p[:, 0:T], [p], [z])
            B.cp("pool", z[:, 0:1], zlast[:, cidx:cidx + 1], [zlast], [z])
            B.tt("dve", dd.ap, z[:, 0:T], z[:, 1:T + 1], ALU.subtract, [z], [dd])
            B.stt("dve", dest.ap, dd.ap, vcol(l, "mu", cidx), z[:, 1:T + 1], ALU.mult, ALU.add, [dd, vec[l], z], [dest])
            B.cp("pool", zlast[:, cidx:cidx + 1], z[:, T:T + 1], [z], [zlast])

        wv, ws = wcols("w_in", l, 448 + 1536, 256, 8)
        zchunk(wv, ws, 0, 12, zs12)
        zchunk(wv, ws, 128, 13, zs13)
        B.A(txw[0:64, :], zs12[0:64, :], AF.Tanh, [zs12], [txw])
        B.cp("act", xab[64:128, :], zs12[64:128, :], [zs12], [xab])
        B.A(sgb.ap, zs13.ap, AF.Sigmoid, [zs13], [sgb])
        if stage == 21:
            return
        s_wa = lwa
        B.dma("sp", s_wa.ap[0:64, 0:512], wb["rw_w_up"][l], R=[wdep[("rw_w_up", l)]], W=[s_wa])
        B.dma("sp", s_wa.ap[64:128, 0:512], wb["rw_a_up"][l], R=[wdep[("rw_a_up", l)]], W=[s_wa])
        gups = lwg
        gupv = lwg.ap
        B.dma("sp", gupv, wb["rw_g_up"][l], R=[wdep[("rw_g_up", l)]], W=[lwg])
        sm = small

        def stageA(hp):
            qs = hp % 2
            s4 = wsl[wsl_i[0]]
            wsl_i[0] = (wsl_i[0] + 1) % NW
            wv4 = s4.ap[:, 0:3072].rearrange("p (j k n) -> p j k n", j=3, k=8)
            ws4 = s4
            for j_ in range(3):
                c0_ = 448 + j_ * 512 + hp * 128
                B.dma("sp", wv4[:, j_, :, :], wb["w_in"][l, :, c0_:c0_ + 128].rearrange("(k p) n -> p k n", p=128),
                      R=[wdep[("w_in", l)]], W=[s4])
            zchunk(wv4[:, 0, :, :], ws4, None, hp, zr)
            yield 1
            zchunk(wv4[:, 1, :, :], ws4, None, 4 + hp, zk)
            yield 1
            zchunk(wv4[:, 2, :, :], ws4, None, 8 + hp, zv)
            yield 1
            X = tmp
            cols = slice(hp * 128, (hp + 1) * 128)
            p = psd()
            B.mm(p[:, 0:T], s_wa.ap[0:64, cols], txw[0:64, :], [s_wa, txw], [p])
            B.A(X["sgw"].ap, p[:, 0:T], AF.Sigmoid, [p, vec[l]], [X["sgw"]], bias=vcol(l, "w0", hp))
            p = psd()
            B.mm(p[:, 0:T], s_wa.ap[64:128, cols], xab[64:128, :], [s_wa, xab], [p])
            B.A(X["aa"].ap, p[:, 0:T], AF.Sigmoid, [p, vec[l]], [X["aa"]], bias=vcol(l, "a0", hp))
            p = psd()
            B.mm(p[:, 0:T], gupv[:, cols], sgb.ap, [gups, sgb], [p])
            B.cp("act", g3[:, hp, :], p[:, 0:T], [p], [gbuf])
            yield 1
            cs = X["cs"]
            onec = cst[:, CI_ONE:CI_ONE + 128]
            for c in range(2):
                sl_ = slice(c * 128, (c + 1) * 128)
                B.op("dve", lambda e, sl_=sl_: e.tensor_tensor_scan(X["cs"][:, sl_], onec, X["sgw"][:, sl_], 0.0,
                                                                    ALU.mult, ALU.add), [cst, X["sgw"]], [X["cs"]])
            for c in range(2):
                B.ts("dve", X["csc"][:, c * 128:(c + 1) * 128], cs[:, c * 128:(c + 1) * 128],
                     cs[:, c * 128 + 63:c * 128 + 64], ALU.subtract, [cs], [X["csc"]])
            for c in range(2):
                B.cp("pool", sm[:, hp * 2 + c:hp * 2 + c + 1], cs[:, c * 128 + 63:c * 128 + 64], [cs], [sm])
                B.cp("pool", sm[:, 8 + hp * 2 + c:8 + hp * 2 + c + 1], X["csc"][:, c * 128 + 127:c * 128 + 128],
                     [X["csc"]], [sm])
            B.tt("dve", X["csx"].ap, X["csc"].ap, X["sgw"].ap, ALU.subtract, [X["csc"], X["sgw"]], [X["csx"]])
            for c in range(2):
                B.ts("dve", X["dC"][:, c * 128:(c + 1) * 128], X["csc"][:, c * 128:(c + 1) * 128],
                     X["csc"][:, c * 128 + 127:c * 128 + 128], ALU.subtract, [X["csc"]], [X["dC"]])
            B.A(X["E1"].ap, X["csc"].ap, AF.Exp, [X["csc"]], [X["E1"]], scale=-CC)
            B.A(X["E2"].ap, X["csc"].ap, AF.Exp, [X["csc"]], [X["E2"]], scale=CC)
            B.A(X["E3"].ap, X["csx"].ap, AF.Exp, [X["csx"]], [X["E3"]], scale=-CC)
            B.A(X["E4"].ap, X["dC"].ap, AF.Exp, [X["dC"]], [X["E4"]], scale=CC)
            yield 1
            B.ts("dve", X["kk"].ap, zk.ap, vcol(l, "k_k", hp), ALU.mult, [zk, vec[l]], [X["kk"]])
            B.A(kk2.ap, X["kk"].ap, AF.Square, [X["kk"]], [kk2])
            p = psd()
            B.mm(p[:, 0:T], blk_bf, kk2.ap, [cbf, kk2], [p])
            B.ts("dve", X["ssm"].ap, p[:, 0:T], 1e-24, ALU.max, [p], [X["ssm"]])
            B.A(X["rs"].ap, X["ssm"].ap, AF.Sqrt, [X["ssm"]], [X["rs"]])
            B.rcp(X["ssm"].ap, X["rs"].ap, [X["rs"]], [X["ssm"]])
            B.tt("dve", X["kkn"].ap, X["kk"].ap, X["ssm"].ap, ALU.mult, [X["kk"], X["ssm"]], [X["kkn"]])
            yield 1
            B.stt("dve", aT_3[:, hp, :], X["kkn"].ap, -1.0, X["E3"].ap, ALU.mult, ALU.mult, [X["kkn"], X["E3"]], [aTt])
            B.tt("dve", X["tb"].ap, X["kkn"].ap, X["aa"].ap, ALU.mult, [X["kkn"], X["aa"]], [X["tb"]])
            B.tt("dve", bT_3[:, hp, :], X["tb"].ap, X["E2"].ap, ALU.mult, [X["tb"], X["E2"]], [bTt])
            B.tt("pool", bh.ap, X["tb"].ap, X["E4"].ap, ALU.mult, [X["tb"], X["E4"]], [bh])
            B.ts("dve", X["tc"].ap, X["aa"].ap, vcol(l, "k_a", hp), ALU.mult, [X["aa"], vec[l], omka[l]], [X["tc"]],
                 s2=omka[l][:, hp:hp + 1], op1=ALU.add)
            B.tt("dve", X["km"].ap, zk.ap, X["tc"].ap, ALU.mult, [zk, X["tc"]], [X["km"]])
            B.tt("dve", kT_3[:, hp, :], X["km"].ap, X["E2"].ap, ALU.mult, [X["km"], X["E2"]], [kTt])
            B.tt("pool", kh.ap, X["km"].ap, X["E4"].ap, ALU.mult, [X["km"], X["E4"]], [kh])
            yield 1
            B.tt("dve", rT_3[:, hp, :], zr.ap, X["E1"].ap, ALU.mult, [zr, X["E1"]], [rTt])
            B.stt("dve", rkr.ap, zr.ap, vcol(l, "r_k", hp), X["km"].ap, ALU.mult, ALU.mult, [zr, vec[l], X["km"]], [rkr])
            p = psd()
            B.mm(p[:, 0:T], blk_bf, rkr.ap, [cbf, rkr], [p])
            B.tt("dve", bonus3[:, hp, :], p[:, 0:T], zv.ap, ALU.mult, [p, zv], [bonus])
            B.cp("pool", vbf.ap, zv.ap, [zv], [vbf])
            yield 1
            pt = PS[7]
            ptb = pt.ap.bitcast(BF16)
            ptb4 = ptb[:, 0:768].rearrange("p (j c n) -> p j c n", j=3, c=2)
            for j_, sbuf_ in enumerate((vbf, bh, kh)):
                for c in range(2):
                    B.tr(ptb4[:, j_, c, :], sbuf_[:, c * 128:(c + 1) * 128], ident_bf, [sbuf_, cbf], [pt])
            B.cp("act", TM5[:, :, :, hp, :], ptb4, [pt], [TM])
            yield "pre6"
            bA, bB, bC = PS[4], PS[5], PS[6]
            bA4 = v3(bA.ap, 4)
            bB4 = v3(bB.ap, 4)
            bC4 = v3(bC.ap, 4)
            bks = ((PS[4], PS[5], PS[6]), (PS[1], PS[2], PS[3]))
            Q4 = Qd[qs].ap.rearrange("p (c e n) -> p c e n", c=2, e=2)
            QT4 = QTd[qs].ap.rearrange("p (c e n) -> p c e n", c=2, e=2)
            for e in range(2):
                kA, kB, kC = bks[e]
                kA4 = kA.ap.rearrange("p (c t n) -> p c t n", c=2, t=2)
                kB4 = kB.ap.rearrange("p (c t n) -> p c t n", c=2, t=2)
                kC3 = v3(kC.ap[:, 0:256], 2)
                pr = slice(e * 64, e * 64 + 64)
                for c in range(2):
                    cs_ = slice(c * 128, (c + 1) * 128)
                    B.mm(kA4[:, c, 0, :], kT_3[pr, hp, cs_], aT_3[pr, hp, cs_], [kTt, aTt], [kA])
                    B.mm(kA4[:, c, 1, :], bT_3[pr, hp, cs_], aT_3[pr, hp, cs_], [bTt, aTt], [kA])
                    B.mm(kB4[:, c, 0, :], bT_3[pr, hp, cs_], rT_3[pr, hp, cs_], [bTt, rTt], [kB])
                    B.mm(kB4[:, c, 1, :], kT_3[pr, hp, cs_], rT_3[pr, hp, cs_], [kTt, rTt], [kB])
                    B.mm(kC3[:, c, :], aT_3[pr, hp, cs_], bT_3[pr, hp, cs_], [aTt, bTt], [kC])
                amds = [AM_d[0][hp], AM_d[1][hp]]
                B.tt("dve", AM6[:, :, hp, 0, e, :], kA4[:, :, 0, :], m_su.unsqueeze(1).to_broadcast([128, 2, 128]),
                     ALU.mult, [kA, cst], amds)
                B.tt("dve", Q4[:, :, e, :], kA4[:, :, 1, :], m_su.unsqueeze(1).to_broadcast([128, 2, 128]),
                     ALU.mult, [kA, cst], [Qd[qs]])
                for c in range(2):
                    B.tt("dve", AM6[:, c, hp, 1:3, e, :], kB4[:, c, :, :], m_u.unsqueeze(1).to_broadcast([128, 2, 128]),
                         ALU.mult, [kB, cst], [amds[c]])
                B.tt("dve", QT4[:, :, e, :], kC3, m_sl.unsqueeze(1).to_broadcast([128, 2, 128]), ALU.mult, [kC, cst],
                     [QTd[qs]])
            yield 1

        def stageB(hp):
            qs = hp % 2
            bA, bB, bC = PS[4], PS[5], PS[6]
            bA4 = v3(bA.ap, 4)
            bB4 = v3(bB.ap, 4)
            bC4 = v3(bC.ap, 4)
            B.tt("dve", v3(Xd[qs].ap, 4), v3(Qd[qs].ap, 4), ident_f.unsqueeze(1).to_broadcast([128, 4, 128]), ALU.add,
                 [Qd[qs], cst], [Xd[qs]])
            B.cp("pool", Xbbs[qs].ap, Xd[qs].ap, [Xd[qs]], [Xbbs[qs]])
            Qc, QTc, Xc = v3(Qd[qs].ap, 4), v3(QTd[qs].ap, 4), v3(Xd[qs].ap, 4)
            Xb4 = v3(Xbbs[qs].ap, 4)
            for j in range(6):
                for m in range(4):
                    B.mm(bA4[:, m, :], Qc[:, m, :], QTc[:, m, :], [Qd[qs], QTd[qs]], [bA])
                if j < 5:
                    for m in range(4):
                        B.mm(bB4[:, m, :], QTc[:, m, :], Qc[:, m, :], [Qd[qs], QTd[qs]], [bB])
                B.cp("act", QTc, bA4, [bA], [QTd[qs]])
                if j < 5:
                    B.cp("act", Qc, bB4, [bB], [Qd[qs]])
                for m in range(4):
                    B.mm(bC4[:, m, :], QTc[:, m, :], Xb4[:, m, :], [QTd[qs], Xbbs[qs]], [bC])
                if j < 5:
                    B.tt("dve", Xc, Xc, bC4, ALU.add, [Xd[qs], bC], [Xd[qs]])
                    B.cp("pool", Xbbs[qs].ap, Xd[qs].ap, [Xd[qs]], [Xbbs[qs]])
                else:
                    for c in range(2):
                        B.tt("dve", AM6[:, c, hp, 3, :, :], Xc[:, c * 2:c * 2 + 2, :], bC4[:, c * 2:c * 2 + 2, :],
                             ALU.add, [Xd[qs], bC], [AM_d[c][hp]])
                yield 1

        rot["n"] = 2
        a = stageA(0)
        hit = False
        while True:
            ca_alive = next(ca, "END") != "END"
            if not hit:
                v_ = next(a, "END")
                if v_ == "pre6" or v_ == "END":
                    hit = True
            if not ca_alive:
                break
        rot["n"] = 4
        for _ in a:
            pass
        for hp in range(4):
            b_ = stageB(hp)
            a = stageA(hp + 1) if hp < 3 else iter(())
            al_a = al_b = True
            while al_a or al_b:
                if al_a:
                    al_a = next(a, "END") != "END"
                if al_b:
                    al_b = next(b_, "END") != "END"
        mark("rw_chain", l, tt)
        B.A(sm[:, 16:24], sm[:, 0:8], AF.Exp, [sm], [sm], scale=-CC)
        B.A(sm[:, 24:32], sm[:, 8:16], AF.Exp, [sm], [sm], scale=-CC)
        Pm3 = sm[:, 16:24].rearrange("p (h c) -> p h c", c=2)
        PC3 = sm[:, 24:32].rearrange("p (h c) -> p h c", c=2)
        S3_ = v3(Sst.ap, 4)
        Smid3 = v3(Smid.ap, 4)
        Smb3 = v3(Smb.ap, 4)
        allAM = [AM_d[c][h] for c in range(2) for h in range(4)]
        for c in range(2):
            cs_ = slice(c * 128, (c + 1) * 128)
            amc = AM_d[c]
            B.tt("dve", Smid3, S3_, Pm3[:, :, c:c + 1].to_broadcast([128, 4, 64]), ALU.mult, [Sst, sm], [Smid])
            B.cp("dve", Smb.ap, Smid.ap, [Smid], [Smb])
            pWe = (PS[0], PS[1])
            pU, pS = PS[2], PS[3]
            pYe = (PS[4], PS[5])
            pU3 = v3(pU.ap, 8)
            pS3 = v3(pS.ap[:, 0:256], 4)
            W0b3 = v3(W0b.ap, 8)
            Ut3 = v3(Ut.ap, 8)
            W0b4 = W0b.ap.rearrange("p (h e v) -> p h e v", h=4, e=2)
            for e in range(2):
                pr = slice(e * 64, e * 64 + 64)
                pW3 = v3(pWe[e].ap[:, 0:256], 4)
                for hp in range(4):
                    B.mm(pW3[:, hp, :], AM6[:, c, hp, 0, e, :], TM5[:, 0, c, hp, e * 64:e * 64 + 64], [amc[hp], TM],
                         [pWe[e]], start=True, stop=False)
                    B.mm(pW3[:, hp, :], aT_3[pr, hp, cs_], Smb3[pr, hp, :], [aTt, Smb], [pWe[e]], start=False, stop=True)
                B.cp("act", W0b4[:, :, e, :], pW3, [pWe[e]], [W0b])
            for hp in range(4):
                for e in range(2):
                    h = hp * 2 + e
                    B.mm(pU3[:, h, :], AM6[:, c, hp, 3, e, :], W0b3[:, h, :], [amc[hp], W0b], [pU])
            B.cp("act", Ut.ap, pU.ap, [pU], [Ut])
            for e in range(2):
                pr = slice(e * 64, e * 64 + 64)
                pY3 = v3(pYe[e].ap, 4)
                for hp in range(4):
                    h = hp * 2 + e
                    B.mm(pY3[pr, hp, :], Smb3[pr, hp, :], rT_3[pr, hp, cs_], [Smb, rTt], [pYe[e]], start=True, stop=False)
                    B.mm(pY3[pr, hp, :], Ut3[:, h, :], AM6[:, c, hp, 1, e, :], [Ut, amc[hp]], [pYe[e]], start=False,
                         stop=False)
                    B.mm(pY3[pr, hp, :], TM5[:, 0, c, hp, e * 64:e * 64 + 64], AM6[:, c, hp, 2, e, :], [TM, amc[hp]],
                         [pYe[e]], start=False, stop=True)
                B.cp("act", y3[pr, :, cs_], pY3[pr, :, :], [pYe[e]], [yb])
            for hp in range(4):
                for e in range(2):
                    h = hp * 2 + e
                    pr = slice(e * 64, e * 64 + 64)
                    B.mm(pS3[pr, hp, :], TM5[:, 1, c, hp, e * 64:e * 64 + 64], Ut3[:, h, :], [TM, Ut], [pS], start=True,
                         stop=False)
                    B.mm(pS3[pr, hp, :], TM5[:, 2, c, hp, e * 64:e * 64 + 64], TM5[:, 0, c, hp, e * 64:e * 64 + 64],
                         [TM], [pS], start=False, stop=True)
            B.tt("dve", S3_, Smid3, PC3[:, :, c:c + 1].to_broadcast([128, 4, 64]), ALU.mult, [Smid, sm], [Sst])
            B.tt("dve", S3_, S3_, pS3, ALU.add, [Sst, pS], [Sst])
        if stage == 26:
            return
        mark("rw_norm", l, tt)
        for hp in range(4):
            B.cp("pool", ybf.ap, y3[:, hp, :], [yb], [ybf])
            p = psd()
            B.mm(p[:, 0:T], blk_bf, ybf.ap, [cbf, ybf], [p])
            B.stt("dve", yc.ap, p[:, 0:T], -1.0 / 64, y3[:, hp, :], ALU.mult, ALU.add, [p, yb], [yc])
            B.A(ysq.ap, yc.ap, AF.Square, [yc], [ysq])
            p = psd()
            B.mm(p[:, 0:T], blk_bf, ysq.ap, [cbf, ysq], [p])
            B.A(sd.ap, p[:, 0:T], AF.Sqrt, [p, eps_t], [sd], scale=1.0 / 64, bias=eps_ap(LN_EPS))
            B.rcp(t1.ap, sd.ap, [sd], [t1])
            B.tt("dve", t2.ap, yc.ap, t1.ap, ALU.mult, [yc, t1], [t2])
            B.ts("dve", t1.ap, t2.ap, vcol(l, "ln_w", hp), ALU.mult, [t2, vec[l]], [t1], s2=vcol(l, "ln_b", hp),
                 op1=ALU.add)
            B.tt("dve", t2.ap, t1.ap, bonus3[:, hp, :], ALU.add, [t1, bonus], [t2])
            B.tt("dve", o_rw[:, hp * T:(hp + 1) * T], t2.ap, g3[:, hp, :], ALU.mult, [t2, gbuf], [o_rw])

    for l in range(nl):
        layer(l)
    B.emit(block)
    es.__exit__(None, None, None)
    return nc


def _consts():
    c = np.zeros((128, NCONST), np.float32)
    i = np.arange(128)
    c[:, CI_ID:CI_ID + 128] = np.eye(128)
    c[:, CI_J:CI_J + 128] = np.eye(128)[::-1]
    c[:, CI_ONE:CI_ONE + 128] = 1.0
    blk = np.zeros((128, 128), np.float32)
    blk[:64, :64] = 1
    blk[64:, 64:] = 1
    c[:, CI_BLK:CI_BLK + 128] = blk
    c[:, CI_SU:CI_SU + 128] = (i[None, :] > i[:, None])
    c[:, CI_U:CI_U + 128] = (i[None, :] >= i[:, None])
    c[:, CI_SL:CI_SL + 128] = (i[None, :] < i[:, None])
    half = 32
    invf = (1.0 / (np.float32(10000.0) ** (np.arange(half, dtype=np.float32) / np.float32(half)))).astype(np.float32)
    c[:64, CI_INVF] = np.concatenate([invf, invf])
    c[:64, CI_SGN] = np.concatenate([-np.ones(32), np.ones(32)]) * TWO_PI
    return c


def prep_shared(inp, nl=NL):
    f = lambda a: np.ascontiguousarray(np.asarray(a, dtype=np.float32))
    sh = {}
    w_in = f(inp["w_in"])[:nl]
    sh["w_in"] = w_in
    kr = w_in[:, :, 384:448]
    sh["w_in_sw"] = np.ascontiguousarray(np.concatenate([kr[:, :, 32:], kr[:, :, :32]], axis=-1))
    wuq = f(inp["mla_w_uq"])[:nl]
    sh["w_uq"] = wuq
    r4 = wuq.reshape(nl, 256, 4, 192)[:, :, :, 128:]
    sh["w_uq_sw"] = np.ascontiguousarray(np.concatenate([r4[..., 32:], r4[..., :32]], axis=-1).reshape(nl, 256, 256))
    wukv = f(inp["mla_w_ukv"])[:nl]
    sh["w_ukv"] = wukv
    nope = wukv.reshape(nl, 128, 4, 256)[:, :, :, :128]
    sh["w_ukT"] = np.ascontiguousarray(nope.transpose(0, 2, 3, 1).reshape(nl, 512, 128))
    sh["rw_w_up"] = f(inp["rw_w_up"])[:nl]
    sh["rw_a_up"] = f(inp["rw_a_up"])[:nl]
    sh["rw_g_up"] = f(inp["rw_g_up"])[:nl]
    for n in ("w_branch", "w_out", "w_ff1", "w_ff2", "w_ple_gate", "w_ple_proj"):
        sh[n] = f(inp[n])[:nl]
    vecs = np.zeros((nl, 128, NV), np.float32)

    def put(name, arr):
        a = f(arr)[:nl].reshape(nl, -1, 128)
        vecs[:, :, VC[name]:VC[name] + a.shape[1]] = a.transpose(0, 2, 1)

    put("pre_mix_g", inp["pre_mix_g"])
    put("q_g", inp["mla_q_norm_g"])
    put("kv_g", inp["mla_kv_norm_g"])
    put("mu", inp["rw_mu"])
    put("w0", inp["rw_w0"])
    put("a0", inp["rw_a0"])
    put("k_k", inp["rw_k_k"])
    put("k_a", inp["rw_k_a"])
    put("r_k", np.asarray(inp["rw_r_k"]).reshape(-1, 512))
    put("ln_w", inp["rw_ln_w"])
    put("ln_b", inp["rw_ln_b"])
    put("post_mix_g", inp["post_mix_g"])
    put("pre_ff_g", inp["pre_ff_g"])
    put("post_ff_g", inp["post_ff_g"])
    sh["vecs"] = vecs
    sh["consts"] = _consts()
    rel = f(inp["ca_rel_bias"])[:nl]
    sh["relr"] = np.ascontiguousarray(rel[:, ::-1, :].transpose(0, 2, 1))
    return sh


def prep_core(inp, b, S, nl=NL):
    d = {}
    d["xT"] = np.ascontiguousarray(np.asarray(inp["x"], dtype=np.float32)[b, :S].T)
    d["pT"] = np.ascontiguousarray(np.asarray(inp["p"], dtype=np.float32)[:nl, b, :S].transpose(0, 2, 1))
    d["pos"] = np.ascontiguousarray(np.asarray(inp["positions"]).astype(np.int32)[b:b + 1, :S])
    return d


_NC_CACHE = {}
PHASES = []


def kernel(**inputs):
    key = (SEQ, NL)
    if key not in _NC_CACHE:
        _NC_CACHE[key] = build(SEQ, NL)
    nc = _NC_CACHE[key]
    sh = prep_shared(inputs)
    in_maps = []
    for b in range(NB):
        m = dict(sh)
        m.update(prep_core(inputs, b, SEQ))
        in_maps.append(m)
    res = run_bass_kernel_spmd(nc, in_maps, core_ids=list(range(NB)))
    out = np.stack([np.asarray(r["outT"]).T for r in res.results], axis=0)
    return np.ascontiguousarray(out.astype(np.float32))
```

```python
import contextlib
import numpy as np
import ml_dtypes
import concourse.bass as bass
import concourse.mybir as mybir
from concourse.bass_utils import run_bass_kernel_spmd

F32 = mybir.dt.float32
BF16 = mybir.dt.bfloat16
I32 = mybir.dt.int32
AF = mybir.ActivationFunctionType
ALU = mybir.AluOpType

D = 1024
DFF = 4096
DPLE = 256
INC = 6848
NL = 2
SEQ = 4096
NB = 8
T = 256
EPS = 1e-6
LN_EPS = 64e-5
CC = float(np.exp(-0.5))
MLA_SCALE = float(192 ** -0.5)
TWO_PI = 6.2831845
NEG = -30000.0

VC = {}
_o = 0
for _n, _k in [("pre_mix_g", 8), ("q_g", 2), ("kv_g", 1), ("mu", 14), ("w0", 4), ("a0", 4), ("k_k", 4),
               ("k_a", 4), ("r_k", 4), ("ln_w", 4), ("ln_b", 4), ("post_mix_g", 8), ("pre_ff_g", 8),
               ("post_ff_g", 8)]:
    VC[_n] = _o
    _o += _k
NV = _o
CI_ID, CI_J, CI_ONE, CI_BLK, CI_SU, CI_U, CI_SL = [i * 128 for i in range(7)]
CI_INVF = 7 * 128
CI_SGN = CI_INVF + 1
NCONST = CI_SGN + 1

WSPEC = {
    "w_in": (1024, INC), "w_in_sw": (1024, 64), "w_uq": (256, 768), "w_uq_sw": (256, 256),
    "w_ukT": (512, 128), "w_ukv": (128, 1024), "rw_w_up": (64, 512), "rw_a_up": (64, 512),
    "rw_g_up": (128, 512), "w_branch": (1536, 1024), "w_out": (1024, 1024), "w_ff1": (1024, 4096),
    "w_ff2": (4096, 1024), "w_ple_gate": (1024, 1024), "w_ple_proj": (256, 1024),
}
WORDER = ["w_in", "w_in_sw", "w_uq", "w_uq_sw", "w_ukT", "w_ukv", "rw_w_up", "rw_a_up", "rw_g_up",
          "w_branch", "w_out", "w_ff1", "w_ff2", "w_ple_gate", "w_ple_proj"]


class Dep:
    __slots__ = ("w", "r", "al")

    def __init__(self):
        self.w = None
        self.r = {}
        self.al = []


class Buf:
    def __init__(self, ap, d=None):
        self.ap = ap
        self.d = d if d is not None else Dep()

    def __getitem__(self, k):
        return self.ap[k]


def v3(ap, a):
    return ap.rearrange("p (a b) -> p a b", a=a)


ENGS = ("pe", "act", "dve", "pool", "sp")
NDS = 8


class Builder:
    def __init__(self, nc, es):
        self.nc = nc
        self.ops = {e: [] for e in ENGS}
        self.cnt = {e: 0 for e in ENGS}
        self.waited = {e: {} for e in ENGS}
        self.sems = []
        self.semid = {}
        for e in ENGS:
            self.semid[e] = len(self.sems)
            self.sems.append(es.enter_context(nc.semaphore("s_" + e)))
        self.dsem = {}
        self.dcnt = {}
        for q in ("sp", "pool", "act"):
            self.dsem[q] = []
            self.dcnt[q] = 0
            for i in range(NDS):
                self.dsem[q].append(len(self.sems))
                self.sems.append(es.enter_context(nc.semaphore("d_%s%d" % (q, i))))

    def _waits(self, eng, reads, writes):
        need = {}

        def add(sid, val):
            if need.get(sid, 0) < val:
                need[sid] = val

        for d in reads:
            if d.w is not None:
                add(*d.w)
        for d in writes:
            if d.w is not None:
                add(*d.w)
            for sid, val in d.r.items():
                add(sid, val)
            for a in d.al:
                if a.w is not None:
                    add(*a.w)
                for sid, val in a.r.items():
                    add(sid, val)
        out = []
        wd = self.waited[eng]
        pe_sid = self.semid["pe"]
        for sid, val in need.items():
            if eng == "pe" and sid == pe_sid:
                continue
            if wd.get(sid, 0) >= val:
                continue
            wd[sid] = val
            out.append((sid, val))
        return out

    def _mark(self, reads, writes, sid, val):
        for d in reads:
            if d.r.get(sid, 0) < val:
                d.r[sid] = val
        for d in writes:
            d.w = (sid, val)
            d.r = {}

    def op(self, eng, fn, R=(), W=()):
        R = [x.d if isinstance(x, Buf) else x for x in R]
        W = [x.d if isinstance(x, Buf) else x for x in W]
        ws = self._waits(eng, R, W)
        self.cnt[eng] += 1
        sid = self.semid[eng]
        self.ops[eng].append((ws, fn, sid, 1))
        self._mark(R, W, sid, self.cnt[eng])

    def dma(self, q, out, in_, R=(), W=(), **kw):
        R = [x.d if isinstance(x, Buf) else x for x in R]
        W = [x.d if isinstance(x, Buf) else x for x in W]
        ws = self._waits(q, R, W)
        i = self.dcnt[q]
        self.dcnt[q] += 1
        sid = self.dsem[q][i % NDS]
        val = 16 * (i // NDS + 1)
        if val > 16 and self.waited[q].get(sid, 0) < val - 16:
            self.waited[q][sid] = val - 16
            ws.append((sid, val - 16))
        self.ops[q].append((ws, lambda e, o=out, i_=in_, k=kw: e.dma_start(out=o, in_=i_, **k), sid, 16))
        self._mark(R, W, sid, val)

    def mm(self, out, lhsT, rhs, R, W, start=True, stop=True):
        self.pes = getattr(self, "pes", 0) + (2 if lhsT.dtype == F32 else 1)
        self.op("pe", lambda e: e.matmul(out, lhsT, rhs, start=start, stop=stop, skip_group_check=True), R, W)

    def tr(self, out, in_, ident, R, W):
        self.pes = getattr(self, "pes", 0) + 1
        self.op("pe", lambda e: e.transpose(out, in_, ident), R, W)

    def A(self, out, in_, func, R, W, scale=None, bias=None):
        kw = {}
        if scale is not None:
            kw["scale"] = scale
        if bias is not None:
            kw["bias"] = bias
        self.op("act", lambda e: e.activation(out, in_, func, **kw), R, W)

    def tt(self, eng, out, a, b, op, R, W):
        self.op(eng, lambda e: e.tensor_tensor(out, a, b, op), R, W)

    def ts(self, eng, out, a, s1, op0, R, W, s2=None, op1=None):
        if op1 is None:
            self.op(eng, lambda e: e.tensor_scalar(out, a, s1, None, op0), R, W)
        else:
            self.op(eng, lambda e: e.tensor_scalar(out, a, s1, s2, op0, op1), R, W)

    def stt(self, eng, out, a, s, b, op0, op1, R, W):
        self.op(eng, lambda e: e.scalar_tensor_tensor(out, a, s, b, op0, op1), R, W)

    def cp(self, eng, out, in_, R, W):
        if eng == "act":
            self.op("act", lambda e: e.activation(out, in_, AF.Copy), R, W)
        else:
            self.op(eng, lambda e: e.tensor_copy(out, in_), R, W)

    def rcp(self, out, in_, R, W):
        self.op("dve", lambda e: e.reciprocal(out, in_), R, W)

    def ms(self, eng, ap, val, W):
        self.op(eng, lambda e: e.memset(ap, val), (), W)

    def emit(self, block):
        sems = self.sems
        B = self

        def run(e, name):
            for ws, fn, sid, inc in B.ops[name]:
                for s_, v_ in ws:
                    e.wait_ge(sems[s_], v_)
                fn(e).then_inc(sems[sid], inc)

        fin = []
        for en in ENGS:
            if self.cnt[en] > 0:
                fin.append((self.semid[en], self.cnt[en]))
        for q in ("sp", "pool", "act"):
            n = self.dcnt[q]
            for k in range(NDS):
                cntk = (n - k + NDS - 1) // NDS if n > k else 0
                if cntk > 0:
                    fin.append((self.dsem[q][k], 16 * cntk))

        @block.tensor
        def _(e):
            run(e, "pe")

        @block.scalar
        def _(e):
            run(e, "act")

        @block.vector
        def _(e):
            run(e, "dve")

        @block.gpsimd
        def _(e):
            run(e, "pool")

        @block.sync
        def _(e):
            run(e, "sp")
            for s_, v_ in fin:
                e.wait_ge(sems[s_], v_)


def build(S=SEQ, nl=NL, dbg=False, stage=99):
    NT = S // T
    NBLK = S // 128
    nc = bass.Bass("TRN2", target_bir_lowering=False)
    es = contextlib.ExitStack()
    es.__enter__()
    B = Builder(nc, es)

    def dram(name, shape, dt, kind):
        return nc.dram_tensor(name, list(shape), dt, kind=kind).ap()

    xT = dram("xT", [D, S], F32, "ExternalInput")
    pT = dram("pT", [nl, DPLE, S], F32, "ExternalInput")
    pos = dram("pos", [1, S], I32, "ExternalInput")
    vecs = dram("vecs", [nl, 128, NV], F32, "ExternalInput")
    consts = dram("consts", [128, NCONST], F32, "ExternalInput")
    relr = dram("relr", [nl, 8, 320], F32, "ExternalInput")
    wf = {}
    wb = {}
    wdep = {}
    for n in WORDER:
        r, c = WSPEC[n]
        wf[n] = dram(n, [nl, r, c], F32, "ExternalInput")
        wb[n] = dram(n + "_b", [nl, r, c], BF16, "Internal")
        for l in range(nl):
            wdep[(n, l)] = Dep()
    outT = dram("outT", [D, S], F32, "ExternalOutput")
    hscr = [dram("hscr%d" % i, [D, S], F32, "Internal") for i in range(max(nl - 1, 1))]
    hscr_d = [[Dep() for _ in range(NT)] for _ in range(max(nl - 1, 1))]
    ext = dram("ext", [nl, 8, 768], F32, "Internal")
    ext_d = [Dep() for _ in range(nl)]
    dbg_t = {}
    if dbg:
        for n in ("o_mla", "o_rw", "o_ca"):
            dbg_t[n] = dram("dbg_" + n, [512, S], BF16, "ExternalOutput")

    def sb(name, shape, dt):
        return Buf(es.enter_context(nc.sbuf_tensor(name, list(shape), dt))[:])

    cst = sb("cst", [128, NCONST], F32)
    vec = [sb("vec%d" % l, [128, NV], F32) for l in range(nl)]
    omka = [sb("omka%d" % l, [128, 4], F32) for l in range(nl)]
    cbf = sb("cbf", [128, 4 * 128], BF16)
    ident_bf = cbf[:, 0:128]
    J_bf = cbf[:, 128:256]
    ones_bf = cbf[:, 256:384]
    blk_bf = cbf[:, 384:512]
    ident_f = cst[:, CI_ID:CI_ID + 128]
    m_su = cst[:, CI_SU:CI_SU + 128]
    m_u = cst[:, CI_U:CI_U + 128]
    m_sl = cst[:, CI_SL:CI_SL + 128]

    uT = sb("uT", [128, 8 * T], BF16)
    uT3 = v3(uT.ap, 8)
    NW = 3
    wsl = [sb("wsl%d" % i, [128, 4096], BF16) for i in range(NW)]
    wsl_i = [0]
    Kc = sb("Kc", [128, S], BF16)
    Kr = sb("Kr", [64, S], BF16)
    Vc = sb("Vc", [128, S], BF16)
    Vc3 = v3(Vc.ap, NBLK)
    Kc_d = [Dep() for _ in range(NT)]
    CK = sb("CK", [128, 4 * 1024], BF16)
    CK3 = v3(CK.ap, 4)
    CV = sb("CV", [128, 8 * 512], BF16)
    CV3 = v3(CV.ap, 8)
    CK_d = [Dep() for _ in range(4)]
    CV_d = [Dep() for _ in range(8)]
    Xb = sb("Xb", [128, 40 * 128], BF16)
    Xb3 = v3(Xb.ap, 40)
    o_mla = sb("o_mla", [128, 4 * T], BF16)
    o_rw = sb("o_rw", [128, 4 * T], BF16)
    o_ca = sb("o_ca", [128, 4 * T], BF16)
    Sst = sb("Sst", [128, 256], F32)
    zlast = sb("zlast", [128, 16], F32)
    small = sb("small", [128, 64], F32)

    AW = 28900
    arena = es.enter_context(nc.sbuf_tensor("arena", [128, AW], F32))[:]
    abufs = []
    aoff = {}

    def al(phase, name, n, dt):
        words = (n + 1) // 2 if dt == BF16 else n
        o = aoff.get(phase, 0)
        aoff[phase] = o + words
        assert o + words <= AW, (phase, name, o + words)
        ap = arena[:, o:o + words]
        if dt == BF16:
            ap = ap.bitcast(BF16)[:, 0:n]
        b = Buf(ap)
        for (ph2, o2, e2, b2) in abufs:
            if ph2 != phase and o2 < o + words and o < e2:
                b.d.al.append(b2.d)
                b2.d.al.append(b.d)
        abufs.append((phase, o, o + words, b))
        return b

    def al_all(name, n, dt):
        words = (n + 1) // 2 if dt == BF16 else n
        o = max([aoff.get(p, 0) for p in ("mla", "ca", "rw", "ffn")])
        for p in ("mla", "ca", "rw", "ffn"):
            assert aoff.get(p, 0) <= o
            aoff[p] = o + words
        ap = arena[:, o:o + words]
        if dt == BF16:
            ap = ap.bitcast(BF16)[:, 0:n]
        return Buf(ap)

    sqb = al_all("sqb", 8 * T, BF16)
    sqb3 = v3(sqb.ap, 8)
    rt = al_all("rt", T, F32)
    rt2 = al_all("rt2", T, F32)

    PS = [Buf(es.enter_context(nc.psum_tensor("ps%d" % i, [128, 512], F32))[:]) for i in range(8)]
    rot = {"d": 0, "n": 4}

    def psd():
        i = rot["d"] % rot["n"]
        rot["d"] = (i + 1) % rot["n"]
        return PS[i]

    block = es.enter_context(nc.Block())

    B.dma("sp", cst.ap, consts, W=[cst])
    for l in range(nl):
        B.dma("sp", vec[l].ap, vecs[l], W=[vec[l]])
    for l in range(nl):
        for n in WORDER:
            r, c = WSPEC[n]
            npc = (c + 2047) // 2048
            pc = c // npc
            assert pc * npc == c
            for i in range(npc):
                B.dma("pool", wb[n][l, :, i * pc:(i + 1) * pc], wf[n][l, :, i * pc:(i + 1) * pc], W=[wdep[(n, l)]])
    B.cp("dve", cbf[:, 0:512], cst[:, 0:512], [cst], [cbf])
    for l in range(nl):
        ka = vec[l][:, VC["k_a"]:VC["k_a"] + 4]
        B.ts("dve", omka[l].ap, ka, -1.0, ALU.mult, [vec[l]], [omka[l]], s2=1.0, op1=ALU.add)

    def vcol(l, name, i):
        c = VC[name] + i
        return vec[l][:, c:c + 1]

    def wload(name, l, dram_ap, shape, prt=None):
        s = wsl[wsl_i[0]]
        wsl_i[0] = (wsl_i[0] + 1) % NW
        n = int(np.prod(shape[1:]))
        p0, p1 = prt if prt is not None else (0, shape[0])
        view = s.ap[p0:p1, 0:n]
        if len(shape) == 3:
            view = view.rearrange("p (a b) -> p a b", a=shape[1])
        elif len(shape) == 4:
            view = view.rearrange("p (a b c) -> p a b c", a=shape[1], b=shape[2])
        B.dma("sp", view, dram_ap, R=[wdep[(name, l)]], W=[s])
        return view, s

    def wcols(name, l, c0, n, kc):
        ap = wb[name][l, 0:kc * 128, c0:c0 + n].rearrange("(k p) n -> p k n", p=128)
        return wload(name, l, ap, [128, kc, n])

    def rms(src3, nk, ktot, l, gname, out_fn, R, Wd, eps=EPS, ps=None):
        B.A(sqb3[:, 0:nk, :], src3, AF.Square, R, [sqb])
        p = psd()
        for k in range(nk):
            B.mm(p[:, 0:T], ones_bf, sqb3[:, k, :], [sqb, cbf], [p], start=(k == 0), stop=(k == nk - 1))
        B.A(rt.ap, p[:, 0:T], AF.Sqrt, [p], [rt], scale=1.0 / ktot, bias=eps_ap(eps))
        B.rcp(rt2.ap, rt.ap, [rt], [rt2])

    eps_t = sb("eps_t", [128, 4], F32)
    B.ms("dve", eps_t[:, 0:1], EPS, [eps_t])
    B.ms("dve", eps_t[:, 1:2], LN_EPS, [eps_t])
    B.ms("dve", eps_t[:, 2:3], 0.0, [eps_t])
    B.ms("dve", eps_t[:, 3:4], 0.25, [eps_t])

    def eps_ap(e):
        return eps_t[:, 0:1] if e == EPS else eps_t[:, 1:2]

    zq = al("mla", "zq", 2 * T, F32)
    zq3 = v3(zq.ap, 2)
    zqn = al("mla", "zqn", 2 * T, BF16)
    zqn3 = v3(zqn.ap, 2)
    qn = [al("mla", "qn%d" % i, T, BF16) for i in range(2)]
    Qabs = al("mla", "Qabs", 4 * T, BF16)
    Qabs3 = v3(Qabs.ap, 4)
    Qrope = al("mla", "Qrope", 4 * T, BF16)
    Qrope3 = v3(Qrope.ap, 4)
    zkv = al("mla", "zkv", T, F32)
    cosT = al("mla", "cosT", T, F32)
    sinT = al("mla", "sinT", T, F32)
    posi = al("mla", "posi", T, F32)
    posf = al("mla", "posf", T, F32)
    tq = al("mla", "tq", T, F32)
    tq2 = al("mla", "tq2", T, F32)
    rp1 = al("mla", "rp1", T, F32)
    rp2 = al("mla", "rp2", T, F32)
    PTm = [al("mla", "PTm%d" % i, 2 * T, BF16) for i in range(2)]
    rinv = al("mla", "rinv", 2 * T, F32)
    On = al("mla", "On", 2 * T, BF16)
    Qs = al("rw", "Qs", 4 * T, BF16)
    Qs3 = v3(Qs.ap, 4)
    PTc = [al("rw", "PTc%d" % i, 512, BF16) for i in range(2)]
    rinvc = al("rw", "rinvc", 512, F32)
    hT = al("ffn", "hT", 8 * T, F32)
    hT3 = v3(hT.ap, 8)
    mo = al("ffn", "mo", 8 * T, F32)
    mo3 = v3(mo.ap, 8)
    aT = al("ffn", "aT", 32 * T, BF16)
    aT3 = v3(aT.ap, 32)
    rl = [al("ffn", "rl%d" % i, T, F32) for i in range(2)]
    mrg = al("ffn", "mrg", 8 * T, F32)
    mrg3 = v3(mrg.ap, 8)
    mrgb = al("ffn", "mrgb", 8 * T, BF16)
    mrgb3 = v3(mrgb.ap, 8)
    gsb = [al("ffn", "gsb%d" % i, T, F32) for i in range(2)]
    tmpf = [al("ffn", "tmpf%d" % i, T, F32) for i in range(2)]
    pb = al("ffn", "pb", 2 * T, BF16)
    pb3 = v3(pb.ap, 2)
    zb = [al("rw", "zb%d" % i, T + 2, F32) for i in range(2)]
    dd = al("rw", "dd", T, F32)
    zrs = [al("rw", "zr%d" % i, T, F32) for i in range(2)]
    zks = [al("rw", "zk%d" % i, T, F32) for i in range(2)]
    zs12 = zrs[0]
    zs13 = zks[0]
    zvs = [al("rw", "zv%d" % i, T, F32) for i in range(2)]
    txw = al("rw", "txw", T, BF16)
    xab = al("rw", "xab", T, BF16)
    sgb = al("rw", "sgb", T, BF16)
    tmps = []
    for i_ in range(2):
        tmp = {n: al("rw", n + str(i_), T, F32) for n in ("sgw", "cs", "csc", "dC", "E1", "aa", "kk", "ssm", "rs", "tb", "tc", "km")}
        tmp["csx"] = tmp["cs"]
        tmp["E3"] = tmp["cs"]
        tmp["E2"] = tmp["csc"]
        tmp["E4"] = tmp["dC"]
        tmp["kkn"] = tmp["kk"]
        tmps.append(tmp)
    kk2s = [al("rw", "kk2%d" % i, T, BF16) for i in range(2)]
    rkrs = [al("rw", "rkr%d" % i, T, BF16) for i in range(2)]
    vbfs = [al("rw", "vbf%d" % i, T, BF16) for i in range(2)]
    bhs = [al("rw", "bh%d" % i, T, BF16) for i in range(2)]
    khs = [al("rw", "kh%d" % i, T, BF16) for i in range(2)]
    aTt = al("rw", "aTt", 4 * T, BF16)
    bTt = al("rw", "bTt", 4 * T, BF16)
    kTt = al("rw", "kTt", 4 * T, BF16)
    rTt = al("rw", "rTt", 4 * T, BF16)
    aT_3, bT_3, kT_3, rT_3 = (v3(x.ap, 4) for x in (aTt, bTt, kTt, rTt))
    TM = al("rw", "TM", 3 * 2 * 4 * 128, BF16)
    TM5 = TM.ap.rearrange("p (j c h n) -> p j c h n", j=3, c=2, h=4)
    AM = al("rw", "AM", 2 * 4 * 4 * 2 * 128, BF16)
    AM6 = AM.ap.rearrange("p (c h t e n) -> p c h t e n", c=2, h=4, t=4, e=2)
    AM_d = [[Dep() for _ in range(4)] for _ in range(2)]
    for cc_ in range(2):
        for hh_ in range(4):
            AM_d[cc_][hh_].al = AM.d.al
    Qd = [al("rw", "Qd%d" % i, 4 * 128, BF16) for i in range(2)]
    QTd = [al("rw", "QTd%d" % i, 4 * 128, BF16) for i in range(2)]
    Xd = [al("rw", "Xd%d" % i, 4 * 128, F32) for i in range(2)]
    Xbbs = [al("rw", "Xbb%d" % i, 4 * 128, BF16) for i in range(2)]
    yb = al("rw", "y", 4 * T, F32)
    y3 = v3(yb.ap, 4)
    bonus = al("rw", "bonus", 4 * T, F32)
    bonus3 = v3(bonus.ap, 4)
    gbuf = al("rw", "g", 4 * T, BF16)
    g3 = v3(gbuf.ap, 4)
    Smid = al("rw", "Smid", 256, F32)
    Smb = al("rw", "Smb", 256, BF16)
    W0b = al("rw", "W0b", 512, BF16)
    Ut = al("rw", "Ut", 512, BF16)
    lwa = al("rw", "lwa", 512, BF16)
    lwg = al("rw", "lwg", 512, BF16)
    ybf = al("rw", "ybf", T, BF16)
    yc = al("rw", "yc", T, F32)
    ysq = al("rw", "ysq", T, BF16)
    sd = al("rw", "sd", T, F32)
    t1 = al("rw", "t1", T, F32)
    t2 = al("rw", "t2", T, F32)

    def layer(l):
        src = xT if l == 0 else hscr[l - 1]
        dst = outT if l == nl - 1 else hscr[l]
        src_d = None if l == 0 else hscr_d[l - 1]
        dst_d = None if l == nl - 1 else hscr_d[l]
        B.ms("dve", Sst.ap, 0.0, [Sst])
        B.ms("dve", zlast.ap, 0.0, [zlast])
        etap = mo.ap[0:8, 0:768]
        rl_ap = mo.ap[0:8, 768:768 + 320]
        B.dma("sp", rl_ap, relr[l], W=[mo])
        B.cp("dve", etap[:, 383:703], rl_ap, [mo], [mo])
        B.cp("dve", etap[:, 0:383], rl_ap[:, 0:1].to_broadcast([8, 383]), [mo], [mo])
        B.cp("dve", etap[:, 703:768], rl_ap[:, 319:320].to_broadcast([8, 65]), [mo], [mo])
        B.dma("sp", ext[l], etap, R=[mo], W=[ext_d[l]])
        for r in range(5):
            for h in range(8):
                off = 639 - 128 * r - 127
                srcap = bass.AP(tensor=ext.tensor, offset=(l * 8 + h) * 768 + off, ap=[[1, 128], [1, 128]])
                B.dma("pool", Xb3[:, r * 8 + h, :], srcap, R=[ext_d[l]], W=[Xb])
        for h in range(8):
            B.ms("pool", Xb3[64:128, 0 * 8 + h, 64:128], NEG, [Xb])
            B.ms("pool", Xb3[0:64, 4 * 8 + h, 0:64], NEG, [Xb])

        for tt in range(NT):
            tile(l, tt, src, dst, src_d, dst_d)

    def mark(name, l, tt):
        PHASES.append((name, l, tt, getattr(B, "pes", 0)))

    def tile(l, tt, src, dst, src_d, dst_d):
        t0 = tt * T
        mark("start", l, tt)
        R_src = [] if src_d is None else [src_d[tt]]
        B.dma("sp", hT3, src[:, t0:t0 + T].rearrange("(k p) t -> p k t", p=128), R=R_src, W=[hT])
        rms(hT3, 8, D, l, "pre_mix_g", None, [hT], None)
        for k in range(8):
            B.stt("dve", uT3[:, k, :], hT3[:, k, :], vcol(l, "pre_mix_g", k), rt2.ap, ALU.mult, ALU.mult,
                  [hT, vec[l], rt2], [uT])

        def fin():
            W_dst = [] if dst_d is None else [dst_d[tt]]
            B.dma("sp", dst[:, t0:t0 + T].rearrange("(k p) t -> p k t", p=128), hT3, R=[hT], W=W_dst)

        if stage <= 0:
            return fin()

        def zmm(p, wv, ws, c0, n, M=None):
            for k in range(8):
                B.mm(p[0:n, 0:T], wv[:, k, c0:c0 + n], uT3[:, k, :], [ws, uT], [p], start=(k == 0), stop=(k == 7))

        mark("mla", l, tt)
        wv, ws = wcols("w_in", l, 0, 448, 8)
        wsw, wsws = wcols("w_in_sw", l, 0, 64, 8)
        for c in range(2):
            p = psd()
            zmm(p, wv, ws, c * 128, 128)
            B.cp("act", zq3[:, c, :], p[:, 0:T], [p], [zq])
        p = psd()
        zmm(p, wv, ws, 256, 128)
        B.cp("act", zkv.ap, p[:, 0:T], [p], [zkv])
        B.dma("sp", posi.ap[0:64, :].bitcast(I32), pos[0:1, t0:t0 + T].to_broadcast([64, T]), W=[posi])
        B.cp("dve", posf[0:64, :], posi.ap[0:64, :].bitcast(I32), [posi], [posf])
        B.ts("dve", tq[0:64, :], posf[0:64, :], cst[0:64, CI_INVF:CI_INVF + 1], ALU.mult, [posf, cst], [tq],
             s2=float(1.0 / (2 * np.pi)), op1=ALU.mult)
        MAGIC = 12582912.0
        for (dstb, shift) in ((sinT, 0.0), (cosT, 0.25)):
            if shift != 0.0:
                B.ts("dve", tq2[0:64, :], tq[0:64, :], shift, ALU.add, [tq], [tq2])
                srcq = tq2
            else:
                srcq = tq
            B.ts("dve", rp1[0:64, :], srcq[0:64, :], MAGIC, ALU.add, [srcq], [rp1], s2=MAGIC, op1=ALU.subtract)
            B.tt("dve", rp2[0:64, :], srcq[0:64, :], rp1[0:64, :], ALU.subtract, [srcq, rp1], [rp2])
            if shift == 0.0:
                B.A(dstb[0:64, :], rp2[0:64, :], AF.Sin, [rp2, cst], [dstb], scale=cst[0:64, CI_SGN:CI_SGN + 1])
            else:
                B.A(dstb[0:64, :], rp2[0:64, :], AF.Sin, [rp2], [dstb], scale=TWO_PI)
        p1 = psd()
        zmm(p1, wv, ws, 384, 64)
        p2 = psd()
        zmm(p2, wsw, wsws, 0, 64)
        B.tt("dve", rp1[0:64, :], p1[0:64, 0:T], cosT[0:64, :], ALU.mult, [p1, cosT], [rp1])
        B.tt("dve", rp2[0:64, :], p2[0:64, 0:T], sinT[0:64, :], ALU.mult, [p2, sinT], [rp2])
        B.tt("dve", Kr[0:64, t0:t0 + T], rp1[0:64, :], rp2[0:64, :], ALU.add, [rp1, rp2], [Kc_d[tt]])
        rms(zkv.ap.rearrange("p (a b) -> p a b", a=1), 1, 128, l, "kv_g", None, [zkv], None)
        B.stt("dve", Kc[:, t0:t0 + T], zkv.ap, vcol(l, "kv_g", 0), rt2.ap, ALU.mult, ALU.mult, [zkv, vec[l], rt2],
              [Kc_d[tt]])
        pt = PS[7]
        ptb = pt.ap.bitcast(BF16)
        for i in range(2):
            B.tr(ptb[:, i * 128:(i + 1) * 128], Kc[:, t0 + i * 128:t0 + (i + 1) * 128], ident_bf, [Kc_d[tt], cbf], [pt])
        B.cp("act", Vc[:, (2 * tt) * 128:(2 * tt + 2) * 128], ptb[:, 0:256], [pt], [Kc_d[tt]])
        rms(zq3, 2, 256, l, "q_g", None, [zq], None)
        for c in range(2):
            B.stt("dve", zqn3[:, c, :], zq3[:, c, :], vcol(l, "q_g", c), rt2.ap, ALU.mult, ALU.mult,
                  [zq, vec[l], rt2], [zqn])
        wq = wb["w_uq"][l].rearrange("(k p) n -> p k n", p=128)
        wqv, wqs = wload("w_uq", l, wq, [128, 2, 768])
        wqsw = wb["w_uq_sw"][l].rearrange("(k p) n -> p k n", p=128)
        wqswv, wqsws = wload("w_uq_sw", l, wqsw, [128, 2, 256])
        wkt = wb["w_ukT"][l].rearrange("(h p) n -> p h n", p=128)
        wktv, wkts = wload("w_ukT", l, wkt, [128, 4, 128])
        for h in range(4):
            p = psd()
            for k in range(2):
                B.mm(p[:, 0:T], wqv[:, k, h * 192:h * 192 + 128], zqn3[:, k, :], [wqs, zqn], [p], start=(k == 0),
                     stop=(k == 1))
            q_ = qn[h % 2]
            B.cp("act", q_.ap, p[:, 0:T], [p], [q_])
            p = psd()
            B.mm(p[:, 0:T], wktv[:, h, :], q_.ap, [wkts, q_], [p])
            B.cp("act", Qabs3[:, h, :], p[:, 0:T], [p], [Qabs])
            p1 = psd()
            for k in range(2):
                B.mm(p1[0:64, 0:T], wqv[:, k, h * 192 + 128:h * 192 + 192], zqn3[:, k, :], [wqs, zqn], [p1],
                     start=(k == 0), stop=(k == 1))
            p2 = psd()
            for k in range(2):
                B.mm(p2[0:64, 0:T], wqswv[:, k, h * 64:(h + 1) * 64], zqn3[:, k, :], [wqsws, zqn], [p2],
                     start=(k == 0), stop=(k == 1))
            B.tt("dve", rp1[0:64, :], p1[0:64, 0:T], cosT[0:64, :], ALU.mult, [p1, cosT], [rp1])
            B.tt("dve", rp2[0:64, :], p2[0:64, 0:T], sinT[0:64, :], ALU.mult, [p2, sinT], [rp2])
            B.tt("dve", Qrope3[0:64, h, :], rp1[0:64, :], rp2[0:64, :], ALU.add, [rp1, rp2], [Qrope])
        mark("mla_attn", l, tt)
        wkv = wb["w_ukv"][l]
        wkvv, wkvs = wload("w_ukv", l, wkv, [128, 1024])
        nkb = 2 * (tt + 1)
        Kdeps = [Kc_d[j // 2] for j in range(nkb)]
        for hp in range(2):
            Ob, Sb = PS[4], PS[5]
            O3 = v3(Ob.ap, 2)
            S3 = v3(Sb.ap, 2)
            def m_scores(j):
                jd = j - 2 * tt
                q0 = max(jd, 0) * 128
                ps_ = PS[j % 2 + 2]
                ps3 = v3(ps_.ap, 2)
                for hh in range(2):
                    h = 2 * hp + hh
                    B.mm(ps3[:, hh, q0:T], Kc[:, j * 128:(j + 1) * 128], Qabs3[:, h, q0:T], [Kdeps[j], Qabs], [ps_],
                         start=True, stop=False)
                    B.mm(ps3[:, hh, q0:T], Kr[0:64, j * 128:(j + 1) * 128], Qrope3[0:64, h, q0:T],
                         [Kdeps[j], Qrope], [ps_], start=False, stop=True)

            def m_rest(j):
                jd = j - 2 * tt
                q0 = max(jd, 0) * 128
                ps_ = PS[j % 2 + 2]
                ps3 = v3(ps_.ap, 2)
                PT = PTm[j % 2]
                PT3 = v3(PT.ap, 2)
                B.A(PT3[:, :, q0:T], ps3[:, :, q0:T], AF.Exp, [ps_], [PT], scale=MLA_SCALE)
                if jd >= 0:
                    B.ms("pool", PT3[64:128, :, q0:q0 + 64], 0.0, [PT])
                for hh in range(2):
                    B.mm(O3[:, hh, q0:T], Vc3[:, j, :], PT3[:, hh, q0:T], [Kdeps[j], PT], [Ob],
                         start=(j == 0 and hh == 0), stop=(j == nkb - 1))
                for hh in range(2):
                    B.mm(S3[:, hh, q0:T], ones_bf, PT3[:, hh, q0:T], [cbf, PT], [Sb],
                         start=(j == 0 and hh == 0), stop=(j == nkb - 1))

            m_scores(0)
            for j in range(nkb):
                if j + 1 < nkb:
                    m_scores(j + 1)
                m_rest(j)
            B.rcp(rinv.ap, Sb.ap, [Sb], [rinv])
            B.tt("dve", On.ap, Ob.ap, rinv.ap, ALU.mult, [Ob, rinv], [On])
            On3 = v3(On.ap, 2)
            for hh in range(2):
                h = 2 * hp + hh
                p = psd()
                B.mm(p[:, 0:T], wkvv[:, h * 256 + 128:h * 256 + 256], On3[:, hh, :], [wkvs, On], [p])
                B.cp("act", o_mla[:, h * T:(h + 1) * T], p[:, 0:T], [p], [o_mla])

        if stage <= 1:
            return fin()
        mark("ca", l, tt)
        wv, ws = wcols("w_in", l, 2240, 512, 8)
        for c in range(4):
            p = psd()
            zmm(p, wv, ws, c * 128, 128)
            B.A(Qs3[:, c, :], p[:, 0:T], AF.Copy, [p], [Qs], scale=0.125)
        wv, ws = wcols("w_in", l, 2752, 512, 8)
        sl0 = (2 * tt) % 8
        for c in range(4):
            p = psd()
            zmm(p, wv, ws, c * 128, 128)
            B.cp("act", CK3[:, c, sl0 * 128:sl0 * 128 + T], p[:, 0:T], [p], [CK_d[sl0 // 2]])
        wv, ws = wcols("w_in", l, 3264, 512, 8)
        for i in range(2):
            p = psd()
            for k in range(8):
                B.mm(p.ap, uT3[:, k, i * 128:(i + 1) * 128], wv[:, k, :], [uT, ws], [p], start=(k == 0), stop=(k == 7))
            B.cp("act", CV3[:, sl0 + i, :], p.ap, [p], [CV_d[sl0 + i]])
        def ca_attn():
            for i in range(2):
                qb = 2 * tt + i
                Ob, Sb = PS[4], PS[5]
                O3 = v3(Ob.ap, 4)
                S3 = v3(Sb.ap, 4)
                bl = list(range(max(0, qb - 4), qb + 1))
                units = [(b, e) for b in bl for e in range(2)]

                def c_scores(b, e):
                    r = qb - b
                    slot = b % 8
                    ps_ = PS[2 + e]
                    ps3 = v3(ps_.ap, 4)
                    pb_ = e * 64
                    for ch in range(4):
                        h = ch * 2 + e
                        B.mm(ps3[:, ch, :], CK3[pb_:pb_ + 64, ch, slot * 128:(slot + 1) * 128],
                             Qs3[pb_:pb_ + 64, ch, i * 128:(i + 1) * 128], [CK_d[slot // 2], Qs], [ps_],
                             start=True, stop=False)
                        B.mm(ps3[:, ch, :], Xb3[:, r * 8 + h, :], J_bf, [Xb, cbf], [ps_], start=False, stop=True)

                def c_rest(b, e):
                    slot = b % 8
                    ps_ = PS[2 + e]
                    PT = PTc[e]
                    PT3 = v3(PT.ap, 4)
                    pb_ = e * 64
                    B.A(PT.ap, ps_.ap, AF.Exp, [ps_], [PT])
                    for ch in range(4):
                        h = ch * 2 + e
                        first = (b == bl[0] and ch == 0)
                        B.mm(O3[pb_:pb_ + 64, ch, :], CV3[:, slot, h * 64:(h + 1) * 64], PT3[:, ch, :],
                             [CV_d[slot], PT], [Ob], start=first, stop=True)
                    for ch in range(4):
                        first = (b == bl[0] and ch == 0)
                        B.mm(S3[pb_:pb_ + 64, ch, :], ones_bf[:, 0:64], PT3[:, ch, :], [cbf, PT], [Sb],
                             start=first, stop=True)

                c_scores(*units[0])
                for ui, u_ in enumerate(units):
                    if ui + 1 < len(units):
                        c_scores(*units[ui + 1])
                    c_rest(*u_)
                    yield 1
                B.rcp(rinvc.ap, Sb.ap, [Sb], [rinvc])
                B.tt("dve", v3(o_ca.ap, 4)[:, :, i * 128:(i + 1) * 128], O3, v3(rinvc.ap, 4), ALU.mult, [Ob, rinvc],
                     [o_ca])


        if stage <= 2:
            for _ in ca_attn():
                pass
            return fin()
        mark("rw", l, tt)
        rwkv(l, tt, ca_attn())
        if dbg and l == 0 and tt == 0:
            for n, bsrc in (("aT", aTt), ("bT", bTt), ("kT", kTt), ("rT", rTt), ("y", yb), ("bonus", bonus), ("g", gbuf),
                            ("TM", TM), ("AM", AM), ("Sst", Sst), ("small", small)):
                if n not in dbg_t:
                    dbg_t[n] = dram("dbg_" + n, [128, bsrc.ap.shape[1]], bsrc.ap.dtype, "ExternalOutput")
                B.dma("sp", dbg_t[n], bsrc.ap, R=[bsrc] + ([AM_d[c_][h_] for c_ in range(2) for h_ in range(4)] if n == "AM" else []))
        if stage <= 3 or (stage >= 21 and stage <= 26):
            return fin()

        if dbg and l == 0:
            for n, bsrc in (("o_mla", o_mla), ("o_rw", o_rw), ("o_ca", o_ca)):
                B.dma("sp", dbg_t[n][:, t0:t0 + T].rearrange("(k p) t -> p k t", p=128), v3(bsrc.ap, 4), R=[bsrc])

        mark("merge", l, tt)
        obr = (o_mla, o_rw, o_ca)
        for cg in range(2):
            for n in range(3):
                gv, gs = wcols("w_in", l, 3776 + n * 1024 + cg * 512, 512, 8)
                bap = wb["w_branch"][l, n * 512:(n + 1) * 512, cg * 512:(cg + 1) * 512].rearrange(
                    "(k p) n -> p k n", p=128)
                bv, bs = wload("w_branch", l, bap, [128, 4, 512])
                ob3 = v3(obr[n].ap, 4)
                for cl in range(4):
                    c = cg * 4 + cl
                    pg = psd()
                    zmm(pg, gv, gs, cl * 128, 128)
                    gb = gsb[(c * 3 + n) % 2]
                    B.A(gb.ap, pg[:, 0:T], AF.Sigmoid, [pg], [gb])
                    py = psd()
                    for k in range(4):
                        B.mm(py[:, 0:T], bv[:, k, cl * 128:(cl + 1) * 128], ob3[:, k, :], [bs, obr[n]], [py],
                             start=(k == 0), stop=(k == 3))
                    if n == 0:
                        B.tt("dve", mrg3[:, c, :], py[:, 0:T], gb.ap, ALU.mult, [py, gb], [mrg])
                    else:
                        tf = tmpf[(c * 3 + n) % 2]
                        B.tt("dve", tf.ap, py[:, 0:T], gb.ap, ALU.mult, [py, gb], [tf])
                        if n == 1:
                            B.tt("pool", mrg3[:, c, :], mrg3[:, c, :], tf.ap, ALU.add, [mrg, tf], [mrg])
                        else:
                            B.tt("pool", mrgb3[:, c, :], mrg3[:, c, :], tf.ap, ALU.add, [mrg, tf], [mrgb])
        B.dma("sp", hT3, src[:, t0:t0 + T].rearrange("(k p) t -> p k t", p=128), R=R_src, W=[hT])
        for cg in range(2):
            wv, ws = wcols("w_out", l, cg * 512, 512, 8)
            for cl in range(4):
                c = cg * 4 + cl
                p = psd()
                for k in range(8):
                    B.mm(p[:, 0:T], wv[:, k, cl * 128:(cl + 1) * 128], mrgb3[:, k, :], [ws, mrgb], [p], start=(k == 0),
                         stop=(k == 7))
                B.cp("act", mo3[:, c, :], p[:, 0:T], [p], [mo])
        rms(mo3, 8, D, l, "post_mix_g", None, [mo], None)
        for k in range(8):
            tf = tmpf[k % 2]
            B.stt("dve", tf.ap, mo3[:, k, :], vcol(l, "post_mix_g", k), rt2.ap, ALU.mult, ALU.mult,
                  [mo, vec[l], rt2], [tf])
            B.tt("pool", hT3[:, k, :], hT3[:, k, :], tf.ap, ALU.add, [hT, tf], [hT])
        mark("ffn", l, tt)
        rms(hT3, 8, D, l, "pre_ff_g", None, [hT], None)
        for k in range(8):
            B.stt("dve", uT3[:, k, :], hT3[:, k, :], vcol(l, "pre_ff_g", k), rt2.ap, ALU.mult, ALU.mult,
                  [hT, vec[l], rt2], [uT])
        for cg in range(8):
            wv, ws = wcols("w_ff1", l, cg * 512, 512, 8)
            for cl in range(4):
                j = cg * 4 + cl
                p = psd()
                zmm(p, wv, ws, cl * 128, 128)
                r_ = rl[j % 2]
                B.A(r_.ap, p[:, 0:T], AF.Relu, [p], [r_])
                B.tt("pool", aT3[:, j, :], r_.ap, r_.ap, ALU.mult, [r_], [aT])
        for cg in range(2):
            accs = [PS[4 + i] for i in range(4)]
            for kg in range(4):
                wap = wb["w_ff2"][l, kg * 1024:(kg + 1) * 1024, cg * 512:(cg + 1) * 512].rearrange(
                    "(k p) n -> p k n", p=128)
                wv, ws = wload("w_ff2", l, wap, [128, 8, 512])
                for cl in range(4):
                    for kk in range(8):
                        B.mm(accs[cl][:, 0:T], wv[:, kk, cl * 128:(cl + 1) * 128], aT3[:, kg * 8 + kk, :], [ws, aT],
                             [accs[cl]], start=(kg == 0 and kk == 0), stop=(kg == 3 and kk == 7))
            for cl in range(4):
                B.cp("act", mo3[:, cg * 4 + cl, :], accs[cl][:, 0:T], [accs[cl]], [mo])
        rms(mo3, 8, D, l, "post_ff_g", None, [mo], None)
        for k in range(8):
            tf = tmpf[k % 2]
            B.stt("dve", tf.ap, mo3[:, k, :], vcol(l, "post_ff_g", k), rt2.ap, ALU.mult, ALU.mult,
                  [mo, vec[l], rt2], [tf])
            B.tt("pool", hT3[:, k, :], hT3[:, k, :], tf.ap, ALU.add, [hT, tf], [hT])
        mark("ple", l, tt)
        B.cp("act", uT.ap, hT.ap, [hT], [uT])
        B.dma("pool", pb3, pT[l, :, t0:t0 + T].rearrange("(k p) t -> p k t", p=128), W=[pb])
        ppv, pps = wload("w_ple_proj", l, wb["w_ple_proj"][l].rearrange("(k p) n -> p k n", p=128), [128, 2, 1024])
        for cg in range(2):
            wv, ws = wcols("w_ple_gate", l, cg * 512, 512, 8)
            for cl in range(4):
                c = cg * 4 + cl
                pg = psd()
                zmm(pg, wv, ws, cl * 128, 128)
                gb = gsb[c % 2]
                B.A(gb.ap, pg[:, 0:T], AF.Sigmoid, [pg], [gb])
                pp = psd()
                for k in range(2):
                    B.mm(pp[:, 0:T], ppv[:, k, c * 128:(c + 1) * 128], pb3[:, k, :], [pps, pb], [pp], start=(k == 0),
                         stop=(k == 1))
                tf = tmpf[c % 2]
                B.tt("dve", tf.ap, pp[:, 0:T], gb.ap, ALU.mult, [pp, gb], [tf])
                B.tt("pool", hT3[:, c, :], hT3[:, c, :], tf.ap, ALU.add, [hT, tf], [hT])
        W_dst = [] if dst_d is None else [dst_d[tt]]
        B.dma("sp", dst[:, t0:t0 + T].rearrange("(k p) t -> p k t", p=128), hT3, R=[hT], W=W_dst)

    def rwkv(l, tt, ca):
        zi = [0]

        def zchunk(wv, ws, c0, cidx, dest):
            p = psd()
            for k in range(8):
                B.mm(p[:, 0:T], wv[:, k, c0:c0 + 128] if c0 is not None else wv[:, k, :], uT3[:, k, :], [ws, uT], [p],
                     start=(k == 0), stop=(k == 7))
            z = zb[zi[0] % 2]
            zi[0] += 1
            B.cp("act", z[:, 1:T + 1], p[:, 0:T], [p], [z])
            B.cp("pool", z[:, 0:1], zlast[:, cidx:cidx + 1], [zlast], [z])
            B.tt("dve", dd.ap, z[:, 0:T], z[:, 1:T + 1], ALU.subtract, [z], [dd])
            B.stt("dve", dest.ap, dd.ap, vcol(l, "mu", cidx), z[:, 1:T + 1], ALU.mult, ALU.add, [dd, vec[l], z], [dest])
            B.cp("pool", zlast[:, cidx:cidx + 1], z[:, T:T + 1], [z], [zlast])

        wv, ws = wcols("w_in", l, 448 + 1536, 256, 8)
        zchunk(wv, ws, 0, 12, zs12)
        zchunk(wv, ws, 128, 13, zs13)
        B.A(txw[0:64, :], zs12[0:64, :], AF.Tanh, [zs12], [txw])
        B.cp("act", xab[64:128, :], zs12[64:128, :], [zs12], [xab])
        B.A(sgb.ap, zs13.ap, AF.Sigmoid, [zs13], [sgb])
        if stage == 21:
            return
        s_wa = lwa
        B.dma("sp", s_wa.ap[0:64, 0:512], wb["rw_w_up"][l], R=[wdep[("rw_w_up", l)]], W=[s_wa])
        B.dma("sp", s_wa.ap[64:128, 0:512], wb["rw_a_up"][l], R=[wdep[("rw_a_up", l)]], W=[s_wa])
        gups = lwg
        gupv = lwg.ap
        B.dma("sp", gupv, wb["rw_g_up"][l], R=[wdep[("rw_g_up", l)]], W=[lwg])
        sm = small

        def stageA(hp):
            qs = hp % 2
            s4 = wsl[wsl_i[0]]
            wsl_i[0] = (wsl_i[0] + 1) % NW
            wv4 = s4.ap[:, 0:3072].rearrange("p (j k n) -> p j k n", j=3, k=8)
            ws4 = s4
            for j_ in range(3):
                c0_ = 448 + j_ * 512 + hp * 128
                B.dma("sp", wv4[:, j_, :, :], wb["w_in"][l, :, c0_:c0_ + 128].rearrange("(k p) n -> p k n", p=128),
                      R=[wdep[("w_in", l)]], W=[s4])
            zchunk(wv4[:, 0, :, :], ws4, None, hp, zrs[qs])
            yield 1
            zchunk(wv4[:, 1, :, :], ws4, None, 4 + hp, zks[qs])
            yield 1
            zchunk(wv4[:, 2, :, :], ws4, None, 8 + hp, zvs[qs])
            yield 1
            X = tmps[qs]
            cols = slice(hp * 128, (hp + 1) * 128)
            p = psd()
            B.mm(p[:, 0:T], s_wa.ap[0:64, cols], txw[0:64, :], [s_wa, txw], [p])
            B.A(X["sgw"].ap, p[:, 0:T], AF.Sigmoid, [p, vec[l]], [X["sgw"]], bias=vcol(l, "w0", hp))
            p = psd()
            B.mm(p[:, 0:T], s_wa.ap[64:128, cols], xab[64:128, :], [s_wa, xab], [p])
            B.A(X["aa"].ap, p[:, 0:T], AF.Sigmoid, [p, vec[l]], [X["aa"]], bias=vcol(l, "a0", hp))
            p = psd()
            B.mm(p[:, 0:T], gupv[:, cols], sgb.ap, [gups, sgb], [p])
            B.cp("act", g3[:, hp, :], p[:, 0:T], [p], [gbuf])
            yield 1
            cs = X["cs"]
            onec = cst[:, CI_ONE:CI_ONE + 128]
            for c in range(2):
                sl_ = slice(c * 128, (c + 1) * 128)
                B.op("dve", lambda e, sl_=sl_: e.tensor_tensor_scan(X["cs"][:, sl_], onec, X["sgw"][:, sl_], 0.0,
                                                                    ALU.mult, ALU.add), [cst, X["sgw"]], [X["cs"]])
            for c in range(2):
                B.ts("dve", X["csc"][:, c * 128:(c + 1) * 128], cs[:, c * 128:(c + 1) * 128],
                     cs[:, c * 128 + 63:c * 128 + 64], ALU.subtract, [cs], [X["csc"]])
            for c in range(2):
                B.cp("pool", sm[:, hp * 2 + c:hp * 2 + c + 1], cs[:, c * 128 + 63:c * 128 + 64], [cs], [sm])
                B.cp("pool", sm[:, 8 + hp * 2 + c:8 + hp * 2 + c + 1], X["csc"][:, c * 128 + 127:c * 128 + 128],
                     [X["csc"]], [sm])
            B.tt("dve", X["csx"].ap, X["csc"].ap, X["sgw"].ap, ALU.subtract, [X["csc"], X["sgw"]], [X["csx"]])
            for c in range(2):
                B.ts("dve", X["dC"][:, c * 128:(c + 1) * 128], X["csc"][:, c * 128:(c + 1) * 128],
                     X["csc"][:, c * 128 + 127:c * 128 + 128], ALU.subtract, [X["csc"]], [X["dC"]])
            B.A(X["E1"].ap, X["csc"].ap, AF.Exp, [X["csc"]], [X["E1"]], scale=-CC)
            B.A(X["E2"].ap, X["csc"].ap, AF.Exp, [X["csc"]], [X["E2"]], scale=CC)
            B.A(X["E3"].ap, X["csx"].ap, AF.Exp, [X["csx"]], [X["E3"]], scale=-CC)
            B.A(X["E4"].ap, X["dC"].ap, AF.Exp, [X["dC"]], [X["E4"]], scale=CC)
            yield 1
            B.ts("dve", X["kk"].ap, zks[qs].ap, vcol(l, "k_k", hp), ALU.mult, [zks[qs], vec[l]], [X["kk"]])
            B.A(kk2s[qs].ap, X["kk"].ap, AF.Square, [X["kk"]], [kk2s[qs]])
            p = psd()
            B.mm(p[:, 0:T], blk_bf, kk2s[qs].ap, [cbf, kk2s[qs]], [p])
            B.ts("dve", X["ssm"].ap, p[:, 0:T], 1e-24, ALU.max, [p], [X["ssm"]])
            B.A(X["rs"].ap, X["ssm"].ap, AF.Sqrt, [X["ssm"]], [X["rs"]])
            B.rcp(X["ssm"].ap, X["rs"].ap, [X["rs"]], [X["ssm"]])
            B.tt("dve", X["kkn"].ap, X["kk"].ap, X["ssm"].ap, ALU.mult, [X["kk"], X["ssm"]], [X["kkn"]])
            yield 1
            B.stt("dve", aT_3[:, hp, :], X["kkn"].ap, -1.0, X["E3"].ap, ALU.mult, ALU.mult, [X["kkn"], X["E3"]], [aTt])
            B.tt("dve", X["tb"].ap, X["kkn"].ap, X["aa"].ap, ALU.mult, [X["kkn"], X["aa"]], [X["tb"]])
            B.tt("dve", bT_3[:, hp, :], X["tb"].ap, X["E2"].ap, ALU.mult, [X["tb"], X["E2"]], [bTt])
            B.tt("pool", bhs[qs].ap, X["tb"].ap, X["E4"].ap, ALU.mult, [X["tb"], X["E4"]], [bhs[qs]])
            B.ts("dve", X["tc"].ap, X["aa"].ap, vcol(l, "k_a", hp), ALU.mult, [X["aa"], vec[l], omka[l]], [X["tc"]],
                 s2=omka[l][:, hp:hp + 1], op1=ALU.add)
            B.tt("dve", X["km"].ap, zks[qs].ap, X["tc"].ap, ALU.mult, [zks[qs], X["tc"]], [X["km"]])
            B.tt("dve", kT_3[:, hp, :], X["km"].ap, X["E2"].ap, ALU.mult, [X["km"], X["E2"]], [kTt])
            B.tt("pool", khs[qs].ap, X["km"].ap, X["E4"].ap, ALU.mult, [X["km"], X["E4"]], [khs[qs]])
            yield 1
            B.tt("dve", rT_3[:, hp, :], zrs[qs].ap, X["E1"].ap, ALU.mult, [zrs[qs], X["E1"]], [rTt])
            B.stt("dve", rkrs[qs].ap, zrs[qs].ap, vcol(l, "r_k", hp), X["km"].ap, ALU.mult, ALU.mult, [zrs[qs], vec[l], X["km"]], [rkrs[qs]])
            p = psd()
            B.mm(p[:, 0:T], blk_bf, rkrs[qs].ap, [cbf, rkrs[qs]], [p])
            B.tt("dve", bonus3[:, hp, :], p[:, 0:T], zvs[qs].ap, ALU.mult, [p, zvs[qs]], [bonus])
            B.cp("pool", vbfs[qs].ap, zvs[qs].ap, [zvs[qs]], [vbfs[qs]])
            yield 1
            pt = PS[7]
            ptb = pt.ap.bitcast(BF16)
            ptb4 = ptb[:, 0:768].rearrange("p (j c n) -> p j c n", j=3, c=2)
            for j_, sbuf_ in enumerate((vbfs[qs], bhs[qs], khs[qs])):
                for c in range(2):
                    B.tr(ptb4[:, j_, c, :], sbuf_[:, c * 128:(c + 1) * 128], ident_bf, [sbuf_, cbf], [pt])
            B.cp("act", TM5[:, :, :, hp, :], ptb4, [pt], [TM])
            yield "pre6"
            bA, bB, bC = PS[4], PS[5], PS[6]
            bA4 = v3(bA.ap, 4)
            bB4 = v3(bB.ap, 4)
            bC4 = v3(bC.ap, 4)
            bks = ((PS[4], PS[5], PS[6]), (PS[1], PS[2], PS[3]))
            Q4 = Qd[qs].ap.rearrange("p (c e n) -> p c e n", c=2, e=2)
            QT4 = QTd[qs].ap.rearrange("p (c e n) -> p c e n", c=2, e=2)
            for e in range(2):
                kA, kB, kC = bks[e]
                kA4 = kA.ap.rearrange("p (c t n) -> p c t n", c=2, t=2)
                kB4 = kB.ap.rearrange("p (c t n) -> p c t n", c=2, t=2)
                kC3 = v3(kC.ap[:, 0:256], 2)
                pr = slice(e * 64, e * 64 + 64)
                for c in range(2):
                    cs_ = slice(c * 128, (c + 1) * 128)
                    B.mm(kA4[:, c, 0, :], kT_3[pr, hp, cs_], aT_3[pr, hp, cs_], [kTt, aTt], [kA])
                    B.mm(kA4[:, c, 1, :], bT_3[pr, hp, cs_], aT_3[pr, hp, cs_], [bTt, aTt], [kA])
                    B.mm(kB4[:, c, 0, :], bT_3[pr, hp, cs_], rT_3[pr, hp, cs_], [bTt, rTt], [kB])
                    B.mm(kB4[:, c, 1, :], kT_3[pr, hp, cs_], rT_3[pr, hp, cs_], [kTt, rTt], [kB])
                    B.mm(kC3[:, c, :], aT_3[pr, hp, cs_], bT_3[pr, hp, cs_], [aTt, bTt], [kC])
                amds = [AM_d[0][hp], AM_d[1][hp]]
                B.tt("dve", AM6[:, :, hp, 0, e, :], kA4[:, :, 0, :], m_su.unsqueeze(1).to_broadcast([128, 2, 128]),
                     ALU.mult, [kA, cst], amds)
                B.tt("dve", Q4[:, :, e, :], kA4[:, :, 1, :], m_su.unsqueeze(1).to_broadcast([128, 2, 128]),
                     ALU.mult, [kA, cst], [Qd[qs]])
                for c in range(2):
                    B.tt("dve", AM6[:, c, hp, 1:3, e, :], kB4[:, c, :, :], m_u.unsqueeze(1).to_broadcast([128, 2, 128]),
                         ALU.mult, [kB, cst], [amds[c]])
                B.tt("dve", QT4[:, :, e, :], kC3, m_sl.unsqueeze(1).to_broadcast([128, 2, 128]), ALU.mult, [kC, cst],
                     [QTd[qs]])
            yield 1

        def stageB(hp):
            qs = hp % 2
            bA, bB, bC = PS[4], PS[5], PS[6]
            bA4 = v3(bA.ap, 4)
            bB4 = v3(bB.ap, 4)
            bC4 = v3(bC.ap, 4)
            B.tt("dve", v3(Xd[qs].ap, 4), v3(Qd[qs].ap, 4), ident_f.unsqueeze(1).to_broadcast([128, 4, 128]), ALU.add,
                 [Qd[qs], cst], [Xd[qs]])
            B.cp("pool", Xbbs[qs].ap, Xd[qs].ap, [Xd[qs]], [Xbbs[qs]])
            Qc, QTc, Xc = v3(Qd[qs].ap, 4), v3(QTd[qs].ap, 4), v3(Xd[qs].ap, 4)
            Xb4 = v3(Xbbs[qs].ap, 4)
            for j in range(6):
                for m in range(4):
                    B.mm(bA4[:, m, :], Qc[:, m, :], QTc[:, m, :], [Qd[qs], QTd[qs]], [bA])
                if j < 5:
                    for m in range(4):
                        B.mm(bB4[:, m, :], QTc[:, m, :], Qc[:, m, :], [Qd[qs], QTd[qs]], [bB])
                B.cp("act", QTc, bA4, [bA], [QTd[qs]])
                if j < 5:
                    B.cp("act", Qc, bB4, [bB], [Qd[qs]])
                for m in range(4):
                    B.mm(bC4[:, m, :], QTc[:, m, :], Xb4[:, m, :], [QTd[qs], Xbbs[qs]], [bC])
                if j < 5:
                    B.tt("dve", Xc, Xc, bC4, ALU.add, [Xd[qs], bC], [Xd[qs]])
                    B.cp("pool", Xbbs[qs].ap, Xd[qs].ap, [Xd[qs]], [Xbbs[qs]])
                else:
                    for c in range(2):
                        B.tt("dve", AM6[:, c, hp, 3, :, :], Xc[:, c * 2:c * 2 + 2, :], bC4[:, c * 2:c * 2 + 2, :],
                             ALU.add, [Xd[qs], bC], [AM_d[c][hp]])
                yield 1

        K0 = 5
        Ag = [stageA(h) for h in range(4)]
        Bg = [stageB(h) for h in range(4)]
        A_cnt = [0] * 4
        A_started = [False] * 4
        A_done = [False] * 4
        A_pre6 = [False] * 4
        B_done = [False] * 4
        ca_done = False
        rot["n"] = 2
        guard = 0
        while not (ca_done and all(A_done) and all(B_done)):
            guard += 1
            assert guard < 10000
            if not ca_done:
                ca_done = next(ca, "END") == "END"
                if ca_done:
                    rot["n"] = 4
            for h in range(4):
                if not A_started[h]:
                    ok = (h == 0) or (A_cnt[h - 1] >= K0) or A_done[h - 1]
                    if h >= 2:
                        ok = ok and (A_pre6[h - 2] or A_done[h - 2])
                    if ok:
                        A_started[h] = True
                if A_started[h] and not A_done[h]:
                    if A_pre6[h] and not A_done[h]:
                        if (not ca_done) or (h >= 2 and not B_done[h - 2]):
                            continue
                    v_ = next(Ag[h], "END")
                    if v_ == "END":
                        A_done[h] = True
                    else:
                        A_cnt[h] += 1
                        if v_ == "pre6":
                            A_pre6[h] = True
            for h in range(4):
                if A_done[h] and not B_done[h]:
                    if next(Bg[h], "END") == "END":
                        B_done[h] = True
        mark("rw_chain", l, tt)
        B.A(sm[:, 16:24], sm[:, 0:8], AF.Exp, [sm], [sm], scale=-CC)
        B.A(sm[:, 24:32], sm[:, 8:16], AF.Exp, [sm], [sm], scale=-CC)
        Pm3 = sm[:, 16:24].rearrange("p (h c) -> p h c", c=2)
        PC3 = sm[:, 24:32].rearrange("p (h c) -> p h c", c=2)
        S3_ = v3(Sst.ap, 4)
        Smid3 = v3(Smid.ap, 4)
        Smb3 = v3(Smb.ap, 4)
        allAM = [AM_d[c][h] for c in range(2) for h in range(4)]
        for c in range(2):
            cs_ = slice(c * 128, (c + 1) * 128)
            amc = AM_d[c]
            B.tt("dve", Smid3, S3_, Pm3[:, :, c:c + 1].to_broadcast([128, 4, 64]), ALU.mult, [Sst, sm], [Smid])
            B.cp("dve", Smb.ap, Smid.ap, [Smid], [Smb])
            pWe = (PS[0], PS[1])
            pU, pS = PS[2], PS[3]
            pYe = (PS[4], PS[5])
            pU3 = v3(pU.ap, 8)
            pS3 = v3(pS.ap[:, 0:256], 4)
            W0b3 = v3(W0b.ap, 8)
            Ut3 = v3(Ut.ap, 8)
            W0b4 = W0b.ap.rearrange("p (h e v) -> p h e v", h=4, e=2)
            for e in range(2):
                pr = slice(e * 64, e * 64 + 64)
                pW3 = v3(pWe[e].ap[:, 0:256], 4)
                for hp in range(4):
                    B.mm(pW3[:, hp, :], AM6[:, c, hp, 0, e, :], TM5[:, 0, c, hp, e * 64:e * 64 + 64], [amc[hp], TM],
                         [pWe[e]], start=True, stop=False)
                    B.mm(pW3[:, hp, :], aT_3[pr, hp, cs_], Smb3[pr, hp, :], [aTt, Smb], [pWe[e]], start=False, stop=True)
                B.cp("act", W0b4[:, :, e, :], pW3, [pWe[e]], [W0b])
            for hp in range(4):
                for e in range(2):
                    h = hp * 2 + e
                    B.mm(pU3[:, h, :], AM6[:, c, hp, 3, e, :], W0b3[:, h, :], [amc[hp], W0b], [pU])
            B.cp("act", Ut.ap, pU.ap, [pU], [Ut])
            for e in range(2):
                pr = slice(e * 64, e * 64 + 64)
                pY3 = v3(pYe[e].ap, 4)
                for hp in range(4):
                    h = hp * 2 + e
                    B.mm(pY3[pr, hp, :], Smb3[pr, hp, :], rT_3[pr, hp, cs_], [Smb, rTt], [pYe[e]], start=True, stop=False)
                    B.mm(pY3[pr, hp, :], Ut3[:, h, :], AM6[:, c, hp, 1, e, :], [Ut, amc[hp]], [pYe[e]], start=False,
                         stop=False)
                    B.mm(pY3[pr, hp, :], TM5[:, 0, c, hp, e * 64:e * 64 + 64], AM6[:, c, hp, 2, e, :], [TM, amc[hp]],
                         [pYe[e]], start=False, stop=True)
                B.cp("act", y3[pr, :, cs_], pY3[pr, :, :], [pYe[e]], [yb])
            for hp in range(4):
                for e in range(2):
                    h = hp * 2 + e
                    pr = slice(e * 64, e * 64 + 64)
                    B.mm(pS3[pr, hp, :], TM5[:, 1, c, hp, e * 64:e * 64 + 64], Ut3[:, h, :], [TM, Ut], [pS], start=True,
                         stop=False)
                    B.mm(pS3[pr, hp, :], TM5[:, 2, c, hp, e * 64:e * 64 + 64], TM5[:, 0, c, hp, e * 64:e * 64 + 64],
                         [TM], [pS], start=False, stop=True)
            B.tt("dve", S3_, Smid3, PC3[:, :, c:c + 1].to_broadcast([128, 4, 64]), ALU.mult, [Smid, sm], [Sst])
            B.tt("dve", S3_, S3_, pS3, ALU.add, [Sst, pS], [Sst])
        if stage == 26:
            return
        mark("rw_norm", l, tt)
        for hp in range(4):
            B.cp("pool", ybf.ap, y3[:, hp, :], [yb], [ybf])
            p = psd()
            B.mm(p[:, 0:T], blk_bf, ybf.ap, [cbf, ybf], [p])
            B.stt("dve", yc.ap, p[:, 0:T], -1.0 / 64, y3[:, hp, :], ALU.mult, ALU.add, [p, yb], [yc])
            B.A(ysq.ap, yc.ap, AF.Square, [yc], [ysq])
            p = psd()
            B.mm(p[:, 0:T], blk_bf, ysq.ap, [cbf, ysq], [p])
            B.A(sd.ap, p[:, 0:T], AF.Sqrt, [p, eps_t], [sd], scale=1.0 / 64, bias=eps_ap(LN_EPS))
            B.rcp(t1.ap, sd.ap, [sd], [t1])
            B.tt("dve", t2.ap, yc.ap, t1.ap, ALU.mult, [yc, t1], [t2])
            B.ts("dve", t1.ap, t2.ap, vcol(l, "ln_w", hp), ALU.mult, [t2, vec[l]], [t1], s2=vcol(l, "ln_b", hp),
                 op1=ALU.add)
            B.tt("dve", t2.ap, t1.ap, bonus3[:, hp, :], ALU.add, [t1, bonus], [t2])
            B.tt("dve", o_rw[:, hp * T:(hp + 1) * T], t2.ap, g3[:, hp, :], ALU.mult, [t2, gbuf], [o_rw])

    for l in range(nl):
        layer(l)
    B.emit(block)
    es.__exit__(None, None, None)
    return nc


def _consts():
    c = np.zeros((128, NCONST), np.float32)
    i = np.arange(128)
    c[:, CI_ID:CI_ID + 128] = np.eye(128)
    c[:, CI_J:CI_J + 128] = np.eye(128)[::-1]
    c[:, CI_ONE:CI_ONE + 128] = 1.0
    blk = np.zeros((128, 128), np.float32)
    blk[:64, :64] = 1
    blk[64:, 64:] = 1
    c[:, CI_BLK:CI_BLK + 128] = blk
    c[:, CI_SU:CI_SU + 128] = (i[None, :] > i[:, None])
    c[:, CI_U:CI_U + 128] = (i[None, :] >= i[:, None])
    c[:, CI_SL:CI_SL + 128] = (i[None, :] < i[:, None])
    half = 32
    invf = (1.0 / (np.float32(10000.0) ** (np.arange(half, dtype=np.float32) / np.float32(half)))).astype(np.float32)
    c[:64, CI_INVF] = np.concatenate([invf, invf])
    c[:64, CI_SGN] = np.concatenate([-np.ones(32), np.ones(32)]) * TWO_PI
    return c


def prep_shared(inp, nl=NL):
    f = lambda a: np.ascontiguousarray(np.asarray(a, dtype=np.float32))
    sh = {}
    w_in = f(inp["w_in"])[:nl]
    sh["w_in"] = w_in
    kr = w_in[:, :, 384:448]
    sh["w_in_sw"] = np.ascontiguousarray(np.concatenate([kr[:, :, 32:], kr[:, :, :32]], axis=-1))
    wuq = f(inp["mla_w_uq"])[:nl]
    sh["w_uq"] = wuq
    r4 = wuq.reshape(nl, 256, 4, 192)[:, :, :, 128:]
    sh["w_uq_sw"] = np.ascontiguousarray(np.concatenate([r4[..., 32:], r4[..., :32]], axis=-1).reshape(nl, 256, 256))
    wukv = f(inp["mla_w_ukv"])[:nl]
    sh["w_ukv"] = wukv
    nope = wukv.reshape(nl, 128, 4, 256)[:, :, :, :128]
    sh["w_ukT"] = np.ascontiguousarray(nope.transpose(0, 2, 3, 1).reshape(nl, 512, 128))
    sh["rw_w_up"] = f(inp["rw_w_up"])[:nl]
    sh["rw_a_up"] = f(inp["rw_a_up"])[:nl]
    sh["rw_g_up"] = f(inp["rw_g_up"])[:nl]
    for n in ("w_branch", "w_out", "w_ff1", "w_ff2", "w_ple_gate", "w_ple_proj"):
        sh[n] = f(inp[n])[:nl]
    vecs = np.zeros((nl, 128, NV), np.float32)

    def put(name, arr):
        a = f(arr)[:nl].reshape(nl, -1, 128)
        vecs[:, :, VC[name]:VC[name] + a.shape[1]] = a.transpose(0, 2, 1)

    put("pre_mix_g", inp["pre_mix_g"])
    put("q_g", inp["mla_q_norm_g"])
    put("kv_g", inp["mla_kv_norm_g"])
    put("mu", inp["rw_mu"])
    put("w0", inp["rw_w0"])
    put("a0", inp["rw_a0"])
    put("k_k", inp["rw_k_k"])
    put("k_a", inp["rw_k_a"])
    put("r_k", np.asarray(inp["rw_r_k"]).reshape(-1, 512))
    put("ln_w", inp["rw_ln_w"])
    put("ln_b", inp["rw_ln_b"])
    put("post_mix_g", inp["post_mix_g"])
    put("pre_ff_g", inp["pre_ff_g"])
    put("post_ff_g", inp["post_ff_g"])
    sh["vecs"] = vecs
    sh["consts"] = _consts()
    rel = f(inp["ca_rel_bias"])[:nl]
    sh["relr"] = np.ascontiguousarray(rel[:, ::-1, :].transpose(0, 2, 1))
    return sh


def prep_core(inp, b, S, nl=NL):
    d = {}
    d["xT"] = np.ascontiguousarray(np.asarray(inp["x"], dtype=np.float32)[b, :S].T)
    d["pT"] = np.ascontiguousarray(np.asarray(inp["p"], dtype=np.float32)[:nl, b, :S].transpose(0, 2, 1))
    d["pos"] = np.ascontiguousarray(np.asarray(inp["positions"]).astype(np.int32)[b:b + 1, :S])
    return d


_NC_CACHE = {}
PHASES = []


def kernel(**inputs):
    key = (SEQ, NL)
    if key not in _NC_CACHE:
        _NC_CACHE[key] = build(SEQ, NL)
    nc = _NC_CACHE[key]
    sh = prep_shared(inputs)
    in_maps = []
    for b in range(NB):
        m = dict(sh)
        m.update(prep_core(inputs, b, SEQ))
        in_maps.append(m)
    res = run_bass_kernel_spmd(nc, in_maps, core_ids=list(range(NB)))
    out = np.stack([np.asarray(r["outT"]).T for r in res.results], axis=0)
    return np.ascontiguousarray(out.astype(np.float32))
```

```python
import contextlib
import numpy as np
import ml_dtypes
import concourse.bass as bass
import concourse.mybir as mybir
from concourse.bass_utils import run_bass_kernel_spmd

F32 = mybir.dt.float32
BF16 = mybir.dt.bfloat16
I32 = mybir.dt.int32
AF = mybir.ActivationFunctionType
ALU = mybir.AluOpType

D = 1024
DFF = 4096
DPLE = 256
INC = 6848
NL = 2
SEQ = 4096
NB = 8
T = 256
EPS = 1e-6
LN_EPS = 64e-5
CC = float(np.exp(-0.5))
MLA_SCALE = float(192 ** -0.5)
TWO_PI = 6.2831845
NEG = -30000.0

VC = {}
_o = 0
for _n, _k in [("pre_mix_g", 8), ("q_g", 2), ("kv_g", 1), ("mu", 14), ("w0", 4), ("a0", 4), ("k_k", 4),
               ("k_a", 4), ("r_k", 4), ("ln_w", 4), ("ln_b", 4), ("post_mix_g", 8), ("pre_ff_g", 8),
               ("post_ff_g", 8)]:
    VC[_n] = _o
    _o += _k
NV = _o
CI_ID, CI_J, CI_ONE, CI_BLK, CI_SU, CI_U, CI_SL = [i * 128 for i in range(7)]
CI_INVF = 7 * 128
CI_SGN = CI_INVF + 1
NCONST = CI_SGN + 1

WSPEC = {
    "w_in": (1024, INC), "w_in_sw": (1024, 64), "w_uq": (256, 768), "w_uq_sw": (256, 256),
    "w_ukT": (512, 128), "w_ukv": (128, 1024), "rw_w_up": (64, 512), "rw_a_up": (64, 512),
    "rw_g_up": (128, 512), "w_branch": (1536, 1024), "w_out": (1024, 1024), "w_ff1": (1024, 4096),
    "w_ff2": (4096, 1024), "w_ple_gate": (1024, 1024), "w_ple_proj": (256, 1024),
}
WORDER = ["w_in", "w_in_sw", "w_uq", "w_uq_sw", "w_ukT", "w_ukv", "rw_w_up", "rw_a_up", "rw_g_up",
          "w_branch", "w_out", "w_ff1", "w_ff2", "w_ple_gate", "w_ple_proj"]


class Dep:
    __slots__ = ("w", "r", "al")

    def __init__(self):
        self.w = None
        self.r = {}
        self.al = []


class Buf:
    def __init__(self, ap, d=None):
        self.ap = ap
        self.d = d if d is not None else Dep()

    def __getitem__(self, k):
        return self.ap[k]


def v3(ap, a):
    return ap.rearrange("p (a b) -> p a b", a=a)


ENGS = ("pe", "act", "dve", "pool", "sp")
NDS = 8


class Builder:
    def __init__(self, nc, es):
        self.nc = nc
        self.ops = {e: [] for e in ENGS}
        self.cnt = {e: 0 for e in ENGS}
        self.waited = {e: {} for e in ENGS}
        self.sems = []
        self.semid = {}
        for e in ENGS:
            self.semid[e] = len(self.sems)
            self.sems.append(es.enter_context(nc.semaphore("s_" + e)))
        self.dsem = {}
        self.dcnt = {}
        for q in ("sp", "pool", "act"):
            self.dsem[q] = []
            self.dcnt[q] = 0
            for i in range(NDS):
                self.dsem[q].append(len(self.sems))
                self.sems.append(es.enter_context(nc.semaphore("d_%s%d" % (q, i))))

    def _waits(self, eng, reads, writes):
        need = {}

        def add(sid, val):
            if need.get(sid, 0) < val:
                need[sid] = val

        for d in reads:
            if d.w is not None:
                add(*d.w)
        for d in writes:
            if d.w is not None:
                add(*d.w)
            for sid, val in d.r.items():
                add(sid, val)
            for a in d.al:
                if a.w is not None:
                    add(*a.w)
                for sid, val in a.r.items():
                    add(sid, val)
        out = []
        wd = self.waited[eng]
        pe_sid = self.semid["pe"]
        for sid, val in need.items():
            if eng == "pe" and sid == pe_sid:
                continue
            if wd.get(sid, 0) >= val:
                continue
            wd[sid] = val
            out.append((sid, val))
        return out

    def _mark(self, reads, writes, sid, val):
        for d in reads:
            if d.r.get(sid, 0) < val:
                d.r[sid] = val
        for d in writes:
            d.w = (sid, val)
            d.r = {}

    def op(self, eng, fn, R=(), W=()):
        R = [x.d if isinstance(x, Buf) else x for x in R]
        W = [x.d if isinstance(x, Buf) else x for x in W]
        ws = self._waits(eng, R, W)
        self.cnt[eng] += 1
        sid = self.semid[eng]
        self.ops[eng].append((ws, fn, sid, 1))
        self._mark(R, W, sid, self.cnt[eng])

    def dma(self, q, out, in_, R=(), W=(), **kw):
        R = [x.d if isinstance(x, Buf) else x for x in R]
        W = [x.d if isinstance(x, Buf) else x for x in W]
        ws = self._waits(q, R, W)
        i = self.dcnt[q]
        self.dcnt[q] += 1
        sid = self.dsem[q][i % NDS]
        val = 16 * (i // NDS + 1)
        if val > 16 and self.waited[q].get(sid, 0) < val - 16:
            self.waited[q][sid] = val - 16
            ws.append((sid, val - 16))
        self.ops[q].append((ws, lambda e, o=out, i_=in_, k=kw: e.dma_start(out=o, in_=i_, **k), sid, 16))
        self._mark(R, W, sid, val)

    def mm(self, out, lhsT, rhs, R, W, start=True, stop=True):
        self.pes = getattr(self, "pes", 0) + (2 if lhsT.dtype == F32 else 1)
        self.op("pe", lambda e: e.matmul(out, lhsT, rhs, start=start, stop=stop, skip_group_check=True), R, W)

    def tr(self, out, in_, ident, R, W):
        self.pes = getattr(self, "pes", 0) + 1
        self.op("pe", lambda e: e.transpose(out, in_, ident), R, W)

    def A(self, out, in_, func, R, W, scale=None, bias=None):
        kw = {}
        if scale is not None:
            kw["scale"] = scale
        if bias is not None:
            kw["bias"] = bias
        self.op("act", lambda e: e.activation(out, in_, func, **kw), R, W)

    def tt(self, eng, out, a, b, op, R, W):
        self.op(eng, lambda e: e.tensor_tensor(out, a, b, op), R, W)

    def ts(self, eng, out, a, s1, op0, R, W, s2=None, op1=None):
        if op1 is None:
            self.op(eng, lambda e: e.tensor_scalar(out, a, s1, None, op0), R, W)
        else:
            self.op(eng, lambda e: e.tensor_scalar(out, a, s1, s2, op0, op1), R, W)

    def stt(self, eng, out, a, s, b, op0, op1, R, W):
        self.op(eng, lambda e: e.scalar_tensor_tensor(out, a, s, b, op0, op1), R, W)

    def cp(self, eng, out, in_, R, W):
        if eng == "act":
            self.op("act", lambda e: e.activation(out, in_, AF.Copy), R, W)
        else:
            self.op(eng, lambda e: e.tensor_copy(out, in_), R, W)

    def rcp(self, out, in_, R, W):
        self.op("dve", lambda e: e.reciprocal(out, in_), R, W)

    def ms(self, eng, ap, val, W):
        self.op(eng, lambda e: e.memset(ap, val), (), W)

    def emit(self, block):
        sems = self.sems
        B = self

        def run(e, name):
            for ws, fn, sid, inc in B.ops[name]:
                for s_, v_ in ws:
                    e.wait_ge(sems[s_], v_)
                fn(e).then_inc(sems[sid], inc)

        fin = []
        for en in ENGS:
            if self.cnt[en] > 0:
                fin.append((self.semid[en], self.cnt[en]))
        for q in ("sp", "pool", "act"):
            n = self.dcnt[q]
            for k in range(NDS):
                cntk = (n - k + NDS - 1) // NDS if n > k else 0
                if cntk > 0:
                    fin.append((self.dsem[q][k], 16 * cntk))

        @block.tensor
        def _(e):
            run(e, "pe")

        @block.scalar
        def _(e):
            run(e, "act")

        @block.vector
        def _(e):
            run(e, "dve")

        @block.gpsimd
        def _(e):
            run(e, "pool")

        @block.sync
        def _(e):
            run(e, "sp")
            for s_, v_ in fin:
                e.wait_ge(sems[s_], v_)


def build(S=SEQ, nl=NL, dbg=False, stage=99):
    NT = S // T
    NBLK = S // 128
    nc = bass.Bass("TRN2", target_bir_lowering=False)
    es = contextlib.ExitStack()
    es.__enter__()
    B = Builder(nc, es)

    def dram(name, shape, dt, kind):
        return nc.dram_tensor(name, list(shape), dt, kind=kind).ap()

    xT = dram("xT", [D, S], F32, "ExternalInput")
    pT = dram("pT", [nl, DPLE, S], F32, "ExternalInput")
    pos = dram("pos", [1, S], I32, "ExternalInput")
    vecs = dram("vecs", [nl, 128, NV], F32, "ExternalInput")
    consts = dram("consts", [128, NCONST], F32, "ExternalInput")
    relr = dram("relr", [nl, 8, 320], F32, "ExternalInput")
    wf = {}
    wb = {}
    wdep = {}
    for n in WORDER:
        r, c = WSPEC[n]
        wf[n] = dram(n, [nl, r, c], F32, "ExternalInput")
        wb[n] = dram(n + "_b", [nl, r, c], BF16, "Internal")
        for l in range(nl):
            wdep[(n, l)] = Dep()
    outT = dram("outT", [D, S], F32, "ExternalOutput")
    hscr = [dram("hscr%d" % i, [D, S], F32, "Internal") for i in range(max(nl - 1, 1))]
    hscr_d = [[Dep() for _ in range(NT)] for _ in range(max(nl - 1, 1))]
    ext = dram("ext", [nl, 8, 768], F32, "Internal")
    ext_d = [Dep() for _ in range(nl)]
    dbg_t = {}
    if dbg:
        for n in ("o_mla", "o_rw", "o_ca"):
            dbg_t[n] = dram("dbg_" + n, [512, S], BF16, "ExternalOutput")

    def sb(name, shape, dt):
        return Buf(es.enter_context(nc.sbuf_tensor(name, list(shape), dt))[:])

    cst = sb("cst", [128, NCONST], F32)
    vec = [sb("vec%d" % l, [128, NV], F32) for l in range(nl)]
    omka = [sb("omka%d" % l, [128, 4], F32) for l in range(nl)]
    cbf = sb("cbf", [128, 4 * 128], BF16)
    ident_bf = cbf[:, 0:128]
    J_bf = cbf[:, 128:256]
    ones_bf = cbf[:, 256:384]
    blk_bf = cbf[:, 384:512]
    ident_f = cst[:, CI_ID:CI_ID + 128]
    m_su = cst[:, CI_SU:CI_SU + 128]
    m_u = cst[:, CI_U:CI_U + 128]
    m_sl = cst[:, CI_SL:CI_SL + 128]

    uT = sb("uT", [128, 8 * T], BF16)
    uT3 = v3(uT.ap, 8)
    NW = 3
    wsl = [sb("wsl%d" % i, [128, 4096], BF16) for i in range(NW)]
    wsl_i = [0]
    Kc = sb("Kc", [128, S], BF16)
    Kr = sb("Kr", [64, S], BF16)
    Vc = sb("Vc", [128, S], BF16)
    Vc3 = v3(Vc.ap, NBLK)
    Kc_d = [Dep() for _ in range(NT)]
    CK = sb("CK", [128, 4 * 1024], BF16)
    CK3 = v3(CK.ap, 4)
    CV = sb("CV", [128, 8 * 512], BF16)
    CV3 = v3(CV.ap, 8)
    CK_d = [Dep() for _ in range(4)]
    CV_d = [Dep() for _ in range(8)]
    Xb = sb("Xb", [128, 40 * 128], BF16)
    Xb3 = v3(Xb.ap, 40)
    o_mla = sb("o_mla", [128, 4 * T], BF16)
    o_rw = sb("o_rw", [128, 4 * T], BF16)
    o_ca = sb("o_ca", [128, 4 * T], BF16)
    Sst = sb("Sst", [128, 256], F32)
    zlast = sb("zlast", [128, 16], F32)
    small = sb("small", [128, 64], F32)

    AW = 28900
    arena = es.enter_context(nc.sbuf_tensor("arena", [128, AW], F32))[:]
    abufs = []
    aoff = {}

    def al(phase, name, n, dt):
        words = (n + 1) // 2 if dt == BF16 else n
        o = aoff.get(phase, 0)
        aoff[phase] = o + words
        assert o + words <= AW, (phase, name, o + words)
        ap = arena[:, o:o + words]
        if dt == BF16:
            ap = ap.bitcast(BF16)[:, 0:n]
        b = Buf(ap)
        for (ph2, o2, e2, b2) in abufs:
            if ph2 != phase and o2 < o + words and o < e2:
                b.d.al.append(b2.d)
                b2.d.al.append(b.d)
        abufs.append((phase, o, o + words, b))
        return b

    def al_all(name, n, dt):
        words = (n + 1) // 2 if dt == BF16 else n
        o = max([aoff.get(p, 0) for p in ("mla", "ca", "rw", "ffn")])
        for p in ("mla", "ca", "rw", "ffn"):
            assert aoff.get(p, 0) <= o
            aoff[p] = o + words
        ap = arena[:, o:o + words]
        if dt == BF16:
            ap = ap.bitcast(BF16)[:, 0:n]
        return Buf(ap)

    sqb = al_all("sqb", 8 * T, BF16)
    sqb3 = v3(sqb.ap, 8)
    rt = al_all("rt", T, F32)
    rt2 = al_all("rt2", T, F32)

    PS = [Buf(es.enter_context(nc.psum_tensor("ps%d" % i, [128, 512], F32))[:]) for i in range(8)]
    rot = {"d": 0, "n": 4}

    def psd():
        i = rot["d"] % rot["n"]
        rot["d"] = (i + 1) % rot["n"]
        return PS[i]

    block = es.enter_context(nc.Block())

    B.dma("sp", cst.ap, consts, W=[cst])
    for l in range(nl):
        B.dma("sp", vec[l].ap, vecs[l], W=[vec[l]])
    for l in range(nl):
        for n in WORDER:
            r, c = WSPEC[n]
            npc = (c + 2047) // 2048
            pc = c // npc
            assert pc * npc == c
            for i in range(npc):
                B.dma("pool", wb[n][l, :, i * pc:(i + 1) * pc], wf[n][l, :, i * pc:(i + 1) * pc], W=[wdep[(n, l)]])
    B.cp("dve", cbf[:, 0:512], cst[:, 0:512], [cst], [cbf])
    for l in range(nl):
        ka = vec[l][:, VC["k_a"]:VC["k_a"] + 4]
        B.ts("dve", omka[l].ap, ka, -1.0, ALU.mult, [vec[l]], [omka[l]], s2=1.0, op1=ALU.add)

    def vcol(l, name, i):
        c = VC[name] + i
        return vec[l][:, c:c + 1]

    def wload(name, l, dram_ap, shape, prt=None):
        s = wsl[wsl_i[0]]
        wsl_i[0] = (wsl_i[0] + 1) % NW
        n = int(np.prod(shape[1:]))
        p0, p1 = prt if prt is not None else (0, shape[0])
        view = s.ap[p0:p1, 0:n]
        if len(shape) == 3:
            view = view.rearrange("p (a b) -> p a b", a=shape[1])
        elif len(shape) == 4:
            view = view.rearrange("p (a b c) -> p a b c", a=shape[1], b=shape[2])
        B.dma("sp", view, dram_ap, R=[wdep[(name, l)]], W=[s])
        return view, s

    def wcols(name, l, c0, n, kc):
        ap = wb[name][l, 0:kc * 128, c0:c0 + n].rearrange("(k p) n -> p k n", p=128)
        return wload(name, l, ap, [128, kc, n])

    def rms(src3, nk, ktot, l, gname, out_fn, R, Wd, eps=EPS, ps=None):
        B.A(sqb3[:, 0:nk, :], src3, AF.Square, R, [sqb])
        p = psd()
        for k in range(nk):
            B.mm(p[:, 0:T], ones_bf, sqb3[:, k, :], [sqb, cbf], [p], start=(k == 0), stop=(k == nk - 1))
        B.A(rt.ap, p[:, 0:T], AF.Sqrt, [p], [rt], scale=1.0 / ktot, bias=eps_ap(eps))
        B.rcp(rt2.ap, rt.ap, [rt], [rt2])

    eps_t = sb("eps_t", [128, 4], F32)
    B.ms("dve", eps_t[:, 0:1], EPS, [eps_t])
    B.ms("dve", eps_t[:, 1:2], LN_EPS, [eps_t])
    B.ms("dve", eps_t[:, 2:3], 0.0, [eps_t])
    B.ms("dve", eps_t[:, 3:4], 0.25, [eps_t])

    def eps_ap(e):
        return eps_t[:, 0:1] if e == EPS else eps_t[:, 1:2]

    zq = al("mla", "zq", 2 * T, F32)
    zq3 = v3(zq.ap, 2)
    zqn = al("mla", "zqn", 2 * T, BF16)
    zqn3 = v3(zqn.ap, 2)
    qn = [al("mla", "qn%d" % i, T, BF16) for i in range(2)]
    Qabs = al("mla", "Qabs", 4 * T, BF16)
    Qabs3 = v3(Qabs.ap, 4)
    Qrope = al("mla", "Qrope", 4 * T, BF16)
    Qrope3 = v3(Qrope.ap, 4)
    zkv = al("mla", "zkv", T, F32)
    cosT = al("mla", "cosT", T, F32)
    sinT = al("mla", "sinT", T, F32)
    posi = al("mla", "posi", T, F32)
    posf = al("mla", "posf", T, F32)
    tq = al("mla", "tq", T, F32)
    tq2 = al("mla", "tq2", T, F32)
    rp1 = al("mla", "rp1", T, F32)
    rp2 = al("mla", "rp2", T, F32)
    PTm = [al("mla", "PTm%d" % i, 2 * T, BF16) for i in range(2)]
    rinv = al("mla", "rinv", 2 * T, F32)
    On = al("mla", "On", 2 * T, BF16)
    Qs = al("rw", "Qs", 4 * T, BF16)
    Qs3 = v3(Qs.ap, 4)
    PTc = [al("rw", "PTc%d" % i, 512, BF16) for i in range(2)]
    rinvc = al("rw", "rinvc", 512, F32)
    hT = al("ffn", "hT", 8 * T, F32)
    hT3 = v3(hT.ap, 8)
    mo = al("ffn", "mo", 8 * T, F32)
    mo3 = v3(mo.ap, 8)
    aT = al("ffn", "aT", 32 * T, BF16)
    aT3 = v3(aT.ap, 32)
    rl = [al("ffn", "rl%d" % i, T, F32) for i in range(2)]
    mrg = al("ffn", "mrg", 8 * T, F32)
    mrg3 = v3(mrg.ap, 8)
    mrgb = al("ffn", "mrgb", 8 * T, BF16)
    mrgb3 = v3(mrgb.ap, 8)
    gsb = [al("ffn", "gsb%d" % i, T, F32) for i in range(2)]
    tmpf = [al("ffn", "tmpf%d" % i, T, F32) for i in range(2)]
    pb = sb("pb", [128, 2 * T], BF16)
    pb3 = v3(pb.ap, 2)
    zb = [al("rw", "zb%d" % i, T + 2, F32) for i in range(2)]
    dd = al("rw", "dd", T, F32)
    zrs = [al("rw", "zr%d" % i, T, F32) for i in range(2)]
    zks = [al("rw", "zk%d" % i, T, F32) for i in range(2)]
    zs12 = zrs[0]
    zs13 = zks[0]
    zvs = [al("rw", "zv%d" % i, T, F32) for i in range(2)]
    txw = al("rw", "txw", T, BF16)
    xab = al("rw", "xab", T, BF16)
    sgb = al("rw", "sgb", T, BF16)
    tmps = []
    for i_ in range(2):
        tmp = {n: al("rw", n + str(i_), T, F32) for n in ("sgw", "cs", "csc", "dC", "E1", "aa", "kk", "ssm", "rs", "tb", "tc", "km")}
        tmp["csx"] = tmp["cs"]
        tmp["E3"] = tmp["cs"]
        tmp["E2"] = tmp["csc"]
        tmp["E4"] = tmp["dC"]
        tmp["kkn"] = tmp["kk"]
        tmps.append(tmp)
    kk2s = [al("rw", "kk2%d" % i, T, BF16) for i in range(2)]
    rkrs = [al("rw", "rkr%d" % i, T, BF16) for i in range(2)]
    vbfs = [al("rw", "vbf%d" % i, T, BF16) for i in range(2)]
    bhs = [al("rw", "bh%d" % i, T, BF16) for i in range(2)]
    khs = [al("rw", "kh%d" % i, T, BF16) for i in range(2)]
    aTt = al("rw", "aTt", 4 * T, BF16)
    bTt = al("rw", "bTt", 4 * T, BF16)
    kTt = al("rw", "kTt", 4 * T, BF16)
    rTt = al("rw", "rTt", 4 * T, BF16)
    aT_3, bT_3, kT_3, rT_3 = (v3(x.ap, 4) for x in (aTt, bTt, kTt, rTt))
    TM = al("rw", "TM", 3 * 2 * 4 * 128, BF16)
    TM5 = TM.ap.rearrange("p (j c h n) -> p j c h n", j=3, c=2, h=4)
    AM = al("rw", "AM", 2 * 4 * 4 * 2 * 128, BF16)
    AM6 = AM.ap.rearrange("p (c h t e n) -> p c h t e n", c=2, h=4, t=4, e=2)
    AM_d = [[Dep() for _ in range(4)] for _ in range(2)]
    for cc_ in range(2):
        for hh_ in range(4):
            AM_d[cc_][hh_].al = AM.d.al
    Qd = [al("rw", "Qd%d" % i, 4 * 128, BF16) for i in range(2)]
    QTd = [al("rw", "QTd%d" % i, 4 * 128, BF16) for i in range(2)]
    Xd = [al("rw", "Xd%d" % i, 4 * 128, F32) for i in range(2)]
    Xbbs = [al("rw", "Xbb%d" % i, 4 * 128, BF16) for i in range(2)]
    yb = al("rw", "y", 4 * T, F32)
    y3 = v3(yb.ap, 4)
    bonus = al("rw", "bonus", 4 * T, F32)
    bonus3 = v3(bonus.ap, 4)
    gbuf = al("rw", "g", 4 * T, BF16)
    g3 = v3(gbuf.ap, 4)
    Smid = al("rw", "Smid", 256, F32)
    Smb = al("rw", "Smb", 256, BF16)
    W0b = al("rw", "W0b", 512, BF16)
    Ut = al("rw", "Ut", 512, BF16)
    lwa = al("rw", "lwa", 512, BF16)
    lwg = al("rw", "lwg", 512, BF16)
    ybf = al("rw", "ybf", T, BF16)
    yc = al("rw", "yc", T, F32)
    ysq = al("rw", "ysq", T, BF16)
    sd = al("rw", "sd", T, F32)
    t1 = al("rw", "t1", T, F32)
    t2 = al("rw", "t2", T, F32)

    def layer(l):
        src = xT if l == 0 else hscr[l - 1]
        dst = outT if l == nl - 1 else hscr[l]
        src_d = None if l == 0 else hscr_d[l - 1]
        dst_d = None if l == nl - 1 else hscr_d[l]
        B.ms("dve", Sst.ap, 0.0, [Sst])
        B.ms("dve", zlast.ap, 0.0, [zlast])
        etap = mo.ap[0:8, 0:768]
        rl_ap = mo.ap[0:8, 768:768 + 320]
        B.dma("sp", rl_ap, relr[l], W=[mo])
        B.cp("dve", etap[:, 383:703], rl_ap, [mo], [mo])
        B.cp("dve", etap[:, 0:383], rl_ap[:, 0:1].to_broadcast([8, 383]), [mo], [mo])
        B.cp("dve", etap[:, 703:768], rl_ap[:, 319:320].to_broadcast([8, 65]), [mo], [mo])
        B.dma("sp", ext[l], etap, R=[mo], W=[ext_d[l]])
        for r in range(5):
            for h in range(8):
                off = 639 - 128 * r - 127
                srcap = bass.AP(tensor=ext.tensor, offset=(l * 8 + h) * 768 + off, ap=[[1, 128], [1, 128]])
                B.dma("pool", Xb3[:, r * 8 + h, :], srcap, R=[ext_d[l]], W=[Xb])
        for h in range(8):
            B.ms("pool", Xb3[64:128, 0 * 8 + h, 64:128], NEG, [Xb])
            B.ms("pool", Xb3[0:64, 4 * 8 + h, 0:64], NEG, [Xb])

        for tt in range(NT):
            tile(l, tt, src, dst, src_d, dst_d)

    def mark(name, l, tt):
        PHASES.append((name, l, tt, getattr(B, "pes", 0)))

    def tile(l, tt, src, dst, src_d, dst_d):
        t0 = tt * T
        mark("start", l, tt)
        R_src = [] if src_d is None else [src_d[tt]]
        if tt == 0:
            B.dma("sp", mo3, src[:, t0:t0 + T].rearrange("(k p) t -> p k t", p=128), R=R_src, W=[mo])
        B.dma("pool", pb3, pT[l, :, t0:t0 + T].rearrange("(k p) t -> p k t", p=128), W=[pb])
        rot["n"] = 8
        rms(mo3, 8, D, l, "pre_mix_g", None, [mo], None)
        for k in range(8):
            B.stt("dve", uT3[:, k, :], mo3[:, k, :], vcol(l, "pre_mix_g", k), rt2.ap, ALU.mult, ALU.mult,
                  [mo, vec[l], rt2], [uT])

        def fin():
            W_dst = [] if dst_d is None else [dst_d[tt]]
            B.dma("sp", dst[:, t0:t0 + T].rearrange("(k p) t -> p k t", p=128), hT3, R=[hT], W=W_dst)

        if stage <= 0:
            return fin()

        def zmm(p, wv, ws, c0, n, M=None):
            for k in range(8):
                B.mm(p[0:n, 0:T], wv[:, k, c0:c0 + n], uT3[:, k, :], [ws, uT], [p], start=(k == 0), stop=(k == 7))

        mark("mla", l, tt)
        wv, ws = wcols("w_in", l, 0, 448, 8)
        wsw, wsws = wcols("w_in_sw", l, 0, 64, 8)
        for c in range(2):
            p = psd()
            zmm(p, wv, ws, c * 128, 128)
            B.cp("act", zq3[:, c, :], p[:, 0:T], [p], [zq])
        p = psd()
        zmm(p, wv, ws, 256, 128)
        B.cp("act", zkv.ap, p[:, 0:T], [p], [zkv])
        B.dma("sp", posi.ap[0:64, :].bitcast(I32), pos[0:1, t0:t0 + T].to_broadcast([64, T]), W=[posi])
        B.cp("dve", posf[0:64, :], posi.ap[0:64, :].bitcast(I32), [posi], [posf])
        B.ts("dve", tq[0:64, :], posf[0:64, :], cst[0:64, CI_INVF:CI_INVF + 1], ALU.mult, [posf, cst], [tq],
             s2=float(1.0 / (2 * np.pi)), op1=ALU.mult)
        MAGIC = 12582912.0
        for (dstb, shift) in ((sinT, 0.0), (cosT, 0.25)):
            if shift != 0.0:
                B.ts("dve", tq2[0:64, :], tq[0:64, :], shift, ALU.add, [tq], [tq2])
                srcq = tq2
            else:
                srcq = tq
            B.ts("dve", rp1[0:64, :], srcq[0:64, :], MAGIC, ALU.add, [srcq], [rp1], s2=MAGIC, op1=ALU.subtract)
            B.tt("dve", rp2[0:64, :], srcq[0:64, :], rp1[0:64, :], ALU.subtract, [srcq, rp1], [rp2])
            if shift == 0.0:
                B.A(dstb[0:64, :], rp2[0:64, :], AF.Sin, [rp2, cst], [dstb], scale=cst[0:64, CI_SGN:CI_SGN + 1])
            else:
                B.A(dstb[0:64, :], rp2[0:64, :], AF.Sin, [rp2], [dstb], scale=TWO_PI)
        p1 = psd()
        zmm(p1, wv, ws, 384, 64)
        p2 = psd()
        zmm(p2, wsw, wsws, 0, 64)
        B.tt("dve", rp1[0:64, :], p1[0:64, 0:T], cosT[0:64, :], ALU.mult, [p1, cosT], [rp1])
        B.tt("dve", rp2[0:64, :], p2[0:64, 0:T], sinT[0:64, :], ALU.mult, [p2, sinT], [rp2])
        B.tt("dve", Kr[0:64, t0:t0 + T], rp1[0:64, :], rp2[0:64, :], ALU.add, [rp1, rp2], [Kc_d[tt]])
        rms(zkv.ap.rearrange("p (a b) -> p a b", a=1), 1, 128, l, "kv_g", None, [zkv], None)
        B.stt("dve", Kc[:, t0:t0 + T], zkv.ap, vcol(l, "kv_g", 0), rt2.ap, ALU.mult, ALU.mult, [zkv, vec[l], rt2],
              [Kc_d[tt]])
        pt = PS[7]
        ptb = pt.ap.bitcast(BF16)
        for i in range(2):
            B.tr(ptb[:, i * 128:(i + 1) * 128], Kc[:, t0 + i * 128:t0 + (i + 1) * 128], ident_bf, [Kc_d[tt], cbf], [pt])
        B.cp("act", Vc[:, (2 * tt) * 128:(2 * tt + 2) * 128], ptb[:, 0:256], [pt], [Kc_d[tt]])
        rms(zq3, 2, 256, l, "q_g", None, [zq], None)
        for c in range(2):
            B.stt("dve", zqn3[:, c, :], zq3[:, c, :], vcol(l, "q_g", c), rt2.ap, ALU.mult, ALU.mult,
                  [zq, vec[l], rt2], [zqn])
        wq = wb["w_uq"][l].rearrange("(k p) n -> p k n", p=128)
        wqv, wqs = wload("w_uq", l, wq, [128, 2, 768])
        wqsw = wb["w_uq_sw"][l].rearrange("(k p) n -> p k n", p=128)
        wqswv, wqsws = wload("w_uq_sw", l, wqsw, [128, 2, 256])
        wkt = wb["w_ukT"][l].rearrange("(h p) n -> p h n", p=128)
        wktv, wkts = wload("w_ukT", l, wkt, [128, 4, 128])
        for h in range(4):
            p = psd()
            for k in range(2):
                B.mm(p[:, 0:T], wqv[:, k, h * 192:h * 192 + 128], zqn3[:, k, :], [wqs, zqn], [p], start=(k == 0),
                     stop=(k == 1))
            q_ = qn[h % 2]
            B.cp("act", q_.ap, p[:, 0:T], [p], [q_])
            p = psd()
            B.mm(p[:, 0:T], wktv[:, h, :], q_.ap, [wkts, q_], [p])
            B.cp("act", Qabs3[:, h, :], p[:, 0:T], [p], [Qabs])
            p1 = psd()
            for k in range(2):
                B.mm(p1[0:64, 0:T], wqv[:, k, h * 192 + 128:h * 192 + 192], zqn3[:, k, :], [wqs, zqn], [p1],
                     start=(k == 0), stop=(k == 1))
            p2 = psd()
            for k in range(2):
                B.mm(p2[0:64, 0:T], wqswv[:, k, h * 64:(h + 1) * 64], zqn3[:, k, :], [wqsws, zqn], [p2],
                     start=(k == 0), stop=(k == 1))
            B.tt("dve", rp1[0:64, :], p1[0:64, 0:T], cosT[0:64, :], ALU.mult, [p1, cosT], [rp1])
            B.tt("dve", rp2[0:64, :], p2[0:64, 0:T], sinT[0:64, :], ALU.mult, [p2, sinT], [rp2])
            B.tt("dve", Qrope3[0:64, h, :], rp1[0:64, :], rp2[0:64, :], ALU.add, [rp1, rp2], [Qrope])
        rot["n"] = 4
        mark("mla_attn", l, tt)
        wkv = wb["w_ukv"][l]
        wkvv, wkvs = wload("w_ukv", l, wkv, [128, 1024])
        nkb = 2 * (tt + 1)
        Kdeps = [Kc_d[j // 2] for j in range(nkb)]
        for hp in range(2):
            Ob, Sb = PS[4], PS[5]
            O3 = v3(Ob.ap, 2)
            S3 = v3(Sb.ap, 2)
            def m_scores(j):
                jd = j - 2 * tt
                q0 = max(jd, 0) * 128
                ps_ = PS[j % 2 + 2]
                ps3 = v3(ps_.ap, 2)
                for hh in range(2):
                    h = 2 * hp + hh
                    B.mm(ps3[:, hh, q0:T], Kc[:, j * 128:(j + 1) * 128], Qabs3[:, h, q0:T], [Kdeps[j], Qabs], [ps_],
                         start=True, stop=False)
                    B.mm(ps3[:, hh, q0:T], Kr[0:64, j * 128:(j + 1) * 128], Qrope3[0:64, h, q0:T],
                         [Kdeps[j], Qrope], [ps_], start=False, stop=True)

            def m_rest(j):
                jd = j - 2 * tt
                q0 = max(jd, 0) * 128
                ps_ = PS[j % 2 + 2]
                ps3 = v3(ps_.ap, 2)
                PT = PTm[j % 2]
                PT3 = v3(PT.ap, 2)
                B.A(PT3[:, :, q0:T], ps3[:, :, q0:T], AF.Exp, [ps_], [PT], scale=MLA_SCALE)
                if jd >= 0:
                    B.ms("pool", PT3[64:128, :, q0:q0 + 64], 0.0, [PT])
                for hh in range(2):
                    B.mm(O3[:, hh, q0:T], Vc3[:, j, :], PT3[:, hh, q0:T], [Kdeps[j], PT], [Ob],
                         start=(j == 0 and hh == 0), stop=(j == nkb - 1))
                for hh in range(2):
                    B.mm(S3[:, hh, q0:T], ones_bf, PT3[:, hh, q0:T], [cbf, PT], [Sb],
                         start=(j == 0 and hh == 0), stop=(j == nkb - 1))

            m_scores(0)
            for j in range(nkb):
                if j + 1 < nkb:
                    m_scores(j + 1)
                m_rest(j)
            B.rcp(rinv.ap, Sb.ap, [Sb], [rinv])
            B.tt("dve", On.ap, Ob.ap, rinv.ap, ALU.mult, [Ob, rinv], [On])
            On3 = v3(On.ap, 2)
            for hh in range(2):
                h = 2 * hp + hh
                p = psd()
                B.mm(p[:, 0:T], wkvv[:, h * 256 + 128:h * 256 + 256], On3[:, hh, :], [wkvs, On], [p])
                B.cp("act", o_mla[:, h * T:(h + 1) * T], p[:, 0:T], [p], [o_mla])

        if stage <= 1:
            return fin()
        mark("ca", l, tt)
        wv, ws = wcols("w_in", l, 2240, 512, 8)
        for c in range(4):
            p = psd()
            zmm(p, wv, ws, c * 128, 128)
            B.A(Qs3[:, c, :], p[:, 0:T], AF.Copy, [p], [Qs], scale=0.125)
        wv, ws = wcols("w_in", l, 2752, 512, 8)
        sl0 = (2 * tt) % 8
        for c in range(4):
            p = psd()
            zmm(p, wv, ws, c * 128, 128)
            B.cp("act", CK3[:, c, sl0 * 128:sl0 * 128 + T], p[:, 0:T], [p], [CK_d[sl0 // 2]])
        wv, ws = wcols("w_in", l, 3264, 512, 8)
        for i in range(2):
            p = psd()
            for k in range(8):
                B.mm(p.ap, uT3[:, k, i * 128:(i + 1) * 128], wv[:, k, :], [uT, ws], [p], start=(k == 0), stop=(k == 7))
            B.cp("act", CV3[:, sl0 + i, :], p.ap, [p], [CV_d[sl0 + i]])
        def ca_attn():
            for i in range(2):
                qb = 2 * tt + i
                Ob, Sb = PS[4], PS[5]
                O3 = v3(Ob.ap, 4)
                S3 = v3(Sb.ap, 4)
                bl = list(range(max(0, qb - 4), qb + 1))
                units = [(b, e) for b in bl for e in range(2)]

                def c_scores(b, e):
                    r = qb - b
                    slot = b % 8
                    ps_ = PS[2 + e]
                    ps3 = v3(ps_.ap, 4)
                    pb_ = e * 64
                    for ch in range(4):
                        h = ch * 2 + e
                        B.mm(ps3[:, ch, :], CK3[pb_:pb_ + 64, ch, slot * 128:(slot + 1) * 128],
                             Qs3[pb_:pb_ + 64, ch, i * 128:(i + 1) * 128], [CK_d[slot // 2], Qs], [ps_],
                             start=True, stop=False)
                        B.mm(ps3[:, ch, :], Xb3[:, r * 8 + h, :], J_bf, [Xb, cbf], [ps_], start=False, stop=True)

                def c_rest(b, e):
                    slot = b % 8
                    ps_ = PS[2 + e]
                    PT = PTc[e]
                    PT3 = v3(PT.ap, 4)
                    pb_ = e * 64
                    B.A(PT.ap, ps_.ap, AF.Exp, [ps_], [PT])
                    for ch in range(4):
                        h = ch * 2 + e
                        first = (b == bl[0] and ch == 0)
                        B.mm(O3[pb_:pb_ + 64, ch, :], CV3[:, slot, h * 64:(h + 1) * 64], PT3[:, ch, :],
                             [CV_d[slot], PT], [Ob], start=first, stop=True)
                    for ch in range(4):
                        first = (b == bl[0] and ch == 0)
                        B.mm(S3[pb_:pb_ + 64, ch, :], ones_bf[:, 0:64], PT3[:, ch, :], [cbf, PT], [Sb],
                             start=first, stop=True)

                c_scores(*units[0])
                for ui, u_ in enumerate(units):
                    if ui + 1 < len(units):
                        c_scores(*units[ui + 1])
                    c_rest(*u_)
                    yield 1
                B.rcp(rinvc.ap, Sb.ap, [Sb], [rinvc])
                B.tt("dve", v3(o_ca.ap, 4)[:, :, i * 128:(i + 1) * 128], O3, v3(rinvc.ap, 4), ALU.mult, [Ob, rinvc],
                     [o_ca])


        if stage <= 2:
            for _ in ca_attn():
                pass
            return fin()
        mark("rw", l, tt)
        rwkv(l, tt, ca_attn())
        if dbg and l == 0 and tt == 0:
            for n, bsrc in (("aT", aTt), ("bT", bTt), ("kT", kTt), ("rT", rTt), ("y", yb), ("bonus", bonus), ("g", gbuf),
                            ("TM", TM), ("AM", AM), ("Sst", Sst), ("small", small)):
                if n not in dbg_t:
                    dbg_t[n] = dram("dbg_" + n, [128, bsrc.ap.shape[1]], bsrc.ap.dtype, "ExternalOutput")
                B.dma("sp", dbg_t[n], bsrc.ap, R=[bsrc] + ([AM_d[c_][h_] for c_ in range(2) for h_ in range(4)] if n == "AM" else []))
        if stage <= 3 or (stage >= 21 and stage <= 26):
            return fin()

        if dbg and l == 0:
            for n, bsrc in (("o_mla", o_mla), ("o_rw", o_rw), ("o_ca", o_ca)):
                B.dma("sp", dbg_t[n][:, t0:t0 + T].rearrange("(k p) t -> p k t", p=128), v3(bsrc.ap, 4), R=[bsrc])

        rot["n"] = 8
        mark("merge", l, tt)
        obr = (o_mla, o_rw, o_ca)
        for cg in range(2):
            for n in range(3):
                gv, gs = wcols("w_in", l, 3776 + n * 1024 + cg * 512, 512, 8)
                bap = wb["w_branch"][l, n * 512:(n + 1) * 512, cg * 512:(cg + 1) * 512].rearrange(
                    "(k p) n -> p k n", p=128)
                bv, bs = wload("w_branch", l, bap, [128, 4, 512])
                ob3 = v3(obr[n].ap, 4)
                for cl in range(4):
                    c = cg * 4 + cl
                    pg = psd()
                    zmm(pg, gv, gs, cl * 128, 128)
                    gb = gsb[(c * 3 + n) % 2]
                    B.A(gb.ap, pg[:, 0:T], AF.Sigmoid, [pg], [gb])
                    py = psd()
                    for k in range(4):
                        B.mm(py[:, 0:T], bv[:, k, cl * 128:(cl + 1) * 128], ob3[:, k, :], [bs, obr[n]], [py],
                             start=(k == 0), stop=(k == 3))
                    if n == 0:
                        B.tt("dve", mrg3[:, c, :], py[:, 0:T], gb.ap, ALU.mult, [py, gb], [mrg])
                    else:
                        tf = tmpf[(c * 3 + n) % 2]
                        B.tt("dve", tf.ap, py[:, 0:T], gb.ap, ALU.mult, [py, gb], [tf])
                        if n == 1:
                            B.tt("pool", mrg3[:, c, :], mrg3[:, c, :], tf.ap, ALU.add, [mrg, tf], [mrg])
                        else:
                            B.tt("pool", mrgb3[:, c, :], mrg3[:, c, :], tf.ap, ALU.add, [mrg, tf], [mrgb])
        B.dma("sp", hT3, src[:, t0:t0 + T].rearrange("(k p) t -> p k t", p=128), R=R_src, W=[hT])
        for cg in range(2):
            wv, ws = wcols("w_out", l, cg * 512, 512, 8)
            for cl in range(4):
                c = cg * 4 + cl
                p = psd()
                for k in range(8):
                    B.mm(p[:, 0:T], wv[:, k, cl * 128:(cl + 1) * 128], mrgb3[:, k, :], [ws, mrgb], [p], start=(k == 0),
                         stop=(k == 7))
                B.cp("act", mo3[:, c, :], p[:, 0:T], [p], [mo])
        rms(mo3, 8, D, l, "post_mix_g", None, [mo], None)
        for k in range(8):
            tf = tmpf[k % 2]
            B.stt("dve", tf.ap, mo3[:, k, :], vcol(l, "post_mix_g", k), rt2.ap, ALU.mult, ALU.mult,
                  [mo, vec[l], rt2], [tf])
            B.tt("pool", hT3[:, k, :], hT3[:, k, :], tf.ap, ALU.add, [hT, tf], [hT])
        mark("ffn", l, tt)
        rms(hT3, 8, D, l, "pre_ff_g", None, [hT], None)
        for k in range(8):
            B.stt("dve", uT3[:, k, :], hT3[:, k, :], vcol(l, "pre_ff_g", k), rt2.ap, ALU.mult, ALU.mult,
                  [hT, vec[l], rt2], [uT])
        for cg in range(8):
            wv, ws = wcols("w_ff1", l, cg * 512, 512, 8)
            for cl in range(4):
                j = cg * 4 + cl
                p = psd()
                zmm(p, wv, ws, cl * 128, 128)
                r_ = rl[j % 2]
                B.A(r_.ap, p[:, 0:T], AF.Relu, [p], [r_])
                B.tt("pool", aT3[:, j, :], r_.ap, r_.ap, ALU.mult, [r_], [aT])
        rot["n"] = 4
        for cg in range(2):
            accs = [PS[4 + i] for i in range(4)]
            for kg in range(4):
                wap = wb["w_ff2"][l, kg * 1024:(kg + 1) * 1024, cg * 512:(cg + 1) * 512].rearrange(
                    "(k p) n -> p k n", p=128)
                wv, ws = wload("w_ff2", l, wap, [128, 8, 512])
                for cl in range(4):
                    for kk in range(8):
                        B.mm(accs[cl][:, 0:T], wv[:, kk, cl * 128:(cl + 1) * 128], aT3[:, kg * 8 + kk, :], [ws, aT],
                             [accs[cl]], start=(kg == 0 and kk == 0), stop=(kg == 3 and kk == 7))
            for cl in range(4):
                B.cp("act", mo3[:, cg * 4 + cl, :], accs[cl][:, 0:T], [accs[cl]], [mo])
        rms(mo3, 8, D, l, "post_ff_g", None, [mo], None)
        for k in range(8):
            tf = tmpf[k % 2]
            B.stt("dve", tf.ap, mo3[:, k, :], vcol(l, "post_ff_g", k), rt2.ap, ALU.mult, ALU.mult,
                  [mo, vec[l], rt2], [tf])
            B.tt("pool", hT3[:, k, :], hT3[:, k, :], tf.ap, ALU.add, [hT, tf], [hT])
        rot["n"] = 8
        mark("ple", l, tt)
        if tt + 1 < NT:
            R_nx = [] if src_d is None else [src_d[tt + 1]]
            B.dma("sp", mo3, src[:, t0 + T:t0 + 2 * T].rearrange("(k p) t -> p k t", p=128), R=R_nx, W=[mo])
        B.cp("act", uT.ap, hT.ap, [hT], [uT])
        ppv, pps = wload("w_ple_proj", l, wb["w_ple_proj"][l].rearrange("(k p) n -> p k n", p=128), [128, 2, 1024])
        for cg in range(2):
            wv, ws = wcols("w_ple_gate", l, cg * 512, 512, 8)
            for cl in range(4):
                c = cg * 4 + cl
                pg = psd()
                zmm(pg, wv, ws, cl * 128, 128)
                gb = gsb[c % 2]
                B.A(gb.ap, pg[:, 0:T], AF.Sigmoid, [pg], [gb])
                pp = psd()
                for k in range(2):
                    B.mm(pp[:, 0:T], ppv[:, k, c * 128:(c + 1) * 128], pb3[:, k, :], [pps, pb], [pp], start=(k == 0),
                         stop=(k == 1))
                tf = tmpf[c % 2]
                B.tt("dve", tf.ap, pp[:, 0:T], gb.ap, ALU.mult, [pp, gb], [tf])
                B.tt("pool", hT3[:, c, :], hT3[:, c, :], tf.ap, ALU.add, [hT, tf], [hT])
        W_dst = [] if dst_d is None else [dst_d[tt]]
        B.dma("sp", dst[:, t0:t0 + T].rearrange("(k p) t -> p k t", p=128), hT3, R=[hT], W=W_dst)
        rot["n"] = 4

    def rwkv(l, tt, ca):
        zi = [0]

        def zchunk(wv, ws, c0, cidx, dest):
            p = psd()
            for k in range(8):
                B.mm(p[:, 0:T], wv[:, k, c0:c0 + 128] if c0 is not None else wv[:, k, :], uT3[:, k, :], [ws, uT], [p],
                     start=(k == 0), stop=(k == 7))
            z = zb[zi[0] % 2]
            zi[0] += 1
            B.cp("act", z[:, 1:T + 1], p[:, 0:T], [p], [z])
            B.cp("pool", z[:, 0:1], zlast[:, cidx:cidx + 1], [zlast], [z])
            B.tt("dve", dd.ap, z[:, 0:T], z[:, 1:T + 1], ALU.subtract, [z], [dd])
            B.stt("dve", dest.ap, dd.ap, vcol(l, "mu", cidx), z[:, 1:T + 1], ALU.mult, ALU.add, [dd, vec[l], z], [dest])
            B.cp("pool", zlast[:, cidx:cidx + 1], z[:, T:T + 1], [z], [zlast])

        wv, ws = wcols("w_in", l, 448 + 1536, 256, 8)
        zchunk(wv, ws, 0, 12, zs12)
        zchunk(wv, ws, 128, 13, zs13)
        B.A(txw[0:64, :], zs12[0:64, :], AF.Tanh, [zs12], [txw])
        B.cp("act", xab[64:128, :], zs12[64:128, :], [zs12], [xab])
        B.A(sgb.ap, zs13.ap, AF.Sigmoid, [zs13], [sgb])
        if stage == 21:
            return
        s_wa = lwa
        B.dma("sp", s_wa.ap[0:64, 0:512], wb["rw_w_up"][l], R=[wdep[("rw_w_up", l)]], W=[s_wa])
        B.dma("sp", s_wa.ap[64:128, 0:512], wb["rw_a_up"][l], R=[wdep[("rw_a_up", l)]], W=[s_wa])
        gups = lwg
        gupv = lwg.ap
        B.dma("sp", gupv, wb["rw_g_up"][l], R=[wdep[("rw_g_up", l)]], W=[lwg])
        sm = small

        def stageA(hp):
            qs = hp % 2
            s4 = wsl[wsl_i[0]]
            wsl_i[0] = (wsl_i[0] + 1) % NW
            wv4 = s4.ap[:, 0:3072].rearrange("p (j k n) -> p j k n", j=3, k=8)
            ws4 = s4
            for j_ in range(3):
                c0_ = 448 + j_ * 512 + hp * 128
                B.dma("sp", wv4[:, j_, :, :], wb["w_in"][l, :, c0_:c0_ + 128].rearrange("(k p) n -> p k n", p=128),
                      R=[wdep[("w_in", l)]], W=[s4])
            zchunk(wv4[:, 0, :, :], ws4, None, hp, zrs[qs])
            yield 1
            zchunk(wv4[:, 1, :, :], ws4, None, 4 + hp, zks[qs])
            yield 1
            zchunk(wv4[:, 2, :, :], ws4, None, 8 + hp, zvs[qs])
            yield 1
            X = tmps[qs]
            cols = slice(hp * 128, (hp + 1) * 128)
            p = psd()
            B.mm(p[:, 0:T], s_wa.ap[0:64, cols], txw[0:64, :], [s_wa, txw], [p])
            B.A(X["sgw"].ap, p[:, 0:T], AF.Sigmoid, [p, vec[l]], [X["sgw"]], bias=vcol(l, "w0", hp))
            p = psd()
            B.mm(p[:, 0:T], s_wa.ap[64:128, cols], xab[64:128, :], [s_wa, xab], [p])
            B.A(X["aa"].ap, p[:, 0:T], AF.Sigmoid, [p, vec[l]], [X["aa"]], bias=vcol(l, "a0", hp))
            p = psd()
            B.mm(p[:, 0:T], gupv[:, cols], sgb.ap, [gups, sgb], [p])
            B.cp("act", g3[:, hp, :], p[:, 0:T], [p], [gbuf])
            yield 1
            cs = X["cs"]
            onec = cst[:, CI_ONE:CI_ONE + 128]
            for c in range(2):
                sl_ = slice(c * 128, (c + 1) * 128)
                B.op("dve", lambda e, sl_=sl_: e.tensor_tensor_scan(X["cs"][:, sl_], onec, X["sgw"][:, sl_], 0.0,
                                                                    ALU.mult, ALU.add), [cst, X["sgw"]], [X["cs"]])
            for c in range(2):
                B.ts("dve", X["csc"][:, c * 128:(c + 1) * 128], cs[:, c * 128:(c + 1) * 128],
                     cs[:, c * 128 + 63:c * 128 + 64], ALU.subtract, [cs], [X["csc"]])
            for c in range(2):
                B.cp("pool", sm[:, hp * 2 + c:hp * 2 + c + 1], cs[:, c * 128 + 63:c * 128 + 64], [cs], [sm])
                B.cp("pool", sm[:, 8 + hp * 2 + c:8 + hp * 2 + c + 1], X["csc"][:, c * 128 + 127:c * 128 + 128],
                     [X["csc"]], [sm])
            B.tt("dve", X["csx"].ap, X["csc"].ap, X["sgw"].ap, ALU.subtract, [X["csc"], X["sgw"]], [X["csx"]])
            for c in range(2):
                B.ts("dve", X["dC"][:, c * 128:(c + 1) * 128], X["csc"][:, c * 128:(c + 1) * 128],
                     X["csc"][:, c * 128 + 127:c * 128 + 128], ALU.subtract, [X["csc"]], [X["dC"]])
            B.A(X["E1"].ap, X["csc"].ap, AF.Exp, [X["csc"]], [X["E1"]], scale=-CC)
            B.A(X["E2"].ap, X["csc"].ap, AF.Exp, [X["csc"]], [X["E2"]], scale=CC)
            B.A(X["E3"].ap, X["csx"].ap, AF.Exp, [X["csx"]], [X["E3"]], scale=-CC)
            B.A(X["E4"].ap, X["dC"].ap, AF.Exp, [X["dC"]], [X["E4"]], scale=CC)
            yield 1
            B.ts("dve", X["kk"].ap, zks[qs].ap, vcol(l, "k_k", hp), ALU.mult, [zks[qs], vec[l]], [X["kk"]])
            B.A(kk2s[qs].ap, X["kk"].ap, AF.Square, [X["kk"]], [kk2s[qs]])
            p = psd()
            B.mm(p[:, 0:T], blk_bf, kk2s[qs].ap, [cbf, kk2s[qs]], [p])
            B.ts("dve", X["ssm"].ap, p[:, 0:T], 1e-24, ALU.max, [p], [X["ssm"]])
            B.A(X["rs"].ap, X["ssm"].ap, AF.Sqrt, [X["ssm"]], [X["rs"]])
            B.rcp(X["ssm"].ap, X["rs"].ap, [X["rs"]], [X["ssm"]])
            B.tt("dve", X["kkn"].ap, X["kk"].ap, X["ssm"].ap, ALU.mult, [X["kk"], X["ssm"]], [X["kkn"]])
            yield 1
            B.stt("dve", aT_3[:, hp, :], X["kkn"].ap, -1.0, X["E3"].ap, ALU.mult, ALU.mult, [X["kkn"], X["E3"]], [aTt])
            B.tt("dve", X["tb"].ap, X["kkn"].ap, X["aa"].ap, ALU.mult, [X["kkn"], X["aa"]], [X["tb"]])
            B.tt("dve", bT_3[:, hp, :], X["tb"].ap, X["E2"].ap, ALU.mult, [X["tb"], X["E2"]], [bTt])
            B.tt("pool", bhs[qs].ap, X["tb"].ap, X["E4"].ap, ALU.mult, [X["tb"], X["E4"]], [bhs[qs]])
            B.ts("dve", X["tc"].ap, X["aa"].ap, vcol(l, "k_a", hp), ALU.mult, [X["aa"], vec[l], omka[l]], [X["tc"]],
                 s2=omka[l][:, hp:hp + 1], op1=ALU.add)
            B.tt("dve", X["km"].ap, zks[qs].ap, X["tc"].ap, ALU.mult, [zks[qs], X["tc"]], [X["km"]])
            B.tt("dve", kT_3[:, hp, :], X["km"].ap, X["E2"].ap, ALU.mult, [X["km"], X["E2"]], [kTt])
            B.tt("pool", khs[qs].ap, X["km"].ap, X["E4"].ap, ALU.mult, [X["km"], X["E4"]], [khs[qs]])
            yield 1
            B.tt("dve", rT_3[:, hp, :], zrs[qs].ap, X["E1"].ap, ALU.mult, [zrs[qs], X["E1"]], [rTt])
            B.stt("dve", rkrs[qs].ap, zrs[qs].ap, vcol(l, "r_k", hp), X["km"].ap, ALU.mult, ALU.mult, [zrs[qs], vec[l], X["km"]], [rkrs[qs]])
            p = psd()
            B.mm(p[:, 0:T], blk_bf, rkrs[qs].ap, [cbf, rkrs[qs]], [p])
            B.tt("dve", bonus3[:, hp, :], p[:, 0:T], zvs[qs].ap, ALU.mult, [p, zvs[qs]], [bonus])
            B.cp("pool", vbfs[qs].ap, zvs[qs].ap, [zvs[qs]], [vbfs[qs]])
            yield 1
            pt = PS[7]
            ptb = pt.ap.bitcast(BF16)
            ptb4 = ptb[:, 0:768].rearrange("p (j c n) -> p j c n", j=3, c=2)
            for j_, sbuf_ in enumerate((vbfs[qs], bhs[qs], khs[qs])):
                for c in range(2):
                    B.tr(ptb4[:, j_, c, :], sbuf_[:, c * 128:(c + 1) * 128], ident_bf, [sbuf_, cbf], [pt])
            B.cp("act", TM5[:, :, :, hp, :], ptb4, [pt], [TM])
            yield "pre6"
            bA, bB, bC = PS[4], PS[5], PS[6]
            bA4 = v3(bA.ap, 4)
            bB4 = v3(bB.ap, 4)
            bC4 = v3(bC.ap, 4)
            bks = ((PS[4], PS[5], PS[6]), (PS[1], PS[2], PS[3]))
            Q4 = Qd[qs].ap.rearrange("p (c e n) -> p c e n", c=2, e=2)
            QT4 = QTd[qs].ap.rearrange("p (c e n) -> p c e n", c=2, e=2)
            for e in range(2):
                kA, kB, kC = bks[e]
                kA4 = kA.ap.rearrange("p (c t n) -> p c t n", c=2, t=2)
                kB4 = kB.ap.rearrange("p (c t n) -> p c t n", c=2, t=2)
                kC3 = v3(kC.ap[:, 0:256], 2)
                pr = slice(e * 64, e * 64 + 64)
                for c in range(2):
                    cs_ = slice(c * 128, (c + 1) * 128)
                    B.mm(kA4[:, c, 0, :], kT_3[pr, hp, cs_], aT_3[pr, hp, cs_], [kTt, aTt], [kA])
                    B.mm(kA4[:, c, 1, :], bT_3[pr, hp, cs_], aT_3[pr, hp, cs_], [bTt, aTt], [kA])
                    B.mm(kB4[:, c, 0, :], bT_3[pr, hp, cs_], rT_3[pr, hp, cs_], [bTt, rTt], [kB])
                    B.mm(kB4[:, c, 1, :], kT_3[pr, hp, cs_], rT_3[pr, hp, cs_], [kTt, rTt], [kB])
                    B.mm(kC3[:, c, :], aT_3[pr, hp, cs_], bT_3[pr, hp, cs_], [aTt, bTt], [kC])
                amds = [AM_d[0][hp], AM_d[1][hp]]
                B.tt("dve", AM6[:, :, hp, 0, e, :], kA4[:, :, 0, :], m_su.unsqueeze(1).to_broadcast([128, 2, 128]),
                     ALU.mult, [kA, cst], amds)
                B.tt("dve", Q4[:, :, e, :], kA4[:, :, 1, :], m_su.unsqueeze(1).to_broadcast([128, 2, 128]),
                     ALU.mult, [kA, cst], [Qd[qs]])
                for c in range(2):
                    B.tt("dve", AM6[:, c, hp, 1:3, e, :], kB4[:, c, :, :], m_u.unsqueeze(1).to_broadcast([128, 2, 128]),
                         ALU.mult, [kB, cst], [amds[c]])
                B.tt("dve", QT4[:, :, e, :], kC3, m_sl.unsqueeze(1).to_broadcast([128, 2, 128]), ALU.mult, [kC, cst],
                     [QTd[qs]])
            yield 1

        def stageB(hp):
            qs = hp % 2
            bA, bB, bC = PS[4], PS[5], PS[6]
            bA4 = v3(bA.ap, 4)
            bB4 = v3(bB.ap, 4)
            bC4 = v3(bC.ap, 4)
            B.tt("dve", v3(Xd[qs].ap, 4), v3(Qd[qs].ap, 4), ident_f.unsqueeze(1).to_broadcast([128, 4, 128]), ALU.add,
                 [Qd[qs], cst], [Xd[qs]])
            B.cp("pool", Xbbs[qs].ap, Xd[qs].ap, [Xd[qs]], [Xbbs[qs]])
            Qc, QTc, Xc = v3(Qd[qs].ap, 4), v3(QTd[qs].ap, 4), v3(Xd[qs].ap, 4)
            Xb4 = v3(Xbbs[qs].ap, 4)
            for j in range(6):
                for m in range(4):
                    B.mm(bA4[:, m, :], Qc[:, m, :], QTc[:, m, :], [Qd[qs], QTd[qs]], [bA])
                if j < 5:
                    for m in range(4):
                        B.mm(bB4[:, m, :], QTc[:, m, :], Qc[:, m, :], [Qd[qs], QTd[qs]], [bB])
                B.cp("act", QTc, bA4, [bA], [QTd[qs]])
                if j < 5:
                    B.cp("act", Qc, bB4, [bB], [Qd[qs]])
                for m in range(4):
                    B.mm(bC4[:, m, :], QTc[:, m, :], Xb4[:, m, :], [QTd[qs], Xbbs[qs]], [bC])
                if j < 5:
                    B.tt("dve", Xc, Xc, bC4, ALU.add, [Xd[qs], bC], [Xd[qs]])
                    B.cp("pool", Xbbs[qs].ap, Xd[qs].ap, [Xd[qs]], [Xbbs[qs]])
                else:
                    for c in range(2):
                        B.tt("dve", AM6[:, c, hp, 3, :, :], Xc[:, c * 2:c * 2 + 2, :], bC4[:, c * 2:c * 2 + 2, :],
                             ALU.add, [Xd[qs], bC], [AM_d[c][hp]])
                yield 1

        K0 = 5
        Ag = [stageA(h) for h in range(4)]
        Bg = [stageB(h) for h in range(4)]
        A_cnt = [0] * 4
        A_started = [False] * 4
        A_done = [False] * 4
        A_pre6 = [False] * 4
        B_done = [False] * 4
        ca_done = False
        rot["n"] = 2
        guard = 0
        while not (ca_done and all(A_done) and all(B_done)):
            guard += 1
            assert guard < 10000
            if not ca_done:
                ca_done = next(ca, "END") == "END"
                if ca_done:
                    rot["n"] = 4
            for h in range(4):
                if not A_started[h]:
                    ok = (h == 0) or (A_cnt[h - 1] >= K0) or A_done[h - 1]
                    if h >= 2:
                        ok = ok and (A_pre6[h - 2] or A_done[h - 2])
                    if ok:
                        A_started[h] = True
                if A_started[h] and not A_done[h]:
                    if A_pre6[h] and not A_done[h]:
                        if (not ca_done) or (h >= 2 and not B_done[h - 2]):
                            continue
                    v_ = next(Ag[h], "END")
                    if v_ == "END":
                        A_done[h] = True
                    else:
                        A_cnt[h] += 1
                        if v_ == "pre6":
                            A_pre6[h] = True
            for h in range(4):
                if A_done[h] and not B_done[h]:
                    if next(Bg[h], "END") == "END":
                        B_done[h] = True
        mark("rw_chain", l, tt)
        B.A(sm[:, 16:24], sm[:, 0:8], AF.Exp, [sm], [sm], scale=-CC)
        B.A(sm[:, 24:32], sm[:, 8:16], AF.Exp, [sm], [sm], scale=-CC)
        Pm3 = sm[:, 16:24].rearrange("p (h c) -> p h c", c=2)
        PC3 = sm[:, 24:32].rearrange("p (h c) -> p h c", c=2)
        S3_ = v3(Sst.ap, 4)
        Smid3 = v3(Smid.ap, 4)
        Smb3 = v3(Smb.ap, 4)
        allAM = [AM_d[c][h] for c in range(2) for h in range(4)]
        for c in range(2):
            cs_ = slice(c * 128, (c + 1) * 128)
            amc = AM_d[c]
            B.tt("dve", Smid3, S3_, Pm3[:, :, c:c + 1].to_broadcast([128, 4, 64]), ALU.mult, [Sst, sm], [Smid])
            B.cp("dve", Smb.ap, Smid.ap, [Smid], [Smb])
            pWe = (PS[0], PS[1])
            pU, pS = PS[2], PS[3]
            pYe = (PS[4], PS[5])
            pU3 = v3(pU.ap, 8)
            pS3 = v3(pS.ap[:, 0:256], 4)
            W0b3 = v3(W0b.ap, 8)
            Ut3 = v3(Ut.ap, 8)
            W0b4 = W0b.ap.rearrange("p (h e v) -> p h e v", h=4, e=2)
            for e in range(2):
                pr = slice(e * 64, e * 64 + 64)
                pW3 = v3(pWe[e].ap[:, 0:256], 4)
                for hp in range(4):
                    B.mm(pW3[:, hp, :], AM6[:, c, hp, 0, e, :], TM5[:, 0, c, hp, e * 64:e * 64 + 64], [amc[hp], TM],
                         [pWe[e]], start=True, stop=False)
                    B.mm(pW3[:, hp, :], aT_3[pr, hp, cs_], Smb3[pr, hp, :], [aTt, Smb], [pWe[e]], start=False, stop=True)
                B.cp("act", W0b4[:, :, e, :], pW3, [pWe[e]], [W0b])
            for hp in range(4):
                for e in range(2):
                    h = hp * 2 + e
                    B.mm(pU3[:, h, :], AM6[:, c, hp, 3, e, :], W0b3[:, h, :], [amc[hp], W0b], [pU])
            B.cp("act", Ut.ap, pU.ap, [pU], [Ut])
            for e in range(2):
                pr = slice(e * 64, e * 64 + 64)
                pY3 = v3(pYe[e].ap, 4)
                for hp in range(4):
                    h = hp * 2 + e
                    B.mm(pY3[pr, hp, :], Smb3[pr, hp, :], rT_3[pr, hp, cs_], [Smb, rTt], [pYe[e]], start=True, stop=False)
                    B.mm(pY3[pr, hp, :], Ut3[:, h, :], AM6[:, c, hp, 1, e, :], [Ut, amc[hp]], [pYe[e]], start=False,
                         stop=False)
                    B.mm(pY3[pr, hp, :], TM5[:, 0, c, hp, e * 64:e * 64 + 64], AM6[:, c, hp, 2, e, :], [TM, amc[hp]],
                         [pYe[e]], start=False, stop=True)
                B.cp("act", y3[pr, :, cs_], pY3[pr, :, :], [pYe[e]], [yb])
            for hp in range(4):
                for e in range(2):
                    h = hp * 2 + e
                    pr = slice(e * 64, e * 64 + 64)
                    B.mm(pS3[pr, hp, :], TM5[:, 1, c, hp, e * 64:e * 64 + 64], Ut3[:, h, :], [TM, Ut], [pS], start=True,
                         stop=False)
                    B.mm(pS3[pr, hp, :], TM5[:, 2, c, hp, e * 64:e * 64 + 64], TM5[:, 0, c, hp, e * 64:e * 64 + 64],
                         [TM], [pS], start=False, stop=True)
            B.tt("dve", S3_, Smid3, PC3[:, :, c:c + 1].to_broadcast([128, 4, 64]), ALU.mult, [Smid, sm], [Sst])
            B.tt("dve", S3_, S3_, pS3, ALU.add, [Sst, pS], [Sst])
        if stage == 26:
            return
        mark("rw_norm", l, tt)
        for hp in range(4):
            B.cp("pool", ybf.ap, y3[:, hp, :], [yb], [ybf])
            p = psd()
            B.mm(p[:, 0:T], blk_bf, ybf.ap, [cbf, ybf], [p])
            B.stt("dve", yc.ap, p[:, 0:T], -1.0 / 64, y3[:, hp, :], ALU.mult, ALU.add, [p, yb], [yc])
            B.A(ysq.ap, yc.ap, AF.Square, [yc], [ysq])
            p = psd()
            B.mm(p[:, 0:T], blk_bf, ysq.ap, [cbf, ysq], [p])
            B.A(sd.ap, p[:, 0:T], AF.Sqrt, [p, eps_t], [sd], scale=1.0 / 64, bias=eps_ap(LN_EPS))
            B.rcp(t1.ap, sd.ap, [sd], [t1])
            B.tt("dve", t2.ap, yc.ap, t1.ap, ALU.mult, [yc, t1], [t2])
            B.ts("dve", t1.ap, t2.ap, vcol(l, "ln_w", hp), ALU.mult, [t2, vec[l]], [t1], s2=vcol(l, "ln_b", hp),
                 op1=ALU.add)
            B.tt("dve", t2.ap, t1.ap, bonus3[:, hp, :], ALU.add, [t1, bonus], [t2])
            B.tt("dve", o_rw[:, hp * T:(hp + 1) * T], t2.ap, g3[:, hp, :], ALU.mult, [t2, gbuf], [o_rw])

    for l in range(nl):
        layer(l)
    B.emit(block)
    es.__exit__(None, None, None)
    return nc


def _consts():
    c = np.zeros((128, NCONST), np.float32)
    i = np.arange(128)
    c[:, CI_ID:CI_ID + 128] = np.eye(128)
    c[:, CI_J:CI_J + 128] = np.eye(128)[::-1]
    c[:, CI_ONE:CI_ONE + 128] = 1.0
    blk = np.zeros((128, 128), np.float32)
    blk[:64, :64] = 1
    blk[64:, 64:] = 1
    c[:, CI_BLK:CI_BLK + 128] = blk
    c[:, CI_SU:CI_SU + 128] = (i[None, :] > i[:, None])
    c[:, CI_U:CI_U + 128] = (i[None, :] >= i[:, None])
    c[:, CI_SL:CI_SL + 128] = (i[None, :] < i[:, None])
    half = 32
    invf = (1.0 / (np.float32(10000.0) ** (np.arange(half, dtype=np.float32) / np.float32(half)))).astype(np.float32)
    c[:64, CI_INVF] = np.concatenate([invf, invf])
    c[:64, CI_SGN] = np.concatenate([-np.ones(32), np.ones(32)]) * TWO_PI
    return c


def prep_shared(inp, nl=NL):
    f = lambda a: np.ascontiguousarray(np.asarray(a, dtype=np.float32))
    sh = {}
    w_in = f(inp["w_in"])[:nl]
    sh["w_in"] = w_in
    kr = w_in[:, :, 384:448]
    sh["w_in_sw"] = np.ascontiguousarray(np.concatenate([kr[:, :, 32:], kr[:, :, :32]], axis=-1))
    wuq = f(inp["mla_w_uq"])[:nl]
    sh["w_uq"] = wuq
    r4 = wuq.reshape(nl, 256, 4, 192)[:, :, :, 128:]
    sh["w_uq_sw"] = np.ascontiguousarray(np.concatenate([r4[..., 32:], r4[..., :32]], axis=-1).reshape(nl, 256, 256))
    wukv = f(inp["mla_w_ukv"])[:nl]
    sh["w_ukv"] = wukv
    nope = wukv.reshape(nl, 128, 4, 256)[:, :, :, :128]
    sh["w_ukT"] = np.ascontiguousarray(nope.transpose(0, 2, 3, 1).reshape(nl, 512, 128))
    sh["rw_w_up"] = f(inp["rw_w_up"])[:nl]
    sh["rw_a_up"] = f(inp["rw_a_up"])[:nl]
    sh["rw_g_up"] = f(inp["rw_g_up"])[:nl]
    for n in ("w_branch", "w_out", "w_ff1", "w_ff2", "w_ple_gate", "w_ple_proj"):
        sh[n] = f(inp[n])[:nl]
    vecs = np.zeros((nl, 128, NV), np.float32)

    def put(name, arr):
        a = f(arr)[:nl].reshape(nl, -1, 128)
        vecs[:, :, VC[name]:VC[name] + a.shape[1]] = a.transpose(0, 2, 1)

    put("pre_mix_g", inp["pre_mix_g"])
    put("q_g", inp["mla_q_norm_g"])
    put("kv_g", inp["mla_kv_norm_g"])
    put("mu", inp["rw_mu"])
    put("w0", inp["rw_w0"])
    put("a0", inp["rw_a0"])
    put("k_k", inp["rw_k_k"])
    put("k_a", inp["rw_k_a"])
    put("r_k", np.asarray(inp["rw_r_k"]).reshape(-1, 512))
    put("ln_w", inp["rw_ln_w"])
    put("ln_b", inp["rw_ln_b"])
    put("post_mix_g", inp["post_mix_g"])
    put("pre_ff_g", inp["pre_ff_g"])
    put("post_ff_g", inp["post_ff_g"])
    sh["vecs"] = vecs
    sh["consts"] = _consts()
    rel = f(inp["ca_rel_bias"])[:nl]
    sh["relr"] = np.ascontiguousarray(rel[:, ::-1, :].transpose(0, 2, 1))
    return sh


def prep_core(inp, b, S, nl=NL):
    d = {}
    d["xT"] = np.ascontiguousarray(np.asarray(inp["x"], dtype=np.float32)[b, :S].T)
    d["pT"] = np.ascontiguousarray(np.asarray(inp["p"], dtype=np.float32)[:nl, b, :S].transpose(0, 2, 1))
    d["pos"] = np.ascontiguousarray(np.asarray(inp["positions"]).astype(np.int32)[b:b + 1, :S])
    return d


_NC_CACHE = {}
PHASES = []


def kernel(**inputs):
    key = (SEQ, NL)
    if key not in _NC_CACHE:
        _NC_CACHE[key] = build(SEQ, NL)
    nc = _NC_CACHE[key]
    sh = prep_shared(inputs)
    in_maps = []
    for b in range(NB):
        m = dict(sh)
        m.update(prep_core(inputs, b, SEQ))
        in_maps.append(m)
    res = run_bass_kernel_spmd(nc, in_maps, core_ids=list(range(NB)))
    out = np.stack([np.asarray(r["outT"]).T for r in res.results], axis=0)
    return np.ascontiguousarray(out.astype(np.float32))
```

```python
import contextlib
import numpy as np
import ml_dtypes
import concourse.bass as bass
import concourse.mybir as mybir
from concourse.bass_utils import run_bass_kernel_spmd

F32 = mybir.dt.float32
BF16 = mybir.dt.bfloat16
I32 = mybir.dt.int32
AF = mybir.ActivationFunctionType
ALU = mybir.AluOpType

D = 1024
DFF = 4096
DPLE = 256
INC = 6848
NL = 2
SEQ = 4096
NB = 8
T = 256
EPS = 1e-6
LN_EPS = 64e-5
CC = float(np.exp(-0.5))
MLA_SCALE = float(192 ** -0.5)
TWO_PI = 6.2831845
NEG = -30000.0

VC = {}
_o = 0
for _n, _k in [("pre_mix_g", 8), ("q_g", 2), ("kv_g", 1), ("mu", 14), ("w0", 4), ("a0", 4), ("k_k", 4),
               ("k_a", 4), ("r_k", 4), ("ln_w", 4), ("ln_b", 4), ("post_mix_g", 8), ("pre_ff_g", 8),
               ("post_ff_g", 8)]:
    VC[_n] = _o
    _o += _k
NV = _o
CI_ID, CI_J, CI_ONE, CI_BLK, CI_SU, CI_U, CI_SL = [i * 128 for i in range(7)]
CI_INVF = 7 * 128
CI_SGN = CI_INVF + 1
NCONST = CI_SGN + 1

WSPEC = {
    "w_in": (1024, INC), "w_in_sw": (1024, 64), "w_uq": (256, 768), "w_uq_sw": (256, 256),
    "w_ukT": (512, 128), "w_ukv": (128, 1024), "rw_w_up": (64, 512), "rw_a_up": (64, 512),
    "rw_g_up": (128, 512), "w_branch": (1536, 1024), "w_out": (1024, 1024), "w_ff1": (1024, 4096),
    "w_ff2": (4096, 1024), "w_ple_gate": (1024, 1024), "w_ple_proj": (256, 1024),
}
WORDER = ["w_in", "w_in_sw", "w_uq", "w_uq_sw", "w_ukT", "w_ukv", "rw_w_up", "rw_a_up", "rw_g_up",
          "w_branch", "w_out", "w_ff1", "w_ff2", "w_ple_gate", "w_ple_proj"]


class Dep:
    __slots__ = ("w", "r", "al")

    def __init__(self):
        self.w = None
        self.r = {}
        self.al = []


class Buf:
    def __init__(self, ap, d=None):
        self.ap = ap
        self.d = d if d is not None else Dep()

    def __getitem__(self, k):
        return self.ap[k]


def v3(ap, a):
    return ap.rearrange("p (a b) -> p a b", a=a)


ENGS = ("pe", "act", "dve", "pool", "sp")
NDS = 8


class Builder:
    def __init__(self, nc, es):
        self.nc = nc
        self.ops = {e: [] for e in ENGS}
        self.cnt = {e: 0 for e in ENGS}
        self.waited = {e: {} for e in ENGS}
        self.sems = []
        self.semid = {}
        for e in ENGS:
            self.semid[e] = len(self.sems)
            self.sems.append(es.enter_context(nc.semaphore("s_" + e)))
        self.dsem = {}
        self.dcnt = {}
        for q in ("sp", "pool", "act"):
            self.dsem[q] = []
            self.dcnt[q] = 0
            for i in range(NDS):
                self.dsem[q].append(len(self.sems))
                self.sems.append(es.enter_context(nc.semaphore("d_%s%d" % (q, i))))

    def _waits(self, eng, reads, writes):
        need = {}

        def add(sid, val):
            if need.get(sid, 0) < val:
                need[sid] = val

        for d in reads:
            if d.w is not None:
                add(*d.w)
        for d in writes:
            if d.w is not None:
                add(*d.w)
            for sid, val in d.r.items():
                add(sid, val)
            for a in d.al:
                if a.w is not None:
                    add(*a.w)
                for sid, val in a.r.items():
                    add(sid, val)
        out = []
        wd = self.waited[eng]
        pe_sid = self.semid["pe"]
        for sid, val in need.items():
            if eng == "pe" and sid == pe_sid:
                continue
            if wd.get(sid, 0) >= val:
                continue
            wd[sid] = val
            out.append((sid, val))
        return out

    def _mark(self, reads, writes, sid, val):
        for d in reads:
            if d.r.get(sid, 0) < val:
                d.r[sid] = val
        for d in writes:
            d.w = (sid, val)
            d.r = {}

    def op(self, eng, fn, R=(), W=()):
        R = [x.d if isinstance(x, Buf) else x for x in R]
        W = [x.d if isinstance(x, Buf) else x for x in W]
        ws = self._waits(eng, R, W)
        self.cnt[eng] += 1
        sid = self.semid[eng]
        self.ops[eng].append((ws, fn, sid, 1))
        self._mark(R, W, sid, self.cnt[eng])

    def dma(self, q, out, in_, R=(), W=(), **kw):
        R = [x.d if isinstance(x, Buf) else x for x in R]
        W = [x.d if isinstance(x, Buf) else x for x in W]
        ws = self._waits(q, R, W)
        i = self.dcnt[q]
        self.dcnt[q] += 1
        sid = self.dsem[q][i % NDS]
        val = 16 * (i // NDS + 1)
        if val > 16 and self.waited[q].get(sid, 0) < val - 16:
            self.waited[q][sid] = val - 16
            ws.append((sid, val - 16))
        self.ops[q].append((ws, lambda e, o=out, i_=in_, k=kw: e.dma_start(out=o, in_=i_, **k), sid, 16))
        self._mark(R, W, sid, val)

    def mm(self, out, lhsT, rhs, R, W, start=True, stop=True):
        self.pes = getattr(self, "pes", 0) + (2 if lhsT.dtype == F32 else 1)
        self.op("pe", lambda e: e.matmul(out, lhsT, rhs, start=start, stop=stop, skip_group_check=True), R, W)

    def tr(self, out, in_, ident, R, W):
        self.pes = getattr(self, "pes", 0) + 1
        self.op("pe", lambda e: e.transpose(out, in_, ident), R, W)

    def A(self, out, in_, func, R, W, scale=None, bias=None):
        kw = {}
        if scale is not None:
            kw["scale"] = scale
        if bias is not None:
            kw["bias"] = bias
        self.op("act", lambda e: e.activation(out, in_, func, **kw), R, W)

    def tt(self, eng, out, a, b, op, R, W):
        self.op(eng, lambda e: e.tensor_tensor(out, a, b, op), R, W)

    def ts(self, eng, out, a, s1, op0, R, W, s2=None, op1=None):
        if op1 is None:
            self.op(eng, lambda e: e.tensor_scalar(out, a, s1, None, op0), R, W)
        else:
            self.op(eng, lambda e: e.tensor_scalar(out, a, s1, s2, op0, op1), R, W)

    def stt(self, eng, out, a, s, b, op0, op1, R, W):
        self.op(eng, lambda e: e.scalar_tensor_tensor(out, a, s, b, op0, op1), R, W)

    def cp(self, eng, out, in_, R, W):
        if eng == "act":
            self.op("act", lambda e: e.activation(out, in_, AF.Copy), R, W)
        else:
            self.op(eng, lambda e: e.tensor_copy(out, in_), R, W)

    def rcp(self, out, in_, R, W):
        self.op("dve", lambda e: e.reciprocal(out, in_), R, W)

    def ms(self, eng, ap, val, W):
        self.op(eng, lambda e: e.memset(ap, val), (), W)

    def emit(self, block):
        sems = self.sems
        B = self

        def run(e, name):
            for ws, fn, sid, inc in B.ops[name]:
                for s_, v_ in ws:
                    e.wait_ge(sems[s_], v_)
                fn(e).then_inc(sems[sid], inc)

        fin = []
        for en in ENGS:
            if self.cnt[en] > 0:
                fin.append((self.semid[en], self.cnt[en]))
        for q in ("sp", "pool", "act"):
            n = self.dcnt[q]
            for k in range(NDS):
                cntk = (n - k + NDS - 1) // NDS if n > k else 0
                if cntk > 0:
                    fin.append((self.dsem[q][k], 16 * cntk))

        @block.tensor
        def _(e):
            run(e, "pe")

        @block.scalar
        def _(e):
            run(e, "act")

        @block.vector
        def _(e):
            run(e, "dve")

        @block.gpsimd
        def _(e):
            run(e, "pool")

        @block.sync
        def _(e):
            run(e, "sp")
            for s_, v_ in fin:
                e.wait_ge(sems[s_], v_)


def build(S=SEQ, nl=NL, dbg=False, stage=99):
    NT = S // T
    NBLK = S // 128
    nc = bass.Bass("TRN2", target_bir_lowering=False)
    es = contextlib.ExitStack()
    es.__enter__()
    B = Builder(nc, es)

    def dram(name, shape, dt, kind):
        return nc.dram_tensor(name, list(shape), dt, kind=kind).ap()

    xT = dram("xT", [D, S], F32, "ExternalInput")
    pT = dram("pT", [nl, DPLE, S], F32, "ExternalInput")
    pos = dram("pos", [1, S], I32, "ExternalInput")
    vecs = dram("vecs", [nl, 128, NV], F32, "ExternalInput")
    consts = dram("consts", [128, NCONST], F32, "ExternalInput")
    relr = dram("relr", [nl, 8, 320], F32, "ExternalInput")
    wf = {}
    wb = {}
    wdep = {}
    for n in WORDER:
        r, c = WSPEC[n]
        wf[n] = dram(n, [nl, r, c], F32, "ExternalInput")
        wb[n] = dram(n + "_b", [nl, r, c], BF16, "Internal")
        for l in range(nl):
            wdep[(n, l)] = Dep()
    outT = dram("outT", [D, S], F32, "ExternalOutput")
    hscr = [dram("hscr%d" % i, [D, S], F32, "Internal") for i in range(max(nl - 1, 1))]
    hscr_d = [[Dep() for _ in range(NT)] for _ in range(max(nl - 1, 1))]
    ext = dram("ext", [nl, 8, 768], F32, "Internal")
    ext_d = [Dep() for _ in range(nl)]
    dbg_t = {}
    if dbg:
        for n in ("o_mla", "o_rw", "o_ca"):
            dbg_t[n] = dram("dbg_" + n, [512, S], BF16, "ExternalOutput")

    def sb(name, shape, dt):
        return Buf(es.enter_context(nc.sbuf_tensor(name, list(shape), dt))[:])

    cst = sb("cst", [128, NCONST], F32)
    vec = [sb("vec%d" % l, [128, NV], F32) for l in range(nl)]
    omka = [sb("omka%d" % l, [128, 4], F32) for l in range(nl)]
    cbf = sb("cbf", [128, 4 * 128], BF16)
    ident_bf = cbf[:, 0:128]
    J_bf = cbf[:, 128:256]
    ones_bf = cbf[:, 256:384]
    blk_bf = cbf[:, 384:512]
    ident_f = cst[:, CI_ID:CI_ID + 128]
    m_su = cst[:, CI_SU:CI_SU + 128]
    m_u = cst[:, CI_U:CI_U + 128]
    m_sl = cst[:, CI_SL:CI_SL + 128]

    uT = sb("uT", [128, 8 * T], BF16)
    uT3 = v3(uT.ap, 8)
    NW = 3
    wsl = [sb("wsl%d" % i, [128, 4096], BF16) for i in range(NW)]
    wsl_i = [0]
    Kc = sb("Kc", [128, S], BF16)
    Kr = sb("Kr", [64, S], BF16)
    Vc = sb("Vc", [128, S], BF16)
    Vc3 = v3(Vc.ap, NBLK)
    Kc_d = [Dep() for _ in range(NT)]
    CK = sb("CK", [128, 4 * 1024], BF16)
    CK3 = v3(CK.ap, 4)
    CV = sb("CV", [128, 8 * 512], BF16)
    CV3 = v3(CV.ap, 8)
    CK_d = [Dep() for _ in range(4)]
    CV_d = [Dep() for _ in range(8)]
    Xb = sb("Xb", [128, 40 * 128], BF16)
    Xb3 = v3(Xb.ap, 40)
    o_mla = sb("o_mla", [128, 4 * T], BF16)
    o_rw = sb("o_rw", [128, 4 * T], BF16)
    o_ca = sb("o_ca", [128, 4 * T], BF16)
    Sst = sb("Sst", [128, 256], F32)
    zlast = sb("zlast", [128, 16], F32)
    small = sb("small", [128, 64], F32)

    AW = 28900
    arena = es.enter_context(nc.sbuf_tensor("arena", [128, AW], F32))[:]
    abufs = []
    aoff = {}

    def al(phase, name, n, dt):
        words = (n + 1) // 2 if dt == BF16 else n
        o = aoff.get(phase, 0)
        aoff[phase] = o + words
        assert o + words <= AW, (phase, name, o + words)
        ap = arena[:, o:o + words]
        if dt == BF16:
            ap = ap.bitcast(BF16)[:, 0:n]
        b = Buf(ap)
        for (ph2, o2, e2, b2) in abufs:
            if ph2 != phase and o2 < o + words and o < e2:
                b.d.al.append(b2.d)
                b2.d.al.append(b.d)
        abufs.append((phase, o, o + words, b))
        return b

    def al_all(name, n, dt):
        words = (n + 1) // 2 if dt == BF16 else n
        o = max([aoff.get(p, 0) for p in ("mla", "ca", "rw", "ffn")])
        for p in ("mla", "ca", "rw", "ffn"):
            assert aoff.get(p, 0) <= o
            aoff[p] = o + words
        ap = arena[:, o:o + words]
        if dt == BF16:
            ap = ap.bitcast(BF16)[:, 0:n]
        return Buf(ap)

    sqb = al_all("sqb", 8 * T, BF16)
    sqb3 = v3(sqb.ap, 8)
    rt = al_all("rt", T, F32)
    rt2 = al_all("rt2", T, F32)

    PS = [Buf(es.enter_context(nc.psum_tensor("ps%d" % i, [128, 512], F32))[:]) for i in range(8)]
    rot = {"d": 0, "n": 4}

    def psd():
        i = rot["d"] % rot["n"]
        rot["d"] = (i + 1) % rot["n"]
        return PS[i]

    block = es.enter_context(nc.Block())

    B.dma("sp", cst.ap, consts, W=[cst])
    for l in range(nl):
        B.dma("sp", vec[l].ap, vecs[l], W=[vec[l]])
    for l in range(nl):
        for n in WORDER:
            r, c = WSPEC[n]
            npc = (c + 2047) // 2048
            pc = c // npc
            assert pc * npc == c
            for i in range(npc):
                B.dma("pool", wb[n][l, :, i * pc:(i + 1) * pc], wf[n][l, :, i * pc:(i + 1) * pc], W=[wdep[(n, l)]])
    B.cp("dve", cbf[:, 0:512], cst[:, 0:512], [cst], [cbf])
    for l in range(nl):
        ka = vec[l][:, VC["k_a"]:VC["k_a"] + 4]
        B.ts("dve", omka[l].ap, ka, -1.0, ALU.mult, [vec[l]], [omka[l]], s2=1.0, op1=ALU.add)

    def vcol(l, name, i):
        c = VC[name] + i
        return vec[l][:, c:c + 1]

    def wload(name, l, dram_ap, shape, prt=None):
        s = wsl[wsl_i[0]]
        wsl_i[0] = (wsl_i[0] + 1) % NW
        n = int(np.prod(shape[1:]))
        p0, p1 = prt if prt is not None else (0, shape[0])
        view = s.ap[p0:p1, 0:n]
        if len(shape) == 3:
            view = view.rearrange("p (a b) -> p a b", a=shape[1])
        elif len(shape) == 4:
            view = view.rearrange("p (a b c) -> p a b c", a=shape[1], b=shape[2])
        B.dma("sp", view, dram_ap, R=[wdep[(name, l)]], W=[s])
        return view, s

    def wcols(name, l, c0, n, kc):
        ap = wb[name][l, 0:kc * 128, c0:c0 + n].rearrange("(k p) n -> p k n", p=128)
        return wload(name, l, ap, [128, kc, n])

    def rms(src3, nk, ktot, l, gname, out_fn, R, Wd, eps=EPS, ps=None):
        B.A(sqb3[:, 0:nk, :], src3, AF.Square, R, [sqb])
        p = psd()
        for k in range(nk):
            B.mm(p[:, 0:T], ones_bf, sqb3[:, k, :], [sqb, cbf], [p], start=(k == 0), stop=(k == nk - 1))
        B.A(rt.ap, p[:, 0:T], AF.Sqrt, [p], [rt], scale=1.0 / ktot, bias=eps_ap(eps))
        B.rcp(rt2.ap, rt.ap, [rt], [rt2])

    eps_t = sb("eps_t", [128, 4], F32)
    B.ms("dve", eps_t[:, 0:1], EPS, [eps_t])
    B.ms("dve", eps_t[:, 1:2], LN_EPS, [eps_t])
    B.ms("dve", eps_t[:, 2:3], 0.0, [eps_t])
    B.ms("dve", eps_t[:, 3:4], 0.25, [eps_t])

    def eps_ap(e):
        return eps_t[:, 0:1] if e == EPS else eps_t[:, 1:2]

    zq = al("mla", "zq", 2 * T, F32)
    zq3 = v3(zq.ap, 2)
    zqn = al("mla", "zqn", 2 * T, BF16)
    zqn3 = v3(zqn.ap, 2)
    qn = [al("mla", "qn%d" % i, T, BF16) for i in range(2)]
    Qabs = al("mla", "Qabs", 4 * T, BF16)
    Qabs3 = v3(Qabs.ap, 4)
    Qrope = al("mla", "Qrope", 4 * T, BF16)
    Qrope3 = v3(Qrope.ap, 4)
    zkv = al("mla", "zkv", T, F32)
    cosT = al("mla", "cosT", T, F32)
    sinT = al("mla", "sinT", T, F32)
    posi = al("mla", "posi", T, F32)
    posf = al("mla", "posf", T, F32)
    tq = al("mla", "tq", T, F32)
    tq2 = al("mla", "tq2", T, F32)
    rp1 = al("mla", "rp1", T, F32)
    rp2 = al("mla", "rp2", T, F32)
    PTm = [al("mla", "PTm%d" % i, 2 * T, BF16) for i in range(2)]
    rinv = al("mla", "rinv", 2 * T, F32)
    On = al("mla", "On", 2 * T, BF16)
    Qs = al("rw", "Qs", 4 * T, BF16)
    Qs3 = v3(Qs.ap, 4)
    PTc = [al("rw", "PTc%d" % i, 512, BF16) for i in range(2)]
    rinvc = al("rw", "rinvc", 512, F32)
    hT = al("ffn", "hT", 8 * T, F32)
    hT3 = v3(hT.ap, 8)
    mo = al("ffn", "mo", 8 * T, F32)
    mo3 = v3(mo.ap, 8)
    aT = al("ffn", "aT", 32 * T, BF16)
    aT3 = v3(aT.ap, 32)
    rl = [al("ffn", "rl%d" % i, T, F32) for i in range(2)]
    mrg = al("ffn", "mrg", 8 * T, F32)
    mrg3 = v3(mrg.ap, 8)
    mrgb = al("ffn", "mrgb", 8 * T, BF16)
    mrgb3 = v3(mrgb.ap, 8)
    gsb = [al("ffn", "gsb%d" % i, T, F32) for i in range(2)]
    gsb8 = [al("ffn", "gsb8_%d" % i, T, F32) for i in range(8)]
    gcnt = [0]
    tmpf = [al("ffn", "tmpf%d" % i, T, F32) for i in range(2)]
    pb = sb("pb", [128, 2 * T], BF16)
    pb3 = v3(pb.ap, 2)
    zb = [al("rw", "zb%d" % i, T + 2, F32) for i in range(2)]
    dd = al("rw", "dd", T, F32)
    zrs = [al("rw", "zr%d" % i, T, F32) for i in range(2)]
    zks = [al("rw", "zk%d" % i, T, F32) for i in range(2)]
    zs12 = zrs[0]
    zs13 = zks[0]
    zvs = [al("rw", "zv%d" % i, T, F32) for i in range(2)]
    txw = al("rw", "txw", T, BF16)
    xab = al("rw", "xab", T, BF16)
    sgb = al("rw", "sgb", T, BF16)
    tmps = []
    for i_ in range(2):
        tmp = {n: al("rw", n + str(i_), T, F32) for n in ("sgw", "cs", "csc", "dC", "E1", "aa", "kk", "ssm", "rs", "tb", "tc", "km")}
        tmp["csx"] = tmp["cs"]
        tmp["E3"] = tmp["cs"]
        tmp["E2"] = tmp["csc"]
        tmp["E4"] = tmp["dC"]
        tmp["kkn"] = tmp["kk"]
        tmps.append(tmp)
    kk2s = [al("rw", "kk2%d" % i, T, BF16) for i in range(2)]
    rkrs = [al("rw", "rkr%d" % i, T, BF16) for i in range(2)]
    vbfs = [al("rw", "vbf%d" % i, T, BF16) for i in range(2)]
    bhs = [al("rw", "bh%d" % i, T, BF16) for i in range(2)]
    khs = [al("rw", "kh%d" % i, T, BF16) for i in range(2)]
    aTt = al("rw", "aTt", 4 * T, BF16)
    bTt = al("rw", "bTt", 4 * T, BF16)
    kTt = al("rw", "kTt", 4 * T, BF16)
    rTt = al("rw", "rTt", 4 * T, BF16)
    aT_3, bT_3, kT_3, rT_3 = (v3(x.ap, 4) for x in (aTt, bTt, kTt, rTt))
    TM = al("rw", "TM", 3 * 2 * 4 * 128, BF16)
    TM5 = TM.ap.rearrange("p (j c h n) -> p j c h n", j=3, c=2, h=4)
    AM = al("rw", "AM", 2 * 4 * 4 * 2 * 128, BF16)
    AM6 = AM.ap.rearrange("p (c h t e n) -> p c h t e n", c=2, h=4, t=4, e=2)
    AM_d = [[Dep() for _ in range(4)] for _ in range(2)]
    for cc_ in range(2):
        for hh_ in range(4):
            AM_d[cc_][hh_].al = AM.d.al
    Qd = [al("rw", "Qd%d" % i, 4 * 128, BF16) for i in range(2)]
    QTd = [al("rw", "QTd%d" % i, 4 * 128, BF16) for i in range(2)]
    Xd = [al("rw", "Xd%d" % i, 4 * 128, F32) for i in range(2)]
    Xbbs = [al("rw", "Xbb%d" % i, 4 * 128, BF16) for i in range(2)]
    yb = al("rw", "y", 4 * T, F32)
    y3 = v3(yb.ap, 4)
    bonus = al("rw", "bonus", 4 * T, F32)
    bonus3 = v3(bonus.ap, 4)
    gbuf = al("rw", "g", 4 * T, BF16)
    g3 = v3(gbuf.ap, 4)
    Smid = al("rw", "Smid", 256, F32)
    Smb = al("rw", "Smb", 256, BF16)
    W0b = al("rw", "W0b", 512, BF16)
    Ut = al("rw", "Ut", 512, BF16)
    lwa = al("rw", "lwa", 512, BF16)
    lwg = al("rw", "lwg", 512, BF16)
    ybf = al("rw", "ybf", T, BF16)
    yc = al("rw", "yc", T, F32)
    ysq = al("rw", "ysq", T, BF16)
    sd = al("rw", "sd", T, F32)
    t1 = al("rw", "t1", T, F32)
    t2 = al("rw", "t2", T, F32)

    def layer(l):
        src = xT if l == 0 else hscr[l - 1]
        dst = outT if l == nl - 1 else hscr[l]
        src_d = None if l == 0 else hscr_d[l - 1]
        dst_d = None if l == nl - 1 else hscr_d[l]
        B.ms("dve", Sst.ap, 0.0, [Sst])
        B.ms("dve", zlast.ap, 0.0, [zlast])
        etap = mo.ap[0:8, 0:768]
        rl_ap = mo.ap[0:8, 768:768 + 320]
        B.dma("sp", rl_ap, relr[l], W=[mo])
        B.cp("dve", etap[:, 383:703], rl_ap, [mo], [mo])
        B.cp("dve", etap[:, 0:383], rl_ap[:, 0:1].to_broadcast([8, 383]), [mo], [mo])
        B.cp("dve", etap[:, 703:768], rl_ap[:, 319:320].to_broadcast([8, 65]), [mo], [mo])
        B.dma("sp", ext[l], etap, R=[mo], W=[ext_d[l]])
        for r in range(5):
            for h in range(8):
                off = 639 - 128 * r - 127
                srcap = bass.AP(tensor=ext.tensor, offset=(l * 8 + h) * 768 + off, ap=[[1, 128], [1, 128]])
                B.dma("pool", Xb3[:, r * 8 + h, :], srcap, R=[ext_d[l]], W=[Xb])
        for h in range(8):
            B.ms("pool", Xb3[64:128, 0 * 8 + h, 64:128], NEG, [Xb])
            B.ms("pool", Xb3[0:64, 4 * 8 + h, 0:64], NEG, [Xb])

        for tt in range(NT):
            tile(l, tt, src, dst, src_d, dst_d)

    def mark(name, l, tt):
        PHASES.append((name, l, tt, getattr(B, "pes", 0)))

    def tile(l, tt, src, dst, src_d, dst_d):
        t0 = tt * T
        mark("start", l, tt)
        R_src = [] if src_d is None else [src_d[tt]]
        if tt == 0:
            B.dma("sp", mo3, src[:, t0:t0 + T].rearrange("(k p) t -> p k t", p=128), R=R_src, W=[mo])
        B.dma("pool", pb3, pT[l, :, t0:t0 + T].rearrange("(k p) t -> p k t", p=128), W=[pb])
        rot["n"] = 8
        rms(mo3, 8, D, l, "pre_mix_g", None, [mo], None)
        for k in range(8):
            B.stt("dve", uT3[:, k, :], mo3[:, k, :], vcol(l, "pre_mix_g", k), rt2.ap, ALU.mult, ALU.mult,
                  [mo, vec[l], rt2], [uT])

        def fin():
            W_dst = [] if dst_d is None else [dst_d[tt]]
            B.dma("sp", dst[:, t0:t0 + T].rearrange("(k p) t -> p k t", p=128), hT3, R=[hT], W=W_dst)

        if stage <= 0:
            return fin()

        def zmm(p, wv, ws, c0, n, M=None):
            for k in range(8):
                B.mm(p[0:n, 0:T], wv[:, k, c0:c0 + n], uT3[:, k, :], [ws, uT], [p], start=(k == 0), stop=(k == 7))

        mark("mla", l, tt)
        wv, ws = wcols("w_in", l, 0, 448, 8)
        wsw, wsws = wcols("w_in_sw", l, 0, 64, 8)
        for c in range(2):
            p = psd()
            zmm(p, wv, ws, c * 128, 128)
            B.cp("act", zq3[:, c, :], p[:, 0:T], [p], [zq])
        p = psd()
        zmm(p, wv, ws, 256, 128)
        B.cp("act", zkv.ap, p[:, 0:T], [p], [zkv])
        B.dma("sp", posi.ap[0:64, :].bitcast(I32), pos[0:1, t0:t0 + T].to_broadcast([64, T]), W=[posi])
        B.cp("dve", posf[0:64, :], posi.ap[0:64, :].bitcast(I32), [posi], [posf])
        B.ts("dve", tq[0:64, :], posf[0:64, :], cst[0:64, CI_INVF:CI_INVF + 1], ALU.mult, [posf, cst], [tq],
             s2=float(1.0 / (2 * np.pi)), op1=ALU.mult)
        MAGIC = 12582912.0
        for (dstb, shift) in ((sinT, 0.0), (cosT, 0.25)):
            if shift != 0.0:
                B.ts("dve", tq2[0:64, :], tq[0:64, :], shift, ALU.add, [tq], [tq2])
                srcq = tq2
            else:
                srcq = tq
            B.ts("dve", rp1[0:64, :], srcq[0:64, :], MAGIC, ALU.add, [srcq], [rp1], s2=MAGIC, op1=ALU.subtract)
            B.tt("dve", rp2[0:64, :], srcq[0:64, :], rp1[0:64, :], ALU.subtract, [srcq, rp1], [rp2])
            if shift == 0.0:
                B.A(dstb[0:64, :], rp2[0:64, :], AF.Sin, [rp2, cst], [dstb], scale=cst[0:64, CI_SGN:CI_SGN + 1])
            else:
                B.A(dstb[0:64, :], rp2[0:64, :], AF.Sin, [rp2], [dstb], scale=TWO_PI)
        p1 = psd()
        zmm(p1, wv, ws, 384, 64)
        p2 = psd()
        zmm(p2, wsw, wsws, 0, 64)
        B.tt("dve", rp1[0:64, :], p1[0:64, 0:T], cosT[0:64, :], ALU.mult, [p1, cosT], [rp1])
        B.tt("dve", rp2[0:64, :], p2[0:64, 0:T], sinT[0:64, :], ALU.mult, [p2, sinT], [rp2])
        B.tt("dve", Kr[0:64, t0:t0 + T], rp1[0:64, :], rp2[0:64, :], ALU.add, [rp1, rp2], [Kc_d[tt]])
        rms(zkv.ap.rearrange("p (a b) -> p a b", a=1), 1, 128, l, "kv_g", None, [zkv], None)
        B.stt("dve", Kc[:, t0:t0 + T], zkv.ap, vcol(l, "kv_g", 0), rt2.ap, ALU.mult, ALU.mult, [zkv, vec[l], rt2],
              [Kc_d[tt]])
        pt = PS[7]
        ptb = pt.ap.bitcast(BF16)
        for i in range(2):
            B.tr(ptb[:, i * 128:(i + 1) * 128], Kc[:, t0 + i * 128:t0 + (i + 1) * 128], ident_bf, [Kc_d[tt], cbf], [pt])
        B.cp("act", Vc[:, (2 * tt) * 128:(2 * tt + 2) * 128], ptb[:, 0:256], [pt], [Kc_d[tt]])
        rms(zq3, 2, 256, l, "q_g", None, [zq], None)
        for c in range(2):
            B.stt("dve", zqn3[:, c, :], zq3[:, c, :], vcol(l, "q_g", c), rt2.ap, ALU.mult, ALU.mult,
                  [zq, vec[l], rt2], [zqn])
        wq = wb["w_uq"][l].rearrange("(k p) n -> p k n", p=128)
        wqv, wqs = wload("w_uq", l, wq, [128, 2, 768])
        wqsw = wb["w_uq_sw"][l].rearrange("(k p) n -> p k n", p=128)
        wqswv, wqsws = wload("w_uq_sw", l, wqsw, [128, 2, 256])
        wkt = wb["w_ukT"][l].rearrange("(h p) n -> p h n", p=128)
        wktv, wkts = wload("w_ukT", l, wkt, [128, 4, 128])
        for h in range(4):
            p = psd()
            for k in range(2):
                B.mm(p[:, 0:T], wqv[:, k, h * 192:h * 192 + 128], zqn3[:, k, :], [wqs, zqn], [p], start=(k == 0),
                     stop=(k == 1))
            q_ = qn[h % 2]
            B.cp("act", q_.ap, p[:, 0:T], [p], [q_])
            p = psd()
            B.mm(p[:, 0:T], wktv[:, h, :], q_.ap, [wkts, q_], [p])
            B.cp("act", Qabs3[:, h, :], p[:, 0:T], [p], [Qabs])
            p1 = psd()
            for k in range(2):
                B.mm(p1[0:64, 0:T], wqv[:, k, h * 192 + 128:h * 192 + 192], zqn3[:, k, :], [wqs, zqn], [p1],
                     start=(k == 0), stop=(k == 1))
            p2 = psd()
            for k in range(2):
                B.mm(p2[0:64, 0:T], wqswv[:, k, h * 64:(h + 1) * 64], zqn3[:, k, :], [wqsws, zqn], [p2],
                     start=(k == 0), stop=(k == 1))
            B.tt("dve", rp1[0:64, :], p1[0:64, 0:T], cosT[0:64, :], ALU.mult, [p1, cosT], [rp1])
            B.tt("dve", rp2[0:64, :], p2[0:64, 0:T], sinT[0:64, :], ALU.mult, [p2, sinT], [rp2])
            B.tt("dve", Qrope3[0:64, h, :], rp1[0:64, :], rp2[0:64, :], ALU.add, [rp1, rp2], [Qrope])
        rot["n"] = 4
        mark("mla_attn", l, tt)
        wkv = wb["w_ukv"][l]
        wkvv, wkvs = wload("w_ukv", l, wkv, [128, 1024])
        nkb = 2 * (tt + 1)
        Kdeps = [Kc_d[j // 2] for j in range(nkb)]
        for hp in range(2):
            Ob, Sb = PS[4], PS[5]
            O3 = v3(Ob.ap, 2)
            S3 = v3(Sb.ap, 2)
            def m_scores(j):
                jd = j - 2 * tt
                q0 = max(jd, 0) * 128
                ps_ = PS[j % 2 + 2]
                ps3 = v3(ps_.ap, 2)
                for hh in range(2):
                    h = 2 * hp + hh
                    B.mm(ps3[:, hh, q0:T], Kc[:, j * 128:(j + 1) * 128], Qabs3[:, h, q0:T], [Kdeps[j], Qabs], [ps_],
                         start=True, stop=False)
                    B.mm(ps3[:, hh, q0:T], Kr[0:64, j * 128:(j + 1) * 128], Qrope3[0:64, h, q0:T],
                         [Kdeps[j], Qrope], [ps_], start=False, stop=True)

            def m_rest(j):
                jd = j - 2 * tt
                q0 = max(jd, 0) * 128
                ps_ = PS[j % 2 + 2]
                ps3 = v3(ps_.ap, 2)
                PT = PTm[j % 2]
                PT3 = v3(PT.ap, 2)
                B.A(PT3[:, :, q0:T], ps3[:, :, q0:T], AF.Exp, [ps_], [PT], scale=MLA_SCALE)
                if jd >= 0:
                    B.ms("pool", PT3[64:128, :, q0:q0 + 64], 0.0, [PT])
                for hh in range(2):
                    B.mm(O3[:, hh, q0:T], Vc3[:, j, :], PT3[:, hh, q0:T], [Kdeps[j], PT], [Ob],
                         start=(j == 0 and hh == 0), stop=(j == nkb - 1))
                for hh in range(2):
                    B.mm(S3[:, hh, q0:T], ones_bf, PT3[:, hh, q0:T], [cbf, PT], [Sb],
                         start=(j == 0 and hh == 0), stop=(j == nkb - 1))

            m_scores(0)
            for j in range(nkb):
                if j + 1 < nkb:
                    m_scores(j + 1)
                m_rest(j)
            B.rcp(rinv.ap, Sb.ap, [Sb], [rinv])
            B.tt("dve", On.ap, Ob.ap, rinv.ap, ALU.mult, [Ob, rinv], [On])
            On3 = v3(On.ap, 2)
            for hh in range(2):
                h = 2 * hp + hh
                p = psd()
                B.mm(p[:, 0:T], wkvv[:, h * 256 + 128:h * 256 + 256], On3[:, hh, :], [wkvs, On], [p])
                B.cp("act", o_mla[:, h * T:(h + 1) * T], p[:, 0:T], [p], [o_mla])

        if stage <= 1:
            return fin()
        mark("ca", l, tt)
        wv, ws = wcols("w_in", l, 2240, 512, 8)
        for c in range(4):
            p = psd()
            zmm(p, wv, ws, c * 128, 128)
            B.A(Qs3[:, c, :], p[:, 0:T], AF.Copy, [p], [Qs], scale=0.125)
        wv, ws = wcols("w_in", l, 2752, 512, 8)
        sl0 = (2 * tt) % 8
        for c in range(4):
            p = psd()
            zmm(p, wv, ws, c * 128, 128)
            B.cp("act", CK3[:, c, sl0 * 128:sl0 * 128 + T], p[:, 0:T], [p], [CK_d[sl0 // 2]])
        wv, ws = wcols("w_in", l, 3264, 512, 8)
        for i in range(2):
            p = psd()
            for k in range(8):
                B.mm(p.ap, uT3[:, k, i * 128:(i + 1) * 128], wv[:, k, :], [uT, ws], [p], start=(k == 0), stop=(k == 7))
            B.cp("act", CV3[:, sl0 + i, :], p.ap, [p], [CV_d[sl0 + i]])
        def ca_attn():
            for i in range(2):
                qb = 2 * tt + i
                Ob, Sb = PS[4], PS[5]
                O3 = v3(Ob.ap, 4)
                S3 = v3(Sb.ap, 4)
                bl = list(range(max(0, qb - 4), qb + 1))
                units = [(b, e) for b in bl for e in range(2)]

                def c_scores(b, e):
                    r = qb - b
                    slot = b % 8
                    ps_ = PS[2 + e]
                    ps3 = v3(ps_.ap, 4)
                    pb_ = e * 64
                    for ch in range(4):
                        h = ch * 2 + e
                        B.mm(ps3[:, ch, :], CK3[pb_:pb_ + 64, ch, slot * 128:(slot + 1) * 128],
                             Qs3[pb_:pb_ + 64, ch, i * 128:(i + 1) * 128], [CK_d[slot // 2], Qs], [ps_],
                             start=True, stop=False)
                        B.mm(ps3[:, ch, :], Xb3[:, r * 8 + h, :], J_bf, [Xb, cbf], [ps_], start=False, stop=True)

                def c_rest(b, e):
                    slot = b % 8
                    ps_ = PS[2 + e]
                    PT = PTc[e]
                    PT3 = v3(PT.ap, 4)
                    pb_ = e * 64
                    B.A(PT.ap, ps_.ap, AF.Exp, [ps_], [PT])
                    for ch in range(4):
                        h = ch * 2 + e
                        first = (b == bl[0] and ch == 0)
                        B.mm(O3[pb_:pb_ + 64, ch, :], CV3[:, slot, h * 64:(h + 1) * 64], PT3[:, ch, :],
                             [CV_d[slot], PT], [Ob], start=first, stop=True)
                    for ch in range(4):
                        first = (b == bl[0] and ch == 0)
                        B.mm(S3[pb_:pb_ + 64, ch, :], ones_bf[:, 0:64], PT3[:, ch, :], [cbf, PT], [Sb],
                             start=first, stop=True)

                c_scores(*units[0])
                for ui, u_ in enumerate(units):
                    if ui + 1 < len(units):
                        c_scores(*units[ui + 1])
                    c_rest(*u_)
                    yield 1
                B.rcp(rinvc.ap, Sb.ap, [Sb], [rinvc])
                B.tt("dve", v3(o_ca.ap, 4)[:, :, i * 128:(i + 1) * 128], O3, v3(rinvc.ap, 4), ALU.mult, [Ob, rinvc],
                     [o_ca])


        if stage <= 2:
            for _ in ca_attn():
                pass
            return fin()
        mark("rw", l, tt)
        rwkv(l, tt, ca_attn())
        if dbg and l == 0 and tt == 0:
            for n, bsrc in (("aT", aTt), ("bT", bTt), ("kT", kTt), ("rT", rTt), ("y", yb), ("bonus", bonus), ("g", gbuf),
                            ("TM", TM), ("AM", AM), ("Sst", Sst), ("small", small)):
                if n not in dbg_t:
                    dbg_t[n] = dram("dbg_" + n, [128, bsrc.ap.shape[1]], bsrc.ap.dtype, "ExternalOutput")
                B.dma("sp", dbg_t[n], bsrc.ap, R=[bsrc] + ([AM_d[c_][h_] for c_ in range(2) for h_ in range(4)] if n == "AM" else []))
        if stage <= 3 or (stage >= 21 and stage <= 26):
            return fin()

        if dbg and l == 0:
            for n, bsrc in (("o_mla", o_mla), ("o_rw", o_rw), ("o_ca", o_ca)):
                B.dma("sp", dbg_t[n][:, t0:t0 + T].rearrange("(k p) t -> p k t", p=128), v3(bsrc.ap, 4), R=[bsrc])

        rot["n"] = 8
        mark("merge", l, tt)
        obr = (o_mla, o_rw, o_ca)
        for cg in range(2):
            for n in range(3):
                gv, gs = wcols("w_in", l, 3776 + n * 1024 + cg * 512, 512, 8)
                bap = wb["w_branch"][l, n * 512:(n + 1) * 512, cg * 512:(cg + 1) * 512].rearrange(
                    "(k p) n -> p k n", p=128)
                bv, bs = wload("w_branch", l, bap, [128, 4, 512])
                ob3 = v3(obr[n].ap, 4)
                gbs = []
                for cl in range(4):
                    pg = psd()
                    zmm(pg, gv, gs, cl * 128, 128)
                    gb = gsb8[gcnt[0] % 8]
                    gcnt[0] += 1
                    B.A(gb.ap, pg[:, 0:T], AF.Sigmoid, [pg], [gb])
                    gbs.append(gb)
                for cl in range(4):
                    c = cg * 4 + cl
                    gb = gbs[cl]
                    py = psd()
                    for k in range(4):
                        B.mm(py[:, 0:T], bv[:, k, cl * 128:(cl + 1) * 128], ob3[:, k, :], [bs, obr[n]], [py],
                             start=(k == 0), stop=(k == 3))
                    if n == 0:
                        B.tt("dve", mrg3[:, c, :], py[:, 0:T], gb.ap, ALU.mult, [py, gb], [mrg])
                    else:
                        tf = tmpf[(c * 3 + n) % 2]
                        B.tt("dve", tf.ap, py[:, 0:T], gb.ap, ALU.mult, [py, gb], [tf])
                        if n == 1:
                            B.tt("pool", mrg3[:, c, :], mrg3[:, c, :], tf.ap, ALU.add, [mrg, tf], [mrg])
                        else:
                            B.tt("pool", mrgb3[:, c, :], mrg3[:, c, :], tf.ap, ALU.add, [mrg, tf], [mrgb])
        B.dma("sp", hT3, src[:, t0:t0 + T].rearrange("(k p) t -> p k t", p=128), R=R_src, W=[hT])
        for cg in range(2):
            wv, ws = wcols("w_out", l, cg * 512, 512, 8)
            for cl in range(4):
                c = cg * 4 + cl
                p = psd()
                for k in range(8):
                    B.mm(p[:, 0:T], wv[:, k, cl * 128:(cl + 1) * 128], mrgb3[:, k, :], [ws, mrgb], [p], start=(k == 0),
                         stop=(k == 7))
                B.cp("act", mo3[:, c, :], p[:, 0:T], [p], [mo])
        rms(mo3, 8, D, l, "post_mix_g", None, [mo], None)
        for k in range(8):
            tf = tmpf[k % 2]
            B.stt("dve", tf.ap, mo3[:, k, :], vcol(l, "post_mix_g", k), rt2.ap, ALU.mult, ALU.mult,
                  [mo, vec[l], rt2], [tf])
            B.tt("pool", hT3[:, k, :], hT3[:, k, :], tf.ap, ALU.add, [hT, tf], [hT])
        mark("ffn", l, tt)
        rms(hT3, 8, D, l, "pre_ff_g", None, [hT], None)
        for k in range(8):
            B.stt("dve", uT3[:, k, :], hT3[:, k, :], vcol(l, "pre_ff_g", k), rt2.ap, ALU.mult, ALU.mult,
                  [hT, vec[l], rt2], [uT])
        for cg in range(8):
            wv, ws = wcols("w_ff1", l, cg * 512, 512, 8)
            for cl in range(4):
                j = cg * 4 + cl
                p = psd()
                zmm(p, wv, ws, cl * 128, 128)
                r_ = rl[j % 2]
                B.A(r_.ap, p[:, 0:T], AF.Relu, [p], [r_])
                B.tt("pool", aT3[:, j, :], r_.ap, r_.ap, ALU.mult, [r_], [aT])
        rot["n"] = 4
        for cg in range(2):
            accs = [PS[4 + i] for i in range(4)]
            for kg in range(4):
                wap = wb["w_ff2"][l, kg * 1024:(kg + 1) * 1024, cg * 512:(cg + 1) * 512].rearrange(
                    "(k p) n -> p k n", p=128)
                wv, ws = wload("w_ff2", l, wap, [128, 8, 512])
                for cl in range(4):
                    for kk in range(8):
                        B.mm(accs[cl][:, 0:T], wv[:, kk, cl * 128:(cl + 1) * 128], aT3[:, kg * 8 + kk, :], [ws, aT],
                             [accs[cl]], start=(kg == 0 and kk == 0), stop=(kg == 3 and kk == 7))
            for cl in range(4):
                B.cp("act", mo3[:, cg * 4 + cl, :], accs[cl][:, 0:T], [accs[cl]], [mo])
        rms(mo3, 8, D, l, "post_ff_g", None, [mo], None)
        for k in range(8):
            tf = tmpf[k % 2]
            B.stt("dve", tf.ap, mo3[:, k, :], vcol(l, "post_ff_g", k), rt2.ap, ALU.mult, ALU.mult,
                  [mo, vec[l], rt2], [tf])
            B.tt("pool", hT3[:, k, :], hT3[:, k, :], tf.ap, ALU.add, [hT, tf], [hT])
        rot["n"] = 8
        mark("ple", l, tt)
        if tt + 1 < NT:
            R_nx = [] if src_d is None else [src_d[tt + 1]]
            B.dma("sp", mo3, src[:, t0 + T:t0 + 2 * T].rearrange("(k p) t -> p k t", p=128), R=R_nx, W=[mo])
        B.cp("act", uT.ap, hT.ap, [hT], [uT])
        ppv, pps = wload("w_ple_proj", l, wb["w_ple_proj"][l].rearrange("(k p) n -> p k n", p=128), [128, 2, 1024])
        for cg in range(2):
            wv, ws = wcols("w_ple_gate", l, cg * 512, 512, 8)
            for cl in range(4):
                c = cg * 4 + cl
                pg = psd()
                zmm(pg, wv, ws, cl * 128, 128)
                gb = gsb[c % 2]
                B.A(gb.ap, pg[:, 0:T], AF.Sigmoid, [pg], [gb])
                pp = psd()
                for k in range(2):
                    B.mm(pp[:, 0:T], ppv[:, k, c * 128:(c + 1) * 128], pb3[:, k, :], [pps, pb], [pp], start=(k == 0),
                         stop=(k == 1))
                tf = tmpf[c % 2]
                B.tt("dve", tf.ap, pp[:, 0:T], gb.ap, ALU.mult, [pp, gb], [tf])
                B.tt("pool", hT3[:, c, :], hT3[:, c, :], tf.ap, ALU.add, [hT, tf], [hT])
        W_dst = [] if dst_d is None else [dst_d[tt]]
        B.dma("sp", dst[:, t0:t0 + T].rearrange("(k p) t -> p k t", p=128), hT3, R=[hT], W=W_dst)
        rot["n"] = 4

    def rwkv(l, tt, ca):
        zi = [0]

        def zchunk(wv, ws, c0, cidx, dest):
            p = psd()
            for k in range(8):
                B.mm(p[:, 0:T], wv[:, k, c0:c0 + 128] if c0 is not None else wv[:, k, :], uT3[:, k, :], [ws, uT], [p],
                     start=(k == 0), stop=(k == 7))
            z = zb[zi[0] % 2]
            zi[0] += 1
            B.cp("act", z[:, 1:T + 1], p[:, 0:T], [p], [z])
            B.cp("pool", z[:, 0:1], zlast[:, cidx:cidx + 1], [zlast], [z])
            B.tt("dve", dd.ap, z[:, 0:T], z[:, 1:T + 1], ALU.subtract, [z], [dd])
            B.stt("dve", dest.ap, dd.ap, vcol(l, "mu", cidx), z[:, 1:T + 1], ALU.mult, ALU.add, [dd, vec[l], z], [dest])
            B.cp("pool", zlast[:, cidx:cidx + 1], z[:, T:T + 1], [z], [zlast])

        wv, ws = wcols("w_in", l, 448 + 1536, 256, 8)
        zchunk(wv, ws, 0, 12, zs12)
        zchunk(wv, ws, 128, 13, zs13)
        B.A(txw[0:64, :], zs12[0:64, :], AF.Tanh, [zs12], [txw])
        B.cp("act", xab[64:128, :], zs12[64:128, :], [zs12], [xab])
        B.A(sgb.ap, zs13.ap, AF.Sigmoid, [zs13], [sgb])
        if stage == 21:
            return
        s_wa = lwa
        B.dma("sp", s_wa.ap[0:64, 0:512], wb["rw_w_up"][l], R=[wdep[("rw_w_up", l)]], W=[s_wa])
        B.dma("sp", s_wa.ap[64:128, 0:512], wb["rw_a_up"][l], R=[wdep[("rw_a_up", l)]], W=[s_wa])
        gups = lwg
        gupv = lwg.ap
        B.dma("sp", gupv, wb["rw_g_up"][l], R=[wdep[("rw_g_up", l)]], W=[lwg])
        sm = small

        def stageA(hp):
            qs = hp % 2
            s4 = wsl[wsl_i[0]]
            wsl_i[0] = (wsl_i[0] + 1) % NW
            wv4 = s4.ap[:, 0:3072].rearrange("p (j k n) -> p j k n", j=3, k=8)
            ws4 = s4
            for j_ in range(3):
                c0_ = 448 + j_ * 512 + hp * 128
                B.dma("sp", wv4[:, j_, :, :], wb["w_in"][l, :, c0_:c0_ + 128].rearrange("(k p) n -> p k n", p=128),
                      R=[wdep[("w_in", l)]], W=[s4])
            zchunk(wv4[:, 0, :, :], ws4, None, hp, zrs[qs])
            yield 1
            zchunk(wv4[:, 1, :, :], ws4, None, 4 + hp, zks[qs])
            yield 1
            zchunk(wv4[:, 2, :, :], ws4, None, 8 + hp, zvs[qs])
            yield 1
            X = tmps[qs]
            cols = slice(hp * 128, (hp + 1) * 128)
            p = psd()
            B.mm(p[:, 0:T], s_wa.ap[0:64, cols], txw[0:64, :], [s_wa, txw], [p])
            B.A(X["sgw"].ap, p[:, 0:T], AF.Sigmoid, [p, vec[l]], [X["sgw"]], bias=vcol(l, "w0", hp))
            p = psd()
            B.mm(p[:, 0:T], s_wa.ap[64:128, cols], xab[64:128, :], [s_wa, xab], [p])
            B.A(X["aa"].ap, p[:, 0:T], AF.Sigmoid, [p, vec[l]], [X["aa"]], bias=vcol(l, "a0", hp))
            p = psd()
            B.mm(p[:, 0:T], gupv[:, cols], sgb.ap, [gups, sgb], [p])
            B.cp("act", g3[:, hp, :], p[:, 0:T], [p], [gbuf])
            yield 1
            cs = X["cs"]
            onec = cst[:, CI_ONE:CI_ONE + 128]
            for c in range(2):
                sl_ = slice(c * 128, (c + 1) * 128)
                B.op("dve", lambda e, sl_=sl_: e.tensor_tensor_scan(X["cs"][:, sl_], onec, X["sgw"][:, sl_], 0.0,
                                                                    ALU.mult, ALU.add), [cst, X["sgw"]], [X["cs"]])
            for c in range(2):
                B.ts("dve", X["csc"][:, c * 128:(c + 1) * 128], cs[:, c * 128:(c + 1) * 128],
                     cs[:, c * 128 + 63:c * 128 + 64], ALU.subtract, [cs], [X["csc"]])
            for c in range(2):
                B.cp("pool", sm[:, hp * 2 + c:hp * 2 + c + 1], cs[:, c * 128 + 63:c * 128 + 64], [cs], [sm])
                B.cp("pool", sm[:, 8 + hp * 2 + c:8 + hp * 2 + c + 1], X["csc"][:, c * 128 + 127:c * 128 + 128],
                     [X["csc"]], [sm])
            B.tt("dve", X["csx"].ap, X["csc"].ap, X["sgw"].ap, ALU.subtract, [X["csc"], X["sgw"]], [X["csx"]])
            for c in range(2):
                B.ts("dve", X["dC"][:, c * 128:(c + 1) * 128], X["csc"][:, c * 128:(c + 1) * 128],
                     X["csc"][:, c * 128 + 127:c * 128 + 128], ALU.subtract, [X["csc"]], [X["dC"]])
            B.A(X["E1"].ap, X["csc"].ap, AF.Exp, [X["csc"]], [X["E1"]], scale=-CC)
            B.A(X["E2"].ap, X["csc"].ap, AF.Exp, [X["csc"]], [X["E2"]], scale=CC)
            B.A(X["E3"].ap, X["csx"].ap, AF.Exp, [X["csx"]], [X["E3"]], scale=-CC)
            B.A(X["E4"].ap, X["dC"].ap, AF.Exp, [X["dC"]], [X["E4"]], scale=CC)
            yield 1
            B.ts("dve", X["kk"].ap, zks[qs].ap, vcol(l, "k_k", hp), ALU.mult, [zks[qs], vec[l]], [X["kk"]])
            B.A(kk2s[qs].ap, X["kk"].ap, AF.Square, [X["kk"]], [kk2s[qs]])
            p = psd()
            B.mm(p[:, 0:T], blk_bf, kk2s[qs].ap, [cbf, kk2s[qs]], [p])
            B.ts("dve", X["ssm"].ap, p[:, 0:T], 1e-24, ALU.max, [p], [X["ssm"]])
            B.A(X["rs"].ap, X["ssm"].ap, AF.Sqrt, [X["ssm"]], [X["rs"]])
            B.rcp(X["ssm"].ap, X["rs"].ap, [X["rs"]], [X["ssm"]])
            B.tt("dve", X["kkn"].ap, X["kk"].ap, X["ssm"].ap, ALU.mult, [X["kk"], X["ssm"]], [X["kkn"]])
            yield 1
            B.stt("dve", aT_3[:, hp, :], X["kkn"].ap, -1.0, X["E3"].ap, ALU.mult, ALU.mult, [X["kkn"], X["E3"]], [aTt])
            B.tt("dve", X["tb"].ap, X["kkn"].ap, X["aa"].ap, ALU.mult, [X["kkn"], X["aa"]], [X["tb"]])
            B.tt("dve", bT_3[:, hp, :], X["tb"].ap, X["E2"].ap, ALU.mult, [X["tb"], X["E2"]], [bTt])
            B.tt("pool", bhs[qs].ap, X["tb"].ap, X["E4"].ap, ALU.mult, [X["tb"], X["E4"]], [bhs[qs]])
            B.ts("dve", X["tc"].ap, X["aa"].ap, vcol(l, "k_a", hp), ALU.mult, [X["aa"], vec[l], omka[l]], [X["tc"]],
                 s2=omka[l][:, hp:hp + 1], op1=ALU.add)
            B.tt("dve", X["km"].ap, zks[qs].ap, X["tc"].ap, ALU.mult, [zks[qs], X["tc"]], [X["km"]])
            B.tt("dve", kT_3[:, hp, :], X["km"].ap, X["E2"].ap, ALU.mult, [X["km"], X["E2"]], [kTt])
            B.tt("pool", khs[qs].ap, X["km"].ap, X["E4"].ap, ALU.mult, [X["km"], X["E4"]], [khs[qs]])
            yield 1
            B.tt("dve", rT_3[:, hp, :], zrs[qs].ap, X["E1"].ap, ALU.mult, [zrs[qs], X["E1"]], [rTt])
            B.stt("dve", rkrs[qs].ap, zrs[qs].ap, vcol(l, "r_k", hp), X["km"].ap, ALU.mult, ALU.mult, [zrs[qs], vec[l], X["km"]], [rkrs[qs]])
            p = psd()
            B.mm(p[:, 0:T], blk_bf, rkrs[qs].ap, [cbf, rkrs[qs]], [p])
            B.tt("dve", bonus3[:, hp, :], p[:, 0:T], zvs[qs].ap, ALU.mult, [p, zvs[qs]], [bonus])
            B.cp("pool", vbfs[qs].ap, zvs[qs].ap, [zvs[qs]], [vbfs[qs]])
            yield 1
            pt = PS[7]
            ptb = pt.ap.bitcast(BF16)
            ptb4 = ptb[:, 0:768].rearrange("p (j c n) -> p j c n", j=3, c=2)
            for j_, sbuf_ in enumerate((vbfs[qs], bhs[qs], khs[qs])):
                for c in range(2):
                    B.tr(ptb4[:, j_, c, :], sbuf_[:, c * 128:(c + 1) * 128], ident_bf, [sbuf_, cbf], [pt])
            B.cp("act", TM5[:, :, :, hp, :], ptb4, [pt], [TM])
            yield "pre6"
            bA, bB, bC = PS[4], PS[5], PS[6]
            bA4 = v3(bA.ap, 4)
            bB4 = v3(bB.ap, 4)
            bC4 = v3(bC.ap, 4)
            bks = ((PS[4], PS[5], PS[6]), (PS[1], PS[2], PS[3]))
            Q4 = Qd[qs].ap.rearrange("p (c e n) -> p c e n", c=2, e=2)
            QT4 = QTd[qs].ap.rearrange("p (c e n) -> p c e n", c=2, e=2)
            for e in range(2):
                kA, kB, kC = bks[e]
                kA4 = kA.ap.rearrange("p (c t n) -> p c t n", c=2, t=2)
                kB4 = kB.ap.rearrange("p (c t n) -> p c t n", c=2, t=2)
                kC3 = v3(kC.ap[:, 0:256], 2)
                pr = slice(e * 64, e * 64 + 64)
                for c in range(2):
                    cs_ = slice(c * 128, (c + 1) * 128)
                    B.mm(kA4[:, c, 0, :], kT_3[pr, hp, cs_], aT_3[pr, hp, cs_], [kTt, aTt], [kA])
                    B.mm(kA4[:, c, 1, :], bT_3[pr, hp, cs_], aT_3[pr, hp, cs_], [bTt, aTt], [kA])
                    B.mm(kB4[:, c, 0, :], bT_3[pr, hp, cs_], rT_3[pr, hp, cs_], [bTt, rTt], [kB])
                    B.mm(kB4[:, c, 1, :], kT_3[pr, hp, cs_], rT_3[pr, hp, cs_], [kTt, rTt], [kB])
                    B.mm(kC3[:, c, :], aT_3[pr, hp, cs_], bT_3[pr, hp, cs_], [aTt, bTt], [kC])
                amds = [AM_d[0][hp], AM_d[1][hp]]
                B.tt("dve", AM6[:, :, hp, 0, e, :], kA4[:, :, 0, :], m_su.unsqueeze(1).to_broadcast([128, 2, 128]),
                     ALU.mult, [kA, cst], amds)
                B.tt("dve", Q4[:, :, e, :], kA4[:, :, 1, :], m_su.unsqueeze(1).to_broadcast([128, 2, 128]),
                     ALU.mult, [kA, cst], [Qd[qs]])
                for c in range(2):
                    B.tt("dve", AM6[:, c, hp, 1:3, e, :], kB4[:, c, :, :], m_u.unsqueeze(1).to_broadcast([128, 2, 128]),
                         ALU.mult, [kB, cst], [amds[c]])
                B.tt("dve", QT4[:, :, e, :], kC3, m_sl.unsqueeze(1).to_broadcast([128, 2, 128]), ALU.mult, [kC, cst],
                     [QTd[qs]])
            yield 1

        def stageB(hp):
            qs = hp % 2
            bA, bB, bC = PS[4], PS[5], PS[6]
            bA4 = v3(bA.ap, 4)
            bB4 = v3(bB.ap, 4)
            bC4 = v3(bC.ap, 4)
            B.tt("dve", v3(Xd[qs].ap, 4), v3(Qd[qs].ap, 4), ident_f.unsqueeze(1).to_broadcast([128, 4, 128]), ALU.add,
                 [Qd[qs], cst], [Xd[qs]])
            B.cp("pool", Xbbs[qs].ap, Xd[qs].ap, [Xd[qs]], [Xbbs[qs]])
            Qc, QTc, Xc = v3(Qd[qs].ap, 4), v3(QTd[qs].ap, 4), v3(Xd[qs].ap, 4)
            Xb4 = v3(Xbbs[qs].ap, 4)
            for j in range(6):
                for m in range(4):
                    B.mm(bA4[:, m, :], Qc[:, m, :], QTc[:, m, :], [Qd[qs], QTd[qs]], [bA])
                if j < 5:
                    for m in range(4):
                        B.mm(bB4[:, m, :], QTc[:, m, :], Qc[:, m, :], [Qd[qs], QTd[qs]], [bB])
                B.cp("act", QTc, bA4, [bA], [QTd[qs]])
                if j < 5:
                    B.cp("act", Qc, bB4, [bB], [Qd[qs]])
                for m in range(4):
                    B.mm(bC4[:, m, :], QTc[:, m, :], Xb4[:, m, :], [QTd[qs], Xbbs[qs]], [bC])
                if j < 5:
                    B.tt("dve", Xb4, Xc, bC4, ALU.add, [Xd[qs], bC], [Xbbs[qs]])
                    B.tt("dve", Xc, Xc, bC4, ALU.add, [Xd[qs], bC], [Xd[qs]])
                else:
                    for c in range(2):
                        B.tt("dve", AM6[:, c, hp, 3, :, :], Xc[:, c * 2:c * 2 + 2, :], bC4[:, c * 2:c * 2 + 2, :],
                             ALU.add, [Xd[qs], bC], [AM_d[c][hp]])
                yield 1

        K0 = 5
        Ag = [stageA(h) for h in range(4)]
        Bg = [stageB(h) for h in range(4)]
        A_cnt = [0] * 4
        A_started = [False] * 4
        A_done = [False] * 4
        A_pre6 = [False] * 4
        B_done = [False] * 4
        ca_done = False
        rot["n"] = 2
        guard = 0
        while not (ca_done and all(A_done) and all(B_done)):
            guard += 1
            assert guard < 10000
            if not ca_done:
                ca_done = next(ca, "END") == "END"
                if ca_done:
                    rot["n"] = 4
            for h in range(4):
                if not A_started[h]:
                    ok = (h == 0) or (A_cnt[h - 1] >= K0) or A_done[h - 1]
                    if h >= 2:
                        ok = ok and (A_pre6[h - 2] or A_done[h - 2])
                    if ok:
                        A_started[h] = True
                if A_started[h] and not A_done[h]:
                    if A_pre6[h] and not A_done[h]:
                        if (not ca_done) or (h >= 2 and not B_done[h - 2]):
                            continue
                    v_ = next(Ag[h], "END")
                    if v_ == "END":
                        A_done[h] = True
                    else:
                        A_cnt[h] += 1
                        if v_ == "pre6":
                            A_pre6[h] = True
            for h in range(4):
                if A_done[h] and not B_done[h]:
                    if next(Bg[h], "END") == "END":
                        B_done[h] = True
        mark("rw_chain", l, tt)
        B.A(sm[:, 16:24], sm[:, 0:8], AF.Exp, [sm], [sm], scale=-CC)
        B.A(sm[:, 24:32], sm[:, 8:16], AF.Exp, [sm], [sm], scale=-CC)
        Pm3 = sm[:, 16:24].rearrange("p (h c) -> p h c", c=2)
        PC3 = sm[:, 24:32].rearrange("p (h c) -> p h c", c=2)
        S3_ = v3(Sst.ap, 4)
        Smid3 = v3(Smid.ap, 4)
        Smb3 = v3(Smb.ap, 4)
        allAM = [AM_d[c][h] for c in range(2) for h in range(4)]
        for c in range(2):
            cs_ = slice(c * 128, (c + 1) * 128)
            amc = AM_d[c]
            B.tt("dve", Smid3, S3_, Pm3[:, :, c:c + 1].to_broadcast([128, 4, 64]), ALU.mult, [Sst, sm], [Smid])
            B.cp("dve", Smb.ap, Smid.ap, [Smid], [Smb])
            pWe = (PS[0], PS[1])
            pU, pS = PS[2], PS[3]
            pYe = (PS[4], PS[5])
            pU3 = v3(pU.ap, 8)
            pS3 = v3(pS.ap[:, 0:256], 4)
            W0b3 = v3(W0b.ap, 8)
            Ut3 = v3(Ut.ap, 8)
            W0b4 = W0b.ap.rearrange("p (h e v) -> p h e v", h=4, e=2)
            for e in range(2):
                pr = slice(e * 64, e * 64 + 64)
                pW3 = v3(pWe[e].ap[:, 0:256], 4)
                for hp in range(4):
                    B.mm(pW3[:, hp, :], AM6[:, c, hp, 0, e, :], TM5[:, 0, c, hp, e * 64:e * 64 + 64], [amc[hp], TM],
                         [pWe[e]], start=True, stop=False)
                    B.mm(pW3[:, hp, :], aT_3[pr, hp, cs_], Smb3[pr, hp, :], [aTt, Smb], [pWe[e]], start=False, stop=True)
                B.cp("act", W0b4[:, :, e, :], pW3, [pWe[e]], [W0b])
            for hp in range(4):
                for e in range(2):
                    h = hp * 2 + e
                    B.mm(pU3[:, h, :], AM6[:, c, hp, 3, e, :], W0b3[:, h, :], [amc[hp], W0b], [pU])
            B.cp("act", Ut.ap, pU.ap, [pU], [Ut])
            for e in range(2):
                pr = slice(e * 64, e * 64 + 64)
                pY3 = v3(pYe[e].ap, 4)
                for hp in range(4):
                    h = hp * 2 + e
                    B.mm(pY3[pr, hp, :], Smb3[pr, hp, :], rT_3[pr, hp, cs_], [Smb, rTt], [pYe[e]], start=True, stop=False)
                    B.mm(pY3[pr, hp, :], Ut3[:, h, :], AM6[:, c, hp, 1, e, :], [Ut, amc[hp]], [pYe[e]], start=False,
                         stop=False)
                    B.mm(pY3[pr, hp, :], TM5[:, 0, c, hp, e * 64:e * 64 + 64], AM6[:, c, hp, 2, e, :], [TM, amc[hp]],
                         [pYe[e]], start=False, stop=True)
                B.cp("act", y3[pr, :, cs_], pY3[pr, :, :], [pYe[e]], [yb])
            for hp in range(4):
                for e in range(2):
                    h = hp * 2 + e
                    pr = slice(e * 64, e * 64 + 64)
                    B.mm(pS3[pr, hp, :], TM5[:, 1, c, hp, e * 64:e * 64 + 64], Ut3[:, h, :], [TM, Ut], [pS], start=True,
                         stop=False)
                    B.mm(pS3[pr, hp, :], TM5[:, 2, c, hp, e * 64:e * 64 + 64], TM5[:, 0, c, hp, e * 64:e * 64 + 64],
                         [TM], [pS], start=False, stop=True)
            B.tt("dve", S3_, Smid3, PC3[:, :, c:c + 1].to_broadcast([128, 4, 64]), ALU.mult, [Smid, sm], [Sst])
            B.tt("dve", S3_, S3_, pS3, ALU.add, [Sst, pS], [Sst])
        if stage == 26:
            return
        mark("rw_norm", l, tt)
        for hp in range(4):
            B.cp("pool", ybf.ap, y3[:, hp, :], [yb], [ybf])
            p = psd()
            B.mm(p[:, 0:T], blk_bf, ybf.ap, [cbf, ybf], [p])
            B.stt("dve", yc.ap, p[:, 0:T], -1.0 / 64, y3[:, hp, :], ALU.mult, ALU.add, [p, yb], [yc])
            B.A(ysq.ap, yc.ap, AF.Square, [yc], [ysq])
            p = psd()
            B.mm(p[:, 0:T], blk_bf, ysq.ap, [cbf, ysq], [p])
            B.A(sd.ap, p[:, 0:T], AF.Sqrt, [p, eps_t], [sd], scale=1.0 / 64, bias=eps_ap(LN_EPS))
            B.rcp(t1.ap, sd.ap, [sd], [t1])
            B.tt("dve", t2.ap, yc.ap, t1.ap, ALU.mult, [yc, t1], [t2])
            B.ts("dve", t1.ap, t2.ap, vcol(l, "ln_w", hp), ALU.mult, [t2, vec[l]], [t1], s2=vcol(l, "ln_b", hp),
                 op1=ALU.add)
            B.tt("dve", t2.ap, t1.ap, bonus3[:, hp, :], ALU.add, [t1, bonus], [t2])
            B.tt("dve", o_rw[:, hp * T:(hp + 1) * T], t2.ap, g3[:, hp, :], ALU.mult, [t2, gbuf], [o_rw])

    for l in range(nl):
        layer(l)
    B.emit(block)
    es.__exit__(None, None, None)
    return nc


def _consts():
    c = np.zeros((128, NCONST), np.float32)
    i = np.arange(128)
    c[:, CI_ID:CI_ID + 128] = np.eye(128)
    c[:, CI_J:CI_J + 128] = np.eye(128)[::-1]
    c[:, CI_ONE:CI_ONE + 128] = 1.0
    blk = np.zeros((128, 128), np.float32)
    blk[:64, :64] = 1
    blk[64:, 64:] = 1
    c[:, CI_BLK:CI_BLK + 128] = blk
    c[:, CI_SU:CI_SU + 128] = (i[None, :] > i[:, None])
    c[:, CI_U:CI_U + 128] = (i[None, :] >= i[:, None])
    c[:, CI_SL:CI_SL + 128] = (i[None, :] < i[:, None])
    half = 32
    invf = (1.0 / (np.float32(10000.0) ** (np.arange(half, dtype=np.float32) / np.float32(half)))).astype(np.float32)
    c[:64, CI_INVF] = np.concatenate([invf, invf])
    c[:64, CI_SGN] = np.concatenate([-np.ones(32), np.ones(32)]) * TWO_PI
    return c


def prep_shared(inp, nl=NL):
    f = lambda a: np.ascontiguousarray(np.asarray(a, dtype=np.float32))
    sh = {}
    w_in = f(inp["w_in"])[:nl]
    sh["w_in"] = w_in
    kr = w_in[:, :, 384:448]
    sh["w_in_sw"] = np.ascontiguousarray(np.concatenate([kr[:, :, 32:], kr[:, :, :32]], axis=-1))
    wuq = f(inp["mla_w_uq"])[:nl]
    sh["w_uq"] = wuq
    r4 = wuq.reshape(nl, 256, 4, 192)[:, :, :, 128:]
    sh["w_uq_sw"] = np.ascontiguousarray(np.concatenate([r4[..., 32:], r4[..., :32]], axis=-1).reshape(nl, 256, 256))
    wukv = f(inp["mla_w_ukv"])[:nl]
    sh["w_ukv"] = wukv
    nope = wukv.reshape(nl, 128, 4, 256)[:, :, :, :128]
    sh["w_ukT"] = np.ascontiguousarray(nope.transpose(0, 2, 3, 1).reshape(nl, 512, 128))
    sh["rw_w_up"] = f(inp["rw_w_up"])[:nl]
    sh["rw_a_up"] = f(inp["rw_a_up"])[:nl]
    sh["rw_g_up"] = f(inp["rw_g_up"])[:nl]
    for n in ("w_branch", "w_out", "w_ff1", "w_ff2", "w_ple_gate", "w_ple_proj"):
        sh[n] = f(inp[n])[:nl]
    vecs = np.zeros((nl, 128, NV), np.float32)

    def put(name, arr):
        a = f(arr)[:nl].reshape(nl, -1, 128)
        vecs[:, :, VC[name]:VC[name] + a.shape[1]] = a.transpose(0, 2, 1)

    put("pre_mix_g", inp["pre_mix_g"])
    put("q_g", inp["mla_q_norm_g"])
    put("kv_g", inp["mla_kv_norm_g"])
    put("mu", inp["rw_mu"])
    put("w0", inp["rw_w0"])
    put("a0", inp["rw_a0"])
    put("k_k", inp["rw_k_k"])
    put("k_a", inp["rw_k_a"])
    put("r_k", np.asarray(inp["rw_r_k"]).reshape(-1, 512))
    put("ln_w", inp["rw_ln_w"])
    put("ln_b", inp["rw_ln_b"])
    put("post_mix_g", inp["post_mix_g"])
    put("pre_ff_g", inp["pre_ff_g"])
    put("post_ff_g", inp["post_ff_g"])
    sh["vecs"] = vecs
    sh["consts"] = _consts()
    rel = f(inp["ca_rel_bias"])[:nl]
    sh["relr"] = np.ascontiguousarray(rel[:, ::-1, :].transpose(0, 2, 1))
    return sh


def prep_core(inp, b, S, nl=NL):
    d = {}
    d["xT"] = np.ascontiguousarray(np.asarray(inp["x"], dtype=np.float32)[b, :S].T)
    d["pT"] = np.ascontiguousarray(np.asarray(inp["p"], dtype=np.float32)[:nl, b, :S].transpose(0, 2, 1))
    d["pos"] = np.ascontiguousarray(np.asarray(inp["positions"]).astype(np.int32)[b:b + 1, :S])
    return d


_NC_CACHE = {}
PHASES = []


def kernel(**inputs):
    key = (SEQ, NL)
    if key not in _NC_CACHE:
        _NC_CACHE[key] = build(SEQ, NL)
    nc = _NC_CACHE[key]
    sh = prep_shared(inputs)
    in_maps = []
    for b in range(NB):
        m = dict(sh)
        m.update(prep_core(inputs, b, SEQ))
        in_maps.append(m)
    res = run_bass_kernel_spmd(nc, in_maps, core_ids=list(range(NB)))
    out = np.stack([np.asarray(r["outT"]).T for r in res.results], axis=0)
    return np.ascontiguousarray(out.astype(np.float32))
```
